# Optimizing a Trainium2 kernel written in Bass

```python
import jax, jax.numpy as jnp
from jax import lax
import numpy as np

D_MODEL = 1024
BATCH = 4
SEQ = 4096
DEPTH = 1
DEC_BATCH = 32
DEC_SEQ = 1
PAST_LEN = 8192
PAGE_SIZE = 128

HEAD_DIM = 64
N_ATTN_HEADS = 8
N_RWKV_HEADS = 8
ATTN_WIDTH = N_ATTN_HEADS * HEAD_DIM
RWKV_WIDTH = N_RWKV_HEADS * HEAD_DIM
MIX_WIDTH = ATTN_WIDTH + RWKV_WIDTH
MOBA_BLOCK = 256
MOBA_TOP_K = 3
Q_CHUNK = 16
DECAY_LORA = 64
AAA_LORA = 64
GATE_LORA = 128
FFN_HIDDEN = 4 * D_MODEL
RMS_EPS = 1e-6
GN_EPS = 64e-5
NEG_INF = -1e30
ATTN_COLS = 3 * ATTN_WIDTH
RWKV_COLS = 3 * RWKV_WIDTH + DECAY_LORA + AAA_LORA + GATE_LORA
IN_COLS = ATTN_COLS + RWKV_COLS
RW_SPLITS = (RWKV_WIDTH, RWKV_WIDTH + DECAY_LORA, 2 * RWKV_WIDTH + DECAY_LORA,
             3 * RWKV_WIDTH + DECAY_LORA, 3 * RWKV_WIDTH + DECAY_LORA + AAA_LORA)

kernel_name = 'hymba_rwkv7_moba_alibi_decode_step'


def rms_norm(x, g):
    xf = x.astype(jnp.float32)
    y = xf * lax.rsqrt(jnp.mean(xf * xf, axis=-1, keepdims=True) + RMS_EPS)
    return (y * g.astype(jnp.float32)).astype(x.dtype)


def alibi_slopes():
    return jnp.exp2(-8.0 * jnp.arange(1, N_ATTN_HEADS + 1, dtype=jnp.float32) / N_ATTN_HEADS)


def moba_query_block(q, q_pos, k_blk, v_blk, k_mean, slopes, n_top):
    B, Qc, H, D = q.shape
    nb = k_mean.shape[2]
    q_blk = q_pos // MOBA_BLOCK
    gate = jnp.einsum('bqhd,bhnd->bhqn', q, k_mean.astype(q.dtype), preferred_element_type=jnp.float32)
    fully_past = jnp.arange(nb, dtype=jnp.int32)[None, :] < q_blk[:, None]
    gate = jnp.where(fully_past[None, None], gate, NEG_INF)
    _, top_idx = lax.top_k(gate, n_top)
    top_idx = top_idx.astype(jnp.int32)
    own = jnp.broadcast_to(q_blk[None, None, :, None], (B, H, Qc, 1))
    sel = jnp.concatenate([top_idx, own], axis=-1)
    sel_ok = jnp.concatenate([top_idx < q_blk[None, None, :, None],
                              jnp.ones((B, H, Qc, 1), dtype=bool)], axis=-1)
    bi = jnp.arange(B)[:, None, None, None]
    hi = jnp.arange(H)[None, :, None, None]
    k_sel = k_blk[bi, hi, sel]
    v_sel = v_blk[bi, hi, sel]
    key_pos = sel[..., None] * MOBA_BLOCK + jnp.arange(MOBA_BLOCK, dtype=jnp.int32)
    dist = q_pos[None, None, :, None, None] - key_pos
    s = jnp.einsum('bqhd,bhqskd->bhqsk', q, k_sel, preferred_element_type=jnp.float32) * (HEAD_DIM ** -0.5)
    s = s - slopes[None, :, None, None, None] * dist.astype(jnp.float32)
    s = jnp.where(sel_ok[..., None] & (dist >= 0), s, NEG_INF)
    n_sel = sel.shape[-1]
    p = jax.nn.softmax(s.reshape(B, H, Qc, n_sel * MOBA_BLOCK), axis=-1).reshape(s.shape)
    return jnp.einsum('bhqsk,bhqskd->bqhd', p.astype(v_sel.dtype), v_sel)


def moba_attention(q, k, v, q_pos):
    B, T, H, D = k.shape
    nb = -(-T // MOBA_BLOCK)
    pad = nb * MOBA_BLOCK - T
    k_blk = jnp.pad(k, ((0, 0), (0, pad), (0, 0), (0, 0))).reshape(B, nb, MOBA_BLOCK, H, D).transpose(0, 3, 1, 2, 4)
    v_blk = jnp.pad(v, ((0, 0), (0, pad), (0, 0), (0, 0))).reshape(B, nb, MOBA_BLOCK, H, D).transpose(0, 3, 1, 2, 4)
    k_mean = jnp.mean(k_blk.astype(jnp.float32), axis=3)
    slopes = alibi_slopes()
    n_top = min(MOBA_TOP_K, nb)
    Qn = q.shape[1]
    qc = Q_CHUNK if Qn % Q_CHUNK == 0 else Qn
    nc = Qn // qc
    q_c = q.reshape(B, nc, qc, H, D).transpose(1, 0, 2, 3, 4)
    pos_c = q_pos.reshape(nc, qc)
    out = lax.map(lambda a: moba_query_block(a[0], a[1], k_blk, v_blk, k_mean, slopes, n_top), (q_c, pos_c))
    return out.transpose(1, 0, 2, 3, 4).reshape(B, Qn, H, D)


def rwkv7_time_mix(p, p_prev, s0, mu_shift, decay_w0, decay_up, iclr_a0, iclr_up, gate_up,
                   k_k, k_a, r_k, ln_x_w, ln_x_b):
    B, T, _ = p.shape
    H, N = N_RWKV_HEADS, HEAD_DIM
    f32 = jnp.float32
    xs = (p + mu_shift * (p_prev - p)).astype(f32)
    r, xw, k, v, xa, xg = jnp.split(xs, RW_SPLITS, axis=-1)
    w = decay_w0.astype(f32) + jnp.tanh(xw) @ decay_up.astype(f32)
    w = -jax.nn.softplus(-w) - 0.5
    decay = jnp.exp(-jnp.exp(w))
    a = jax.nn.sigmoid(iclr_a0.astype(f32) + xa @ iclr_up.astype(f32))
    g = jax.nn.sigmoid(xg) @ gate_up.astype(f32)
    kk = (k * k_k.astype(f32)).reshape(B, T, H, N)
    kk = kk * lax.rsqrt(jnp.maximum(jnp.sum(kk * kk, axis=-1, keepdims=True), 1e-24))
    k = k * (1.0 + (a - 1.0) * k_a.astype(f32))
    heads = lambda t: t.reshape(B, T, H, N)
    r_h, k_h, v_h, w_h, a_h = heads(r), heads(k), heads(v), heads(decay), heads(a)

    def step(S, inp):
        r_t, w_t, k_t, v_t, kk_t, a_t = inp
        sa = jnp.einsum('bhij,bhj->bhi', S, -kk_t)
        S = S * w_t[:, :, None, :] + sa[..., None] * (kk_t * a_t)[:, :, None, :] + v_t[..., None] * k_t[:, :, None, :]
        return S, jnp.einsum('bhij,bhj->bhi', S, r_t)

    xs_t = tuple(jnp.moveaxis(t, 1, 0) for t in (r_h, w_h, k_h, v_h, kk, a_h))
    s_fin, y = lax.scan(step, s0.astype(f32), xs_t)
    y = jnp.moveaxis(y, 0, 1)
    mean = jnp.mean(y, axis=-1, keepdims=True)
    var = jnp.mean(jnp.square(y - mean), axis=-1, keepdims=True)
    y = (y - mean) * lax.rsqrt(var + GN_EPS) * ln_x_w.astype(f32).reshape(H, N) + ln_x_b.astype(f32).reshape(H, N)
    y = y + jnp.sum(r_h * k_h * r_k.astype(f32), axis=-1, keepdims=True) * v_h
    out = y.reshape(B, T, H * N) * g
    return out.astype(p.dtype), s_fin


def decoder_layer(x, k_past, v_past, wkv0, shift0, norm_mix_g, w_in, mu_shift, decay_w0, decay_up,
                  iclr_a0, iclr_up, gate_up, k_k, k_a, r_k, ln_x_w, ln_x_b, w_out,
                  norm_ffn_g, w_ffn_up, w_ffn_down):
    B, T, _ = x.shape
    P = k_past.shape[1]
    xn = rms_norm(x, norm_mix_g)
    proj = xn @ w_in
    p_attn, p_rw = proj[..., :ATTN_COLS], proj[..., ATTN_COLS:]
    q, k_new, v_new = [t.reshape(B, T, N_ATTN_HEADS, HEAD_DIM) for t in jnp.split(p_attn, 3, axis=-1)]
    k_all = jnp.concatenate([k_past.astype(k_new.dtype), k_new], axis=1)
    v_all = jnp.concatenate([v_past.astype(v_new.dtype), v_new], axis=1)
    q_pos = P + jnp.arange(T, dtype=jnp.int32)
    attn = moba_attention(q, k_all, v_all, q_pos).reshape(B, T, ATTN_WIDTH)
    p_prev = jnp.concatenate([shift0[:, None, :].astype(p_rw.dtype), p_rw[:, :-1]], axis=1)
    rw, wkv_fin = rwkv7_time_mix(p_rw, p_prev, wkv0, mu_shift, decay_w0, decay_up, iclr_a0, iclr_up,
                                 gate_up, k_k, k_a, r_k, ln_x_w, ln_x_b)
    h = x + jnp.concatenate([attn, rw], axis=-1) @ w_out
    u = jax.nn.relu(rms_norm(h, norm_ffn_g) @ w_ffn_up)
    h = h + (u * u) @ w_ffn_down
    return h, k_new, v_new, wkv_fin, p_rw[:, -1]


def setup_inputs(seed: int = 0) -> dict:
    key = jax.random.key(seed)
    ks = jax.random.split(key, 24)
    f32 = jnp.float32
    n_pages = PAST_LEN // PAGE_SIZE
    n_used = DEC_BATCH * n_pages
    n_pool = n_used + n_used // 4
    nrm = lambda k, shape, s: s * jax.random.normal(k, shape, f32)
    uni = lambda k, shape, lo, hi: jax.random.uniform(k, shape, f32, lo, hi)
    L = DEPTH
    return {
        'x_prompt': nrm(ks[0], (BATCH, SEQ, D_MODEL), 1.0),
        'x_sample': nrm(ks[1], (DEC_BATCH, DEC_SEQ, D_MODEL), 1.0),
        'cache_k': nrm(ks[2], (L, n_pool, PAGE_SIZE, N_ATTN_HEADS, HEAD_DIM), 1.0),
        'cache_v': nrm(ks[3], (L, n_pool, PAGE_SIZE, N_ATTN_HEADS, HEAD_DIM), 1.0),
        'page_table': jax.random.permutation(ks[4], n_pool)[:n_used].reshape(DEC_BATCH, n_pages).astype(jnp.int32),
        'state_wkv': nrm(ks[5], (L, DEC_BATCH, N_RWKV_HEADS, HEAD_DIM, HEAD_DIM), 0.5),
        'state_shift': nrm(ks[6], (L, DEC_BATCH, RWKV_COLS), 1.0),
        'norm_mix_g': 1.0 + nrm(ks[7], (L, D_MODEL), 0.02),
        'w_in': nrm(ks[8], (L, D_MODEL, IN_COLS), D_MODEL ** -0.5),
        'mu_shift': uni(ks[9], (L, RWKV_COLS), 0.1, 0.9),
        'decay_w0': uni(ks[10], (L, RWKV_WIDTH), -5.0, 0.0),
        'decay_up': nrm(ks[11], (L, DECAY_LORA, RWKV_WIDTH), 0.1),
        'iclr_a0': nrm(ks[12], (L, RWKV_WIDTH), 0.1),
        'iclr_up': nrm(ks[13], (L, AAA_LORA, RWKV_WIDTH), 0.1),
        'gate_up': nrm(ks[14], (L, GATE_LORA, RWKV_WIDTH), GATE_LORA ** -0.5),
        'k_k': 0.85 + nrm(ks[15], (L, RWKV_WIDTH), 0.05),
        'k_a': 1.0 + nrm(ks[16], (L, RWKV_WIDTH), 0.05),
        'r_k': nrm(ks[17], (L, N_RWKV_HEADS, HEAD_DIM), 0.1),
        'ln_x_w': 1.0 + nrm(ks[18], (L, RWKV_WIDTH), 0.02),
        'ln_x_b': nrm(ks[19], (L, RWKV_WIDTH), 0.02),
        'w_out': nrm(ks[20], (L, MIX_WIDTH, D_MODEL), MIX_WIDTH ** -0.5),
        'norm_ffn_g': 1.0 + nrm(ks[21], (L, D_MODEL), 0.02),
        'w_ffn_up': nrm(ks[22], (L, D_MODEL, FFN_HIDDEN), D_MODEL ** -0.5),
        'w_ffn_down': nrm(ks[23], (L, FFN_HIDDEN, D_MODEL), FFN_HIDDEN ** -0.5),
        'norm_final_g': 1.0 + nrm(jax.random.fold_in(key, 99), (D_MODEL,), 0.02),
    }


def reference(x_prompt, x_sample, cache_k, cache_v, page_table, state_wkv, state_shift,
              norm_mix_g, w_in, mu_shift, decay_w0, decay_up, iclr_a0, iclr_up, gate_up,
              k_k, k_a, r_k, ln_x_w, ln_x_b, w_out, norm_ffn_g, w_ffn_up, w_ffn_down, norm_final_g):
    n_seq, n_pages = page_table.shape
    b_p = x_prompt.shape[0]
    hp, hs = x_prompt, x_sample
    kp_l, vp_l, wp_l, sp_l, ks_l, vs_l, ws_l, ss_l = [], [], [], [], [], [], [], []
    for l in range(DEPTH):
        lw = (norm_mix_g[l], w_in[l], mu_shift[l], decay_w0[l], decay_up[l], iclr_a0[l], iclr_up[l],
              gate_up[l], k_k[l], k_a[l], r_k[l], ln_x_w[l], ln_x_b[l], w_out[l],
              norm_ffn_g[l], w_ffn_up[l], w_ffn_down[l])
        empty = jnp.zeros((b_p, 0, N_ATTN_HEADS, HEAD_DIM), hp.dtype)
        wkv0 = jnp.zeros((b_p, N_RWKV_HEADS, HEAD_DIM, HEAD_DIM), jnp.float32)
        sh0 = jnp.zeros((b_p, RWKV_COLS), hp.dtype)
        hp, kp, vp, wp, sp = decoder_layer(hp, empty, empty, wkv0, sh0, *lw)
        k_past = cache_k[l][page_table].reshape(n_seq, n_pages * PAGE_SIZE, N_ATTN_HEADS, HEAD_DIM)
        v_past = cache_v[l][page_table].reshape(n_seq, n_pages * PAGE_SIZE, N_ATTN_HEADS, HEAD_DIM)
        hs, kn, vn, wn, sn = decoder_layer(hs, k_past, v_past, state_wkv[l], state_shift[l], *lw)
        kp_l.append(kp); vp_l.append(vp); wp_l.append(wp); sp_l.append(sp)
        ks_l.append(kn); vs_l.append(vn); ws_l.append(wn); ss_l.append(sn)
    y_prompt = rms_norm(hp, norm_final_g)
    y_sample = rms_norm(hs, norm_final_g)
    return (y_prompt, y_sample,
            jnp.stack(kp_l), jnp.stack(vp_l), jnp.stack(wp_l), jnp.stack(sp_l),
            jnp.stack(ks_l), jnp.stack(vs_l), jnp.stack(ws_l), jnp.stack(ss_l))
```

```python
import contextlib
import numpy as np
import ml_dtypes
import concourse.bass as bass
import concourse.mybir as mybir
from concourse.bass_utils import run_bass_kernel_spmd

F32 = mybir.dt.float32; BF16 = mybir.dt.bfloat16; I32 = mybir.dt.int32
AF = mybir.ActivationFunctionType; ALU = mybir.AluOpType; AX = mybir.AxisListType
T = 4096; D = 1024; NS = 4; NPG = 64
RMS_EPS = 1e-6; GN_EPS = 64e-5
NBIG = 30000.0


class K:
    def __init__(s, nc, same=True):
        s.nc = nc
        s.eng = {'pe': nc.tensor, 'act': nc.scalar, 'dve': nc.vector, 'pool': nc.gpsimd, 'sp': nc.sync}
        s.csem = {}; s.cnt = {}; s.nsem = 0
        for n in s.eng:
            s._newsem(n)
        s.lastw = {}; s.reads = {}
        s.waited = {n: {} for n in s.eng}
        s.same = same
        s.dpool = {}; s.dpos = {}
        s.ninst = 0

    def _newsem(s, n):
        s.nsem += 1
        s.csem[n] = s.nc.alloc_semaphore('c%d_%s' % (s.nsem, n)); s.cnt[n] = 0

    def _need(s, e, reads, writes, extra=()):
        evs = list(extra)
        for r in reads:
            if r in s.lastw: evs.append(s.lastw[r])
        for w in writes:
            if w in s.lastw: evs.append(s.lastw[w])
            evs.extend(s.reads.get(w, ()))
        best = {}
        for (h, v, en) in evs:
            if en == e and (not s.same or e in ('pe', 'sp')): continue
            k = id(h)
            if k not in best or best[k][1] < v: best[k] = (h, v)
        for k, (h, v) in best.items():
            if s.waited[e].get(k, 0) >= v: continue
            s.eng[e].wait_ge(h, v); s.waited[e][k] = v; s.ninst += 1

    def _record(s, ev, reads, writes):
        for w in writes:
            s.lastw[w] = ev; s.reads[w] = []
        for r in reads:
            lst = s.reads.setdefault(r, [])
            lst.append(ev)
            if len(lst) > 16:
                m = {}
                for (h, v, en) in lst:
                    if id(h) not in m or m[id(h)][1] < v: m[id(h)] = (h, v, en)
                s.reads[r] = list(m.values())

    def op(s, e, fn, reads=(), writes=()):
        pr = [r for r in reads if isinstance(r, str) and r.startswith('pb')]
        if pr:
            reads = [r for r in reads if r not in pr]; writes = list(writes) + pr
        s._need(e, reads, writes)
        if s.cnt[e] >= 30000: s._newsem(e)
        ins = fn(s.eng[e])
        s.cnt[e] += 1; s.ninst += 1
        ins.then_inc(s.csem[e], 1)
        ev = (s.csem[e], s.cnt[e], e)
        s._record(ev, reads, writes)
        return ev

    def dma(s, q, out=None, in_=None, reads=(), writes=(), fn=None):
        if q not in s.dpool:
            s.dpool[q] = [[s.nc.alloc_semaphore('d_%s%d' % (q, i)), 0] for i in range(16)]; s.dpos[q] = 0
        slot = s.dpool[q][s.dpos[q] % 16]; s.dpos[q] += 1
        extra = [(slot[0], slot[1], None)] if slot[1] > 0 else []
        s._need(q, reads, writes, extra)
        if fn is None:
            ins = s.eng[q].dma_start(out=out, in_=in_)
        else:
            ins = fn(s.eng[q])
        slot[1] += 16; s.ninst += 1
        ins.then_inc(slot[0], 16)
        ev = (slot[0], slot[1], None)
        s._record(ev, reads, writes)
        return ev

    def barrier(s):
        evs = [(s.csem[n], s.cnt[n], n) for n in s.eng if s.cnt[n] > 0]
        for q in s.dpool:
            for slot in s.dpool[q]:
                if slot[1] > 0: evs.append((slot[0], slot[1], None))
        for e in s.eng:
            best = {}
            for (h, v, en) in evs:
                if en == e: continue
                best[id(h)] = (h, v)
            for kk_, (h, v) in best.items():
                if s.waited[e].get(kk_, 0) >= v: continue
                s.eng[e].wait_ge(h, v); s.waited[e][kk_] = v; s.ninst += 1

    def wait_all(s, e):
        evs = list(s.lastw.values())
        for q in s.dpool:
            for slot in s.dpool[q]:
                if slot[1] > 0: evs.append((slot[0], slot[1], None))
        s._need(e, (), (), evs)


def make_consts():
    bf = ml_dtypes.bfloat16
    c = {}
    c['identf'] = np.eye(128, dtype=np.float32)
    c['identb'] = np.eye(128, dtype=np.float32).astype(bf)
    p = np.arange(128)[:, None]; dl = np.arange(512)[None, :]
    cm = np.zeros((128, 4, 512), np.float32)
    for j in range(4):
        cm[:, j, :] = np.where(dl >= 128 * j + p, 0.0, -NBIG)
    c['cmask'] = cm.astype(bf)
    s = np.arange(T)
    ka = np.zeros((18, T), np.float32)
    for n in range(16):
        ka[n] = (s // 256 == n)
    ka[16] = 1; ka[17] = 1
    c['kaug'] = ka.astype(bf)
    slopes = 2.0 ** (-np.arange(1, 9, dtype=np.float64))
    qa = np.zeros((2, 8, 512), np.float32)
    d = np.arange(512)
    for h in range(8):
        qa[0, h] = -slopes[h] * (d % 256)
        qa[1, h] = -slopes[h] * 256 * (d // 256)
    c['qaug'] = qa.astype(bf)
    ab = np.zeros((128, 8, 36), np.float32)
    for h in range(8):
        for r in range(36):
            ab[:, h, r] = slopes[h] * (128 * (r - 28) + np.arange(128))
    c['ab'] = ab
    gm = np.zeros((17, 16), np.float32); oh = np.zeros((17, 16), np.float32)
    for nq in range(17):
        gm[nq, nq:] = -1e30
        if nq < 16: oh[nq, nq] = 1
    c['gmask'] = np.broadcast_to(gm[None], (128, 17, 16)).copy()
    c['ownhot'] = np.broadcast_to(oh[None], (128, 17, 16)).copy()
    al = np.zeros((128, 65, 8), np.float32)
    for h in range(8):
        for pg in range(64):
            al[:, pg, h] = -slopes[h] * (8192 - (128 * pg + np.arange(128)))
        al[:, 64, h] = -NBIG; al[0, 64, h] = 0.0
    c['alis'] = al
    dm = np.zeros((8, 512), np.float32)
    for h in range(8): dm[h, h * 64:(h + 1) * 64] = 1
    c['diagm'] = dm
    s4 = np.zeros((4, 4, 128), np.float32)
    for i in range(4): s4[i, i, :] = 1
    c['sel4'] = s4
    o4 = np.zeros((128, 4, 4), np.float32)
    for i in range(4): o4[:, i, i] = 1
    c['oh4'] = o4
    s8 = np.zeros((8, 4, 4), np.float32)
    for i in range(4): s8[:, i, i] = 1
    c['sel8'] = s8
    c['iotaf'] = np.arange(128, dtype=np.float32)[:, None].copy()
    return c


def build(n_pool):
    import os
    KSTOP = int(os.environ.get('KSTOP', '9')); NT = int(os.environ.get('KNT', '32'))
    PT = list(range(NT))
    nc = bass.Bass("TRN2", target_bir_lowering=False)
    consts = make_consts()
    dt_of = lambda a: BF16 if a.dtype == ml_dtypes.bfloat16 else (I32 if a.dtype == np.int32 else F32)

    def din(name, shape, dt=F32):
        return nc.dram_tensor(name, list(shape), dt, kind="ExternalInput").ap()

    def dout(name, shape, dt=F32):
        return nc.dram_tensor(name, list(shape), dt, kind="ExternalOutput").ap()

    def dscr(name, shape, dt=F32):
        return nc.dram_tensor(name, list(shape), dt).ap()

    xp = din("xp", [T, D]); xsm = din("xsm", [NS, D])
    ck = din("ck", [n_pool * 128, 512]); cv = din("cv", [n_pool * 128, 512])
    pt = din("pt", [NS, NPG], I32)
    swkv = din("swkv", [NS, 8, 64, 64]); sshift = din("sshift", [NS, 1792])
    w_in = din("w_in", [D, 3328]); w_out = din("w_out", [D, D]); w_up = din("w_up", [D, 4096]); w_dn = din("w_dn", [4096, D])
    g1 = din("g1", [D]); g2 = din("g2", [D]); gf = din("gf", [D])
    mu = din("mu", [1792]); w0 = din("w0", [512]); decay_up = din("decay_up", [64, 512])
    a0 = din("a0", [512]); iclr_up = din("iclr_up", [64, 512]); gate_up = din("gate_up", [128, 512])
    k_k = din("k_k", [512]); k_a = din("k_a", [512]); r_k = din("r_k", [512]); lnw = din("lnw", [512]); lnb = din("lnb", [512])
    cin = {n: din("c_" + n, a.shape, dt_of(a)) for n, a in consts.items()}

    yp = dout("yp", [T, D]); ysm = dout("ysm", [NS, D])
    kp = dout("kp", [T, 512]); vp = dout("vp", [T, 512])
    wkvp = dout("wkvp", [8, 64, 64]); shp = dout("shp", [1, 1792])
    ks = dout("ks", [NS, 512]); vs = dout("vs", [NS, 512])
    wkvs = dout("wkvs", [NS, 8, 64, 64]); shs = dout("shs", [NS, 1792])

    TT = T + 128
    P = dscr("P", [TT + 1, 1792])
    Bs = dscr("Bs", [TT, 512]); KMs = dscr("KMs", [TT, 512]); Vs = dscr("Vs", [TT, 512]); Gs = dscr("Gs", [TT, 512])
    BON = dscr("BON", [TT, 8])
    Ysc = dscr("Ysc", [T + 2, 512])
    Yss = dscr("Yss", [NS, 2, 512])
    mixT = dscr("mixT", [D, TT], BF16)
    QKVs = dscr("QKVs", [NS, 1536])

    k = K(nc)
    top = contextlib.ExitStack()
    top.enter_context(nc.allow_non_contiguous_dma(reason='small strided parameter loads'))
    def SB(es, name, shape, dt): return es.enter_context(nc.sbuf_tensor(name, list(shape), dt))
    pb = [top.enter_context(nc.psum_tensor("pb%d" % i, [128, 512], F32)) for i in range(7)]
    pbT = top.enter_context(nc.psum_tensor("pbT", [128, 1024], BF16))
    identf = SB(top, "identf", [128, 128], F32); identb = SB(top, "identb", [128, 128], BF16)
    zero_sb = SB(top, "zero_sb", [128, 16], F32)
    ones_sb = SB(top, "ones_sb", [128, 64], F32)
    k.dma('sp', identf[:], cin['identf'][:, :], writes=['identf'])
    k.dma('sp', identb[:], cin['identb'][:, :], writes=['identb'])
    k.op('pool', lambda e: e.memset(zero_sb[:], 0.0), writes=['zero_sb'])
    k.op('pool', lambda e: e.memset(ones_sb[:], 1.0), writes=['ones_sb'])
    k.dma('sp', P[0:1, :].rearrange("o (a b) -> (o a) b", b=16), zero_sb[0:112, 0:16], reads=['zero_sb'], writes=['P'])

    rr = [0]
    def evac(out, in_, reads, writes, scale=None):
        rr[0] += 1
        if rr[0] % 2 == 0 and scale is None:
            return k.op('dve', lambda e: e.tensor_copy(out=out, in_=in_), reads=reads, writes=writes)
        if scale is None:
            return k.op('act', lambda e: e.activation(out=out, in_=in_, func=AF.Copy), reads=reads, writes=writes)
        return k.op('act', lambda e: e.activation(out=out, in_=in_, func=AF.Copy, scale=scale), reads=reads, writes=writes)

    def rmsnorm_T(es_tensors, src_tile, srcname, nm):
        ss, rs, xnb, xnT = es_tensors
        k.op('act', lambda e: e.activation(out=xnb[:], in_=src_tile[:], func=AF.Square, accum_out=ss[:, 0:1]), reads=[srcname], writes=['xnb' + nm, 'ss' + nm])
        k.op('act', lambda e: e.activation(out=rs[:], in_=ss[:], func=AF.Sqrt, scale=1.0 / D, bias=RMS_EPS), reads=['ss' + nm], writes=['rs' + nm])
        k.op('dve', lambda e: e.reciprocal(out=rs[:], in_=rs[:]), reads=['rs' + nm], writes=['rs' + nm])
        k.op('dve', lambda e: e.tensor_scalar(out=xnb[:], in0=src_tile[:], scalar1=rs[:, 0:1], scalar2=None, op0=ALU.mult), reads=[srcname, 'rs' + nm], writes=['xnb' + nm])
        for kc in range(8):
            k.op('pe', lambda e, kc=kc: e.transpose(out=pbT[:, kc * 128:(kc + 1) * 128], in_=xnb[:, kc * 128:(kc + 1) * 128], identity=identb[:]),
                 reads=['xnb' + nm, 'identb'], writes=['pbT'])
        k.op('act', lambda e: e.activation(out=xnT[:].rearrange("p a b -> p (a b)"), in_=pbT[:, :], func=AF.Copy), reads=['pbT'], writes=['xnT' + nm])

    def load_weight(wsb, wdram, nkc, ncols, gdram, stg, es_name, cs=3328, lo=None):
        if gdram is not None:
            k.dma('sp', gcol[:, 0:nkc], gdram.rearrange("(a p) -> p a", p=128), writes=['gcol'])
        for kc in range(nkc):
            for c0 in range(0, ncols, cs):
                cw = min(cs, ncols - c0)
                k.dma('sp', stg[:, 0:cw], wdram[kc * 128:(kc + 1) * 128, c0:c0 + cw], writes=['stg'])
                if gdram is not None and lo is not None and c0 == 0:
                    k.op('dve', lambda e, kc=kc, cw=cw: e.tensor_scalar(out=stg[:, 0:cw], in0=stg[:, 0:cw], scalar1=gcol[:, kc:kc + 1], scalar2=None, op0=ALU.mult), reads=['stg', 'gcol'], writes=['stg'])
                    k.op('dve', lambda e, kc=kc, cw=cw: e.tensor_copy(out=wsb[:, kc, 0:cw], in_=stg[:, 0:cw]), reads=['stg'], writes=[es_name])
                    k.op('dve', lambda e, kc=kc: e.tensor_tensor(out=lo[:, kc, :], in0=stg[:, 0:512], in1=wsb[:, kc, 0:512], op=ALU.subtract), reads=['stg', es_name], writes=['wlo'])
                elif gdram is not None:
                    k.op('dve', lambda e, kc=kc, c0=c0, cw=cw: e.tensor_scalar(out=wsb[:, kc, c0:c0 + cw], in0=stg[:, 0:cw], scalar1=gcol[:, kc:kc + 1], scalar2=None, op0=ALU.mult),
                         reads=['stg', 'gcol'], writes=[es_name])
                else:
                    k.op('dve', lambda e, kc=kc, c0=c0, cw=cw: e.tensor_copy(out=wsb[:, kc, c0:c0 + cw], in_=stg[:, 0:cw]), reads=['stg'], writes=[es_name])

    gcol = SB(top, "gcol", [128, 8], F32)

    with contextlib.ExitStack() as es:
        win = SB(es, "win", [128, 8, 3328], BF16)
        kTa = [SB(es, "kTa%d" % h, [82, T], BF16) for h in range(8)]
        vaug = SB(es, "vaug", [128, 32, 8, 65], BF16)
        qTa = SB(es, "qTa", [82, 8, 512], BF16)
        qT32 = SB(es, "qT32", [64, 8, 128], F32)
        ksum2 = SB(es, "ksum2", [64, 8, 32], F32)
        kmT = SB(es, "kmT", [64, 8, 16], F32)
        xt = SB(es, "xt", [128, D], F32)
        ss = SB(es, "ss", [128, 1], F32); rs = SB(es, "rs", [128, 1], F32)
        xnb = SB(es, "xnb", [128, D], BF16); xnT = SB(es, "xnT", [128, 8, 128], BF16)
        xlo = SB(es, "xlo", [128, D], BF16); xloT = SB(es, "xloT", [128, 8, 128], BF16)
        wlo = SB(es, "wlo", [128, 8, 512], BF16)
        proj = SB(es, "proj", [128, 1792], F32)
        cmask = SB(es, "cmask", [128, 4, 512], BF16)
        ab = SB(es, "ab", [128, 8, 36], F32)
        gmaskc = SB(es, "gmaskc", [128, 17, 16], F32); ownhot = SB(es, "ownhot", [128, 17, 16], F32)
        gm = SB(es, "gm", [128, 8, 16], F32); m01 = gm
        top8 = SB(es, "top8", [128, 8, 8], F32); thr = SB(es, "thr", [128, 8], F32)
        selpad = SB(es, "selpad", [128, 8, 80], F32)
        pT = [SB(es, "pT%d" % i, [128, 512], BF16) for i in range(2)]
        stmp = SB(es, "stmp", [128, 512], F32)
        osb = stmp; lsb = stmp
        atT = [SB(es, "atT%d" % i, [64, 512], BF16) for i in range(2)]

        k.dma('sp', cmask[:], cin['cmask'][:, :, :], writes=['cmask'])
        k.dma('sp', ab[:], cin['ab'][:, :, :], writes=['ab'])
        k.dma('sp', gmaskc[:], cin['gmask'][:, :, :], writes=['gmaskc'])
        k.dma('sp', ownhot[:], cin['ownhot'][:, :, :], writes=['ownhot'])
        for h in range(8):
            k.dma('sp', kTa[h][64:82, :], cin['kaug'][:, :], writes=[('kTa', h)])
        k.dma('sp', qTa[80:82, :, :], cin['qaug'][:, :, :], writes=['qTa'])
        k.op('pool', lambda e: e.memset(vaug[:].rearrange("p a b c -> p (a b c)"), 1.0), writes=['vaug'])
        k.op('pool', lambda e: e.memset(kmT[:].rearrange("p a b -> p (a b)"), 0.0), writes=['kmT'])
        k.op('pool', lambda e: e.memset(selpad[:].rearrange("p a b -> p (a b)"), 0.0), writes=['selpad'])
        load_weight(win, w_in, 8, 3328, g1, proj, 'win', cs=1664, lo=wlo)

        chunks = [(0, 512), (512, 512), (1024, 512), (1536, 512), (2048, 512), (2560, 512), (3072, 256)]
        for i in PT + [32]:
            samp = (i == 32)
            if samp:
                k.op('pool', lambda e: e.memset(xt[:], 0.0), writes=['xt'])
                k.dma('sp', xt[0:NS, :], xsm[:, :], reads=['xt'], writes=['xt'])
            else:
                k.dma('sp', xt[:], xp[i * 128:(i + 1) * 128, :], writes=['xt'])
            rmsnorm_T((ss, rs, xnb, xnT), xt, 'xt', '1')
            k.op('dve', lambda e: e.scalar_tensor_tensor(out=xlo[:], in0=xt[:], scalar=rs[:, 0:1], in1=xnb[:], op0=ALU.mult, op1=ALU.subtract), reads=['xt', 'rs1', 'xnb1'], writes=['xlo'])
            for kc in range(8):
                k.op('pe', lambda e, kc=kc: e.transpose(out=pbT[:, kc * 128:(kc + 1) * 128], in_=xlo[:, kc * 128:(kc + 1) * 128], identity=identb[:]), reads=['xlo', 'identb'], writes=['pbT'])
            k.op('act', lambda e: e.activation(out=xloT[:].rearrange("p a b -> p (a b)"), in_=pbT[:, :], func=AF.Copy), reads=['pbT'], writes=['xloT'])
            def pcol(ci):
                return (chunks[ci][0] if ci < 3 else chunks[ci][0] - 1536), (('proj', ci % 3) if ci < 6 else ('proj', 3))
            for ci, (c0, cw) in enumerate(chunks):
                bank = pb[ci % 2]; bn = 'pb%d' % (ci % 2)
                if ci == 0 and not samp:
                    continue
                for kc in range(8):
                    k.op('pe', lambda e, kc=kc, c0=c0, cw=cw, bank=bank: e.matmul(bank[:, 0:cw], xnT[:, kc, :], win[:, kc, c0:c0 + cw], start=(kc == 0), stop=(kc == 7 and ci != 0)),
                         reads=['xnT1', 'win'], writes=[bn])
                if ci == 0:
                    for kc in range(8):
                        k.op('pe', lambda e, kc=kc, bank=bank: e.matmul(bank[:, 0:512], xnT[:, kc, :], wlo[:, kc, :], start=False, stop=False), reads=['xnT1', 'wlo'], writes=[bn])
                    for kc in range(8):
                        k.op('pe', lambda e, kc=kc, bank=bank: e.matmul(bank[:, 0:512], xloT[:, kc, :], win[:, kc, 0:512], start=False, stop=(kc == 7)), reads=['xloT', 'win'], writes=[bn])
                pc0, pres = pcol(ci)
                evac(proj[:, pc0:pc0 + cw], bank[:, 0:cw], [bn], [pres])
                if ci == 2:
                    pr3 = [('proj', 0), ('proj', 1), ('proj', 2)]
                    if not samp:
                        k.dma('sp', kp[i * 128:(i + 1) * 128, :], proj[:, 512:1024], reads=[('proj', 1)], writes=['kp'])
                        k.dma('sp', vp[i * 128:(i + 1) * 128, :], proj[:, 1024:1536], reads=[('proj', 2)], writes=['vp'])
                        k.op('pool', lambda e, i=i: e.tensor_copy(out=vaug[:, i, :, 0:64], in_=proj[:, 1024:1536].rearrange("p (h d) -> p h d", h=8)), reads=[('proj', 2)], writes=['vaug'])
                    else:
                        k.dma('sp', ks[:, :], proj[0:NS, 512:1024], reads=[('proj', 1)], writes=['ks'])
                        k.dma('sp', vs[:, :], proj[0:NS, 1024:1536], reads=[('proj', 2)], writes=['vs'])
                        k.dma('sp', QKVs[:, :], proj[0:NS, 0:1536], reads=pr3, writes=['QKVs'])
            pr4 = [('proj', 0), ('proj', 1), ('proj', 2), ('proj', 3)]
            if not samp:
                k.dma('sp', P[1 + i * 128:1 + (i + 1) * 128, :], proj[:, 0:1792], reads=pr4, writes=['P'])
                if i == 31:
                    k.dma('sp', shp[0:1, :], proj[127:128, 0:1792], reads=pr4, writes=['shp'])
            else:
                k.dma('sp', P[1 + T:1 + T + NS, :], proj[0:NS, 0:1792], reads=pr4, writes=['P'])
                k.dma('sp', shs[:, :], proj[0:NS, 0:1792], reads=pr4, writes=['shs'])
                continue
            tcol = (i % 4) * 128
            nq = i // 2
            for h in range(8):
                bank = pb[2 + h // 4]; bn = 'pb%d' % (2 + h // 4); col = (h % 4) * 128
                for kc in range(8):
                    k.op('pe', lambda e, kc=kc, h=h, bank=bank, col=col: e.matmul(bank[0:64, col:col + 128], win[:, kc, h * 64:(h + 1) * 64], xnT[:, kc, :], start=(kc == 0), stop=False),
                         reads=['xnT1', 'win'], writes=[bn])
                for kc in range(8):
                    k.op('pe', lambda e, kc=kc, h=h, bank=bank, col=col: e.matmul(bank[0:64, col:col + 128], wlo[:, kc, h * 64:(h + 1) * 64], xnT[:, kc, :], start=False, stop=False),
                         reads=['xnT1', 'wlo'], writes=[bn])
                for kc in range(8):
                    k.op('pe', lambda e, kc=kc, h=h, bank=bank, col=col: e.matmul(bank[0:64, col:col + 128], win[:, kc, h * 64:(h + 1) * 64], xloT[:, kc, :], start=False, stop=(kc == 7)),
                         reads=['xloT', 'win'], writes=[bn])
            for h in range(8):
                bank = pb[2 + h // 4]; bn = 'pb%d' % (2 + h // 4); col = (h % 4) * 128
                k.op('act', lambda e, h=h, bank=bank, col=col: e.activation(out=qTa[0:64, h, tcol:tcol + 128], in_=bank[0:64, col:col + 128], func=AF.Copy, scale=0.125),
                     reads=[bn], writes=['qTa'])
                k.op('dve', lambda e, h=h, bank=bank, col=col: e.tensor_copy(out=qT32[:, h, :], in_=bank[0:64, col:col + 128]), reads=[bn], writes=['qT32'])
            for h in range(8):
                bank = pb[2 + h // 4]; bn = 'pb%d' % (2 + h // 4); col = (h % 4) * 128
                for kc in range(8):
                    k.op('pe', lambda e, kc=kc, h=h, bank=bank, col=col: e.matmul(bank[0:64, col:col + 128], win[:, kc, 512 + h * 64:512 + (h + 1) * 64], xnT[:, kc, :], start=(kc == 0), stop=(kc == 7)),
                         reads=['xnT1', 'win'], writes=[bn])
            for h in range(8):
                bank = pb[2 + h // 4]; bn = 'pb%d' % (2 + h // 4); col = (h % 4) * 128
                k.op('act', lambda e, h=h, bank=bank, col=col: e.activation(out=kTa[h][0:64, i * 128:(i + 1) * 128], in_=bank[0:64, col:col + 128], func=AF.Copy, accum_out=ksum2[:, h, i:i + 1]),
                     reads=[bn], writes=[('kTa', h), 'ksum2'])
            for h in range(8):
                k.op('pe', lambda e, h=h: e.matmul(pb[4][:, h * 16:(h + 1) * 16], qT32[:, h, :], kmT[:, h, :], start=True, stop=True), reads=['qT32', 'kmT'], writes=['pb4'])
            k.op('dve', lambda e: e.tensor_tensor(out=gm[:], in0=pb[4][:, 0:128].rearrange("p (h n) -> p h n", h=8), in1=gmaskc[:, nq, :].unsqueeze(1).broadcast_to([128, 8, 16]), op=ALU.add),
                 reads=['pb4', 'gmaskc'], writes=['gm'])
            for h in range(8):
                k.op('dve', lambda e, h=h: e.max(out=top8[:, h, :], in_=gm[:, h, :]), reads=['gm'], writes=['top8'])
            k.op('dve', lambda e: e.tensor_scalar(out=thr[:], in0=top8[:, :, 2], scalar1=-1e29, scalar2=None, op0=ALU.max), reads=['top8'], writes=['thr'])
            k.op('dve', lambda e: e.tensor_tensor(out=m01[:], in0=gm[:], in1=thr[:].unsqueeze(2).broadcast_to([128, 8, 16]), op=ALU.is_ge), reads=['gm', 'thr'], writes=['gm'])
            k.op('dve', lambda e: e.tensor_tensor(out=m01[:], in0=m01[:], in1=ownhot[:, nq, :].unsqueeze(1).broadcast_to([128, 8, 16]), op=ALU.add), reads=['gm', 'ownhot'], writes=['gm'])
            k.op('dve', lambda e: e.tensor_scalar(out=selpad[:, :, 64:80], in0=m01[:], scalar1=-1.0, scalar2=NBIG, op0=ALU.add, op1=ALU.mult), reads=['gm'], writes=['selpad'])
            for h in range(8):
                bank = pb[2 + h // 4]; bn = 'pb%d' % (2 + h // 4); col = (h % 4) * 128
                k.op('pe', lambda e, h=h, bank=bank, col=col: e.matmul(bank[0:80, col:col + 128], selpad[:, h, :], identf[:, :], start=True, stop=True), reads=['selpad', 'identf'], writes=[bn])
            for g in range(2):
                k.op('act', lambda e, g=g: e.activation(out=qTa[64:80, 4 * g:4 * g + 4, tcol:tcol + 128], in_=pb[2 + g][64:80, :].rearrange("p (h t) -> p h t", h=4), func=AF.Copy),
                     reads=['pb%d' % (2 + g)], writes=['qTa'])
            if i % 2 == 1:
                k.op('dve', lambda e: e.tensor_tensor(out=kmT[:, :, nq], in0=ksum2[:, :, i - 1], in1=ksum2[:, :, i], op=ALU.add), reads=['ksum2'], writes=['kmT'])
            if i % 4 != 3:
                continue
            TQ = i // 4
            nkt = 4 * TQ + 4
            for h in range(8):
                for kt in range(nkt):
                    sbk = pb[5 + kt % 2]; sbn = 'pb%d' % (5 + kt % 2); pt_ = pT[kt % 2]; ptn = 'pT%d' % (kt % 2)
                    k.op('pe', lambda e, h=h, kt=kt, sbk=sbk: e.matmul(sbk[:, :], kTa[h][0:82, kt * 128:(kt + 1) * 128], qTa[0:82, h, :], start=True, stop=True),
                         reads=[('kTa', h), 'qTa'], writes=[sbn])
                    rel = kt - 4 * TQ + 28
                    if kt >= 4 * TQ:
                        j = kt - 4 * TQ
                        k.op('dve', lambda e, h=h, sbk=sbk, rel=rel, j=j: e.scalar_tensor_tensor(out=stmp[:], in0=sbk[:, :], scalar=ab[:, h, rel:rel + 1], in1=cmask[:, j, :], op0=ALU.add, op1=ALU.add),
                             reads=[sbn, 'ab', 'cmask'], writes=['stmp', 'osb', 'lsb'])
                        k.op('act', lambda e, pt_=pt_: e.activation(out=pt_[:], in_=stmp[:], func=AF.Exp), reads=['stmp'], writes=[ptn])
                    else:
                        k.op('act', lambda e, h=h, sbk=sbk, pt_=pt_, rel=rel: e.activation(out=pt_[:], in_=sbk[:, :], func=AF.Exp, bias=ab[:, h, rel:rel + 1], scale=1.0),
                             reads=[sbn, 'ab'], writes=[ptn])
                    k.op('pe', lambda e, h=h, kt=kt, pt_=pt_: e.matmul(pb[2][0:65, :], vaug[:, kt, h, :], pt_[:], start=(kt == 0), stop=(kt == nkt - 1)),
                         reads=['vaug', ptn], writes=['pb2'])
                k.op('act', lambda e: e.activation(out=lsb[64:65, :], in_=pb[2][64:65, :], func=AF.Copy), reads=['pb2', 'stmp'], writes=['lsb'])
                k.op('dve', lambda e: e.reciprocal(out=lsb[64:65, :], in_=lsb[64:65, :]), reads=['lsb'], writes=['lsb'])
                k.op('pe', lambda e: e.matmul(pb[3][0:64, :], ones_sb[64:65, 0:64], lsb[64:65, :], start=True, stop=True), reads=['ones_sb', 'lsb'], writes=['pb3'])
                k.op('act', lambda e: e.activation(out=osb[0:64, :], in_=pb[2][0:64, :], func=AF.Copy), reads=['pb2', 'stmp'], writes=['osb'])
                at = atT[h % 2]; atn = 'atT%d' % (h % 2)
                k.op('dve', lambda e, at=at: e.tensor_tensor(out=at[:], in0=osb[0:64, :], in1=pb[3][0:64, :], op=ALU.mult), reads=['osb', 'pb3', 'stmp'], writes=[atn])
                k.dma('sp', mixT[h * 64:(h + 1) * 64, TQ * 512:(TQ + 1) * 512], at[:], reads=[atn], writes=['mixT'])

    k.barrier()
    with contextlib.ExitStack() as es:
        ptb = SB(es, "ptb", [128, NS * NPG], I32); ptf = SB(es, "ptf", [128, NS * NPG], F32)
        iotaf = SB(es, "iotaf", [128, 1], F32); idx = SB(es, "idx", [128, NS * NPG], I32)
        q4s = SB(es, "q4s", [4, 512], F32)
        qkv4 = SB(es, "qkv4", [4, 1536], F32)
        k.dma('sp', qkv4[:, :], QKVs[:, :], reads=['QKVs'], writes=['qkv4'])
        sel4 = SB(es, "sel4", [4, 4, 128], F32); oh4 = SB(es, "oh4", [128, 4, 4], F32); sel8 = SB(es, "sel8", [8, 4, 4], F32)
        alis = SB(es, "alis", [128, 65, 8], F32); diagm = SB(es, "diagm", [8, 512], F32)
        qrep = SB(es, "qrep", [128, 512], F32)
        kx = SB(es, "kx", [128, 512], F32); vx = SB(es, "vx", [128, 512], F32)
        kpg = [SB(es, "kpg%d" % i, [128, 512], F32) for i in range(3)]
        tmp = SB(es, "tmp", [128, 512], F32)
        sc = SB(es, "sc", [128, 65, 8], F32); pS = SB(es, "pS", [128, 65, 8], F32)
        t4 = SB(es, "t4", [4, 512], F32); gate4 = SB(es, "gate4", [4, 32, 8], F32)
        top84 = SB(es, "top84", [4, 8, 8], F32); m4 = SB(es, "m4", [4, 32, 8], F32)
        psp = SB(es, "psp", [128, 8], F32); rl = SB(es, "rl", [8, 1], F32); o8 = SB(es, "o8", [8, 512], F32)
        at4 = SB(es, "at4", [4, 512], BF16); at4T = SB(es, "at4T", [128, 4, 4], BF16)
        k.dma('sp', ptb[:], pt.rearrange("s p -> (s p)").partition_broadcast(128), writes=['ptb'])
        k.dma('sp', iotaf[:], cin['iotaf'][:, :], writes=['iotaf'])
        for (t_, n_) in ((sel4, 'sel4'), (oh4, 'oh4'), (sel8, 'sel8'), (alis, 'alis')):
            k.dma('sp', t_[:], cin[n_][:, :, :], writes=[n_])
        k.dma('sp', diagm[:], cin['diagm'][:, :], writes=['diagm'])
        k.op('dve', lambda e: e.tensor_copy(out=ptf[:], in_=ptb[:]), reads=['ptb'], writes=['ptf'])
        k.op('dve', lambda e: e.tensor_scalar(out=ptf[:], in0=ptf[:], scalar1=128.0, scalar2=iotaf[:, 0:1], op0=ALU.mult, op1=ALU.add), reads=['ptf', 'iotaf'], writes=['ptf'])
        k.op('dve', lambda e: e.tensor_copy(out=idx[:], in_=ptf[:]), reads=['ptf'], writes=['idx'])
        k.op('dve', lambda e: e.tensor_scalar(out=q4s[:], in0=qkv4[:, 0:512], scalar1=0.125, scalar2=None, op0=ALU.mult), reads=['qkv4'], writes=['q4s'])
        k.op('pool', lambda e: e.memset(kx[:], 0.0), writes=['kx'])
        k.op('pool', lambda e: e.memset(vx[:], 0.0), writes=['vx'])
        for s in range(NS):
            k.op('pe', lambda e, s=s: e.matmul(pb[0][:, :], sel4[0:4, s, :], q4s[0:4, :], start=True, stop=True), reads=['sel4', 'q4s'], writes=['pb0'])
            k.op('act', lambda e: e.activation(out=qrep[:], in_=pb[0][:, :], func=AF.Copy), reads=['pb0'], writes=['qrep'])
            k.dma('sp', kx[0:1, :], qkv4[s:s + 1, 512:1024], reads=['qkv4', 'kx'], writes=['kx'])
            k.dma('sp', vx[0:1, :], qkv4[s:s + 1, 1024:1536], reads=['qkv4', 'vx'], writes=['vx'])
            for pg in range(65):
                if pg < 64:
                    kb = kpg[pg % 3]; kbn = 'kpg%d' % (pg % 3); col = s * NPG + pg
                    k.dma('pool', reads=['idx'], writes=[kbn], fn=lambda e, kb=kb, col=col: e.indirect_dma_start(
                        out=kb[:, :], out_offset=None, in_=ck[:, :], in_offset=bass.IndirectOffsetOnAxis(ap=idx[:, col:col + 1], axis=0)))
                else:
                    kb = kx; kbn = 'kx'
                k.op('dve', lambda e, kb=kb: e.tensor_tensor(out=tmp[:], in0=kb[:], in1=qrep[:], op=ALU.mult), reads=[kbn, 'qrep'], writes=['tmp'])
                k.op('dve', lambda e, pg=pg: e.tensor_reduce(out=sc[:, pg, :], in_=tmp[:].rearrange("p (h d) -> p h d", h=8), axis=AX.X, op=ALU.add), reads=['tmp'], writes=['sc'])
                if pg < 64:
                    n = pg // 2; bank = pb[1 + n % 2]; bn = 'pb%d' % (1 + n % 2)
                    k.op('pe', lambda e, s=s, kb=kb, bank=bank, pg=pg: e.matmul(bank[0:4, :], oh4[:, s, :], kb[:], start=(pg % 2 == 0), stop=(pg % 2 == 1)), reads=[kbn, 'oh4'], writes=[bn])
                    if pg % 2 == 1:
                        k.op('dve', lambda e, bank=bank: e.tensor_tensor(out=t4[:], in0=bank[0:4, :], in1=q4s[:], op=ALU.mult), reads=[bn, 'q4s'], writes=['t4'])
                        k.op('dve', lambda e, n=n: e.tensor_reduce(out=gate4[:, n, :], in_=t4[:].rearrange("p (h d) -> p h d", h=8), axis=AX.X, op=ALU.add), reads=['t4'], writes=['gate4'])
            for h in range(8):
                k.op('dve', lambda e, h=h: e.max(out=top84[:, h, :], in_=gate4[:, :, h]), reads=['gate4'], writes=['top84'])
            for h in range(8):
                k.op('dve', lambda e, h=h: e.tensor_scalar(out=m4[:, :, h], in0=gate4[:, :, h], scalar1=top84[:, h, 2:3], scalar2=None, op0=ALU.is_ge), reads=['gate4', 'top84'], writes=['m4'])
            k.op('dve', lambda e: e.tensor_scalar(out=m4[:].rearrange("p a b -> p (a b)"), in0=m4[:].rearrange("p a b -> p (a b)"), scalar1=-1.0, scalar2=NBIG, op0=ALU.add, op1=ALU.mult), reads=['m4'], writes=['m4'])
            k.op('pe', lambda e, s=s: e.matmul(pb[0][:, 0:256], sel4[0:4, s, :], m4[:].rearrange("p a b -> p (a b)"), start=True, stop=True), reads=['sel4', 'm4'], writes=['pb0'])
            k.op('dve', lambda e: e.tensor_tensor(out=sc[:, 0:64, :].rearrange("p (n two) h -> p n two h", two=2), in0=sc[:, 0:64, :].rearrange("p (n two) h -> p n two h", two=2),
                                                  in1=pb[0][:, 0:256].rearrange("p (n h) -> p n h", h=8).unsqueeze(2).broadcast_to([128, 32, 2, 8]), op=ALU.add), reads=['sc', 'pb0'], writes=['sc'])
            k.op('dve', lambda e: e.tensor_tensor(out=sc[:].rearrange("p a b -> p (a b)"), in0=sc[:].rearrange("p a b -> p (a b)"), in1=alis[:].rearrange("p a b -> p (a b)"), op=ALU.add), reads=['sc', 'alis'], writes=['sc'])
            k.op('act', lambda e: e.activation(out=pS[:].rearrange("p a b -> p (a b)"), in_=sc[:].rearrange("p a b -> p (a b)"), func=AF.Exp), reads=['sc'], writes=['pS'])
            k.op('dve', lambda e: e.tensor_reduce(out=psp[:], in_=pS[:].rearrange("p g h -> p h g"), axis=AX.X, op=ALU.add), reads=['pS'], writes=['psp'])
            k.op('pe', lambda e: e.matmul(pb[3][0:8, 0:1], psp[:], ones_sb[:, 0:1], start=True, stop=True), reads=['psp', 'ones_sb'], writes=['pb3'])
            for pg in range(65):
                if pg < 64:
                    vb = kpg[pg % 3]; vbn = 'kpg%d' % (pg % 3); col = s * NPG + pg
                    k.dma('pool', reads=['idx'], writes=[vbn], fn=lambda e, vb=vb, col=col: e.indirect_dma_start(
                        out=vb[:, :], out_offset=None, in_=cv[:, :], in_offset=bass.IndirectOffsetOnAxis(ap=idx[:, col:col + 1], axis=0)))
                else:
                    vb = vx; vbn = 'vx'
                k.op('pe', lambda e, vb=vb, pg=pg: e.matmul(pb[4][0:8, :], pS[:, pg, :], vb[:], start=(pg == 0), stop=(pg == 64)), reads=[vbn, 'pS'], writes=['pb4'])
            k.op('dve', lambda e: e.reciprocal(out=rl[:], in_=pb[3][0:8, 0:1]), reads=['pb3'], writes=['rl'])
            k.op('dve', lambda e: e.scalar_tensor_tensor(out=o8[:], in0=pb[4][0:8, :], scalar=rl[:, 0:1], in1=diagm[:], op0=ALU.mult, op1=ALU.mult), reads=['pb4', 'rl', 'diagm'], writes=['o8'])
            k.op('pe', lambda e, s=s: e.matmul(pb[5][0:4, :], sel8[0:8, s, :], o8[:], start=(s == 0), stop=(s == NS - 1)), reads=['sel8', 'o8'], writes=['pb5'])
        k.op('act', lambda e: e.activation(out=at4[:], in_=pb[5][0:4, :], func=AF.Copy), reads=['pb5'], writes=['at4'])
        for c in range(4):
            k.op('pe', lambda e, c=c: e.transpose(out=pbT[:, c * 4:c * 4 + 4], in_=at4[0:4, c * 128:(c + 1) * 128], identity=identb[0:4, 0:4]), reads=['at4', 'identb'], writes=['pbT'])
        k.op('act', lambda e: e.activation(out=at4T[:].rearrange("p a b -> p (a b)"), in_=pbT[:, 0:16], func=AF.Copy), reads=['pbT'], writes=['at4T'])
        for c in range(4):
            k.dma('sp', mixT[c * 128:(c + 1) * 128, T:T + NS], at4T[:, c, :], reads=['at4T'], writes=['mixT'])

    k.barrier()
    EM05 = float(np.exp(-0.5))
    with contextlib.ExitStack() as es:
      if KSTOP >= 3:
        def rep(name, src, n):
            t_ = SB(es, name, [128, n], F32)
            k.dma('sp', t_[:], src.partition_broadcast(128), writes=[name])
            return t_
        mu_r = rep("mu_r", mu, 1792); w0_r = rep("w0_r", w0, 512); a0_r = rep("a0_r", a0, 512)
        kk_r = rep("kk_r", k_k, 512); ka_r = rep("ka_r", k_a, 512); rk_r = rep("rk_r", r_k, 512)
        lnw_r = rep("lnw_r", lnw, 512); lnb_r = rep("lnb_r", lnb, 512)
        dup = SB(es, "dup", [64, 512], BF16); iup = SB(es, "iup", [64, 512], BF16); gup = SB(es, "gup", [128, 512], BF16)
        stg2 = SB(es, "stg2", [128, 512], F32)
        for (wsb, wd, nr, nm) in ((dup, decay_up, 64, 'dup'), (iup, iclr_up, 64, 'iup'), (gup, gate_up, 128, 'gup')):
            k.dma('sp', stg2[0:nr, :], wd[:, :], writes=['stg2'])
            k.op('dve', lambda e, wsb=wsb, nr=nr: e.tensor_copy(out=wsb[0:nr, :], in_=stg2[0:nr, :]), reads=['stg2'], writes=[nm])
        pc = SB(es, "pc", [128, 1792], F32); pp = SB(es, "pp", [128, 1792], F32)
        lor = SB(es, "lor", [128, 256], BF16); lorT = SB(es, "lorT", [128, 3, 128], BF16)
        dec = SB(es, "dec", [128, 512], F32); aa = SB(es, "aa", [128, 512], F32); gg = SB(es, "gg", [128, 512], F32)
        kkn = SB(es, "kkn", [128, 512], F32); bb = SB(es, "bb", [128, 512], F32); km = SB(es, "km", [128, 512], F32)
        t5 = SB(es, "t5", [128, 512], F32); s8 = SB(es, "s8", [128, 8], F32); bon = SB(es, "bon", [128, 8], F32)
        WT = SB(es, "WT", [128, 4, 128], F32); V1 = SB(es, "V1", [128, 4, 129, 4], F32)
        rlast = SB(es, "rlast", [128, 4], F32)
        ST = SB(es, "ST", [128, 4, 64], F32)
        LLa = SB(es, "LLa", [128, 64, 128], F32); RRa = SB(es, "RRa", [128, 64, 64], F32)
        LLb = SB(es, "LLb", [128, 64, 128], F32); RRb = SB(es, "RRb", [128, 64, 64], F32)
        def LLp(p): return (LLa, 32 * p) if p < 3 else (LLb, 0)
        def RRp(p): return (RRa, 32 * p) if p < 3 else (RRb, 0)
        sin = SB(es, "sin", [64, 128], F32); sout = SB(es, "sout", [64, 128], F32)
        for t_ in (LLa, LLb):
            k.op('pool', lambda e, t_=t_: e.memset(t_[:].rearrange("p a b -> p (a b)"), 0.0), writes=['LL'])
        for t_ in (RRa, RRb):
            k.op('pool', lambda e, t_=t_: e.memset(t_[:].rearrange("p a b -> p (a b)"), 0.0), writes=['RR'])
        k.op('pool', lambda e: e.memset(V1[:].rearrange("p a b c -> p (a b c)"), 0.0), writes=['V1'])
        k.op('pool', lambda e: e.memset(rlast[:], 0.0), writes=['rlast'])

        def prep(row0, nrows, prev_ap, nm):
            if nrows < 128:
                k.op('pool', lambda e: e.memset(pc[:], 0.0), writes=['pc'])
                k.op('pool', lambda e: e.memset(pp[:], 0.0), writes=['pp'])
            k.dma('sp', pc[0:nrows, :], P[1 + row0:1 + row0 + nrows, :], reads=['P', 'pc'], writes=['pc'])
            k.dma('sp', pp[0:nrows, :], prev_ap, reads=['P', 'pp'], writes=['pp'])
            k.op('dve', lambda e: e.tensor_tensor(out=pp[:], in0=pp[:], in1=pc[:], op=ALU.subtract), reads=['pp', 'pc'], writes=['pp'])
            k.op('pool', lambda e: e.tensor_tensor(out=pp[:], in0=pp[:], in1=mu_r[:], op=ALU.mult), reads=['pp', 'mu_r'], writes=['pp'])
            k.op('dve', lambda e: e.tensor_tensor(out=pc[:], in0=pc[:], in1=pp[:], op=ALU.add), reads=['pp', 'pc'], writes=['pc'])
            r_ = pc[:, 0:512]; xw = pc[:, 512:576]; k_ = pc[:, 576:1088]; v_ = pc[:, 1088:1600]; xa = pc[:, 1600:1664]; xg = pc[:, 1664:1792]
            k.op('act', lambda e: e.activation(out=lor[:, 0:64], in_=xw, func=AF.Tanh), reads=['pc'], writes=['lor'])
            k.op('dve', lambda e: e.tensor_copy(out=lor[:, 64:128], in_=xa), reads=['pc'], writes=['lor'])
            k.op('act', lambda e: e.activation(out=lor[:, 128:256], in_=xg, func=AF.Sigmoid), reads=['pc'], writes=['lor'])
            k.op('pe', lambda e: e.transpose(out=pbT[0:64, 0:128], in_=lor[:, 0:64], identity=identb[:]), reads=['lor', 'identb'], writes=['pbT'])
            k.op('pe', lambda e: e.transpose(out=pbT[0:64, 128:256], in_=lor[:, 64:128], identity=identb[:]), reads=['lor', 'identb'], writes=['pbT'])
            k.op('pe', lambda e: e.transpose(out=pbT[:, 256:384], in_=lor[:, 128:256], identity=identb[:]), reads=['lor', 'identb'], writes=['pbT'])
            k.op('act', lambda e: e.activation(out=lorT[0:64, 0:2, :], in_=pbT[0:64, 0:256].rearrange("p (a b) -> p a b", a=2), func=AF.Copy), reads=['pbT'], writes=['lorT'])
            k.op('act', lambda e: e.activation(out=lorT[:, 2, :], in_=pbT[:, 256:384], func=AF.Copy), reads=['pbT'], writes=['lorT'])
            k.op('pe', lambda e: e.matmul(pb[0][:, :], lorT[0:64, 0, :], dup[:, :], start=True, stop=True), reads=['lorT', 'dup'], writes=['pb0'])
            k.op('pe', lambda e: e.matmul(pb[1][:, :], lorT[0:64, 1, :], iup[:, :], start=True, stop=True), reads=['lorT', 'iup'], writes=['pb1'])
            k.op('pe', lambda e: e.matmul(pb[2][:, :], lorT[:, 2, :], gup[:, :], start=True, stop=True), reads=['lorT', 'gup'], writes=['pb2'])
            k.op('dve', lambda e: e.tensor_tensor(out=dec[:], in0=pb[0][:, :], in1=w0_r[:], op=ALU.add), reads=['pb0', 'w0_r'], writes=['dec'])
            k.op('act', lambda e: e.activation(out=dec[:], in_=dec[:], func=AF.Sigmoid), reads=['dec'], writes=['dec'])
            k.op('act', lambda e: e.activation(out=dec[:], in_=dec[:], func=AF.Exp, scale=-EM05), reads=['dec'], writes=['dec'])
            k.op('dve', lambda e: e.tensor_tensor(out=aa[:], in0=pb[1][:, :], in1=a0_r[:], op=ALU.add), reads=['pb1', 'a0_r'], writes=['aa'])
            k.op('act', lambda e: e.activation(out=aa[:], in_=aa[:], func=AF.Sigmoid), reads=['aa'], writes=['aa'])
            k.op('act', lambda e: e.activation(out=gg[:], in_=pb[2][:, :], func=AF.Copy), reads=['pb2'], writes=['gg'])
            k.op('dve', lambda e: e.tensor_tensor(out=kkn[:], in0=k_, in1=kk_r[:], op=ALU.mult), reads=['pc', 'kk_r'], writes=['kkn'])
            k.op('pool', lambda e: e.tensor_tensor(out=t5[:], in0=kkn[:], in1=kkn[:], op=ALU.mult), reads=['kkn'], writes=['t5'])
            k.op('dve', lambda e: e.tensor_reduce(out=s8[:], in_=t5[:].rearrange("p (h d) -> p h d", h=8), axis=AX.X, op=ALU.add), reads=['t5'], writes=['s8'])
            k.op('dve', lambda e: e.tensor_scalar(out=s8[:], in0=s8[:], scalar1=1e-24, scalar2=None, op0=ALU.max), reads=['s8'], writes=['s8'])
            k.op('act', lambda e: e.activation(out=s8[:], in_=s8[:], func=AF.Sqrt), reads=['s8'], writes=['s8'])
            k.op('dve', lambda e: e.reciprocal(out=s8[:], in_=s8[:]), reads=['s8'], writes=['s8'])
            k.op('dve', lambda e: e.tensor_tensor(out=kkn[:].rearrange("p (h d) -> p h d", h=8), in0=kkn[:].rearrange("p (h d) -> p h d", h=8), in1=s8[:].unsqueeze(2).broadcast_to([128, 8, 64]), op=ALU.mult),
                 reads=['kkn', 's8'], writes=['kkn'])
            k.op('pool', lambda e: e.tensor_tensor(out=bb[:], in0=kkn[:], in1=aa[:], op=ALU.mult), reads=['kkn', 'aa'], writes=['bb'])
            k.op('dve', lambda e: e.tensor_scalar(out=kkn[:], in0=kkn[:], scalar1=-1.0, scalar2=None, op0=ALU.mult), reads=['kkn', 'bb'], writes=['kkn'])
            k.op('dve', lambda e: e.scalar_tensor_tensor(out=km[:], in0=aa[:], scalar=-1.0, in1=ka_r[:], op0=ALU.add, op1=ALU.mult), reads=['aa', 'ka_r'], writes=['km'])
            k.op('dve', lambda e: e.scalar_tensor_tensor(out=km[:], in0=km[:], scalar=1.0, in1=k_, op0=ALU.add, op1=ALU.mult), reads=['km', 'pc'], writes=['km'])
            k.op('pool', lambda e: e.tensor_tensor(out=t5[:], in0=r_, in1=km[:], op=ALU.mult), reads=['pc', 'km', 's8'], writes=['t5'])
            k.op('pool', lambda e: e.tensor_tensor(out=t5[:], in0=t5[:], in1=rk_r[:], op=ALU.mult), reads=['t5', 'rk_r'], writes=['t5'])
            k.op('dve', lambda e: e.tensor_reduce(out=bon[:], in_=t5[:].rearrange("p (h d) -> p h d", h=8), axis=AX.X, op=ALU.add), reads=['t5'], writes=['bon'])
            k.dma('sp', Bs[row0:row0 + nrows, :], bb[0:nrows, :], reads=['bb'], writes=['Bs'])
            k.dma('sp', KMs[row0:row0 + nrows, :], km[0:nrows, :], reads=['km'], writes=['KMs'])
            k.dma('sp', Vs[row0:row0 + nrows, :], pc[0:nrows, 1088:1600], reads=['pc'], writes=['Vs'])
            k.dma('sp', Gs[row0:row0 + nrows, :], gg[0:nrows, :], reads=['gg'], writes=['Gs'])
            k.dma('sp', BON[row0:row0 + nrows, :], bon[0:nrows, :], reads=['bon'], writes=['BON'])
            for p in range(4):
                for (src, sname, bank) in ((dec, 'dec', 3), (kkn, 'kkn', 4), (pc, 'pc', 5)):
                    k.op('pe', lambda e, src=src, p=p, bank=bank: e.matmul(pb[bank][:, p * 128:(p + 1) * 128], src[:, p * 128:(p + 1) * 128], identf[:, :], start=True, stop=True),
                         reads=[sname, 'identf'], writes=['pb%d' % bank])
            k.op('act', lambda e: e.activation(out=WT[:].rearrange("p a b -> p (a b)"), in_=pb[3][:, :], func=AF.Copy), reads=['pb3'], writes=['WT'])
            k.op('dve', lambda e: e.tensor_copy(out=V1[0:64, :, 0, 2], in_=rlast[0:64, :]), reads=['rlast', 'V1'], writes=['V1'])
            k.op('dve', lambda e: e.tensor_copy(out=V1[64:128, :, 0, 3], in_=rlast[64:128, :]), reads=['rlast', 'V1'], writes=['V1'])
            k.op('dve', lambda e: e.tensor_copy(out=V1[0:64, :, 0:128, 0], in_=pb[4][0:64, :].rearrange("p (a b) -> p a b", a=4)), reads=['pb4', 'V1'], writes=['V1'])
            k.op('dve', lambda e: e.tensor_copy(out=V1[64:128, :, 0:128, 1], in_=pb[4][64:128, :].rearrange("p (a b) -> p a b", a=4)), reads=['pb4', 'V1'], writes=['V1'])
            k.op('act', lambda e: e.activation(out=V1[0:64, :, 1:129, 2], in_=pb[5][0:64, :].rearrange("p (a b) -> p a b", a=4), func=AF.Copy), reads=['pb5', 'V1'], writes=['V1'])
            k.op('act', lambda e: e.activation(out=V1[64:128, :, 1:129, 3], in_=pb[5][64:128, :].rearrange("p (a b) -> p a b", a=4), func=AF.Copy), reads=['pb5', 'V1'], writes=['V1'])

        def load_rows(row0, nst):
            for p in range(4):
                LL, lb = LLp(p); RR, rb = RRp(p)
                for hh in range(2):
                    c0 = (2 * p + hh) * 64
                    k.dma('sp', LL[lb + hh:lb + hh + 1, 0:nst, hh * 64:(hh + 1) * 64], Bs[row0:row0 + nst, c0:c0 + 64].unsqueeze(0), reads=['Bs', 'LL'], writes=['LL'])
                    k.dma('sp', LL[lb + 4 + hh:lb + 5 + hh, 0:nst, hh * 64:(hh + 1) * 64], KMs[row0:row0 + nst, c0:c0 + 64].unsqueeze(0), reads=['KMs', 'LL'], writes=['LL'])
                    k.dma('sp', RR[rb + 4 + hh:rb + 5 + hh, 0:nst, :], Vs[row0:row0 + nst, c0:c0 + 64].unsqueeze(0), reads=['Vs', 'RR'], writes=['RR'])

        def steps(nst, slot0=0, dummy_last=False):
            for t in range(nst):
                for p in range(4):
                    LL, lb = LLp(p); RR, rb = RRp(p)
                    k.op('pe', lambda e, p=p, t=t: e.matmul(pb[3 + p][0:4, 0:64], V1[:, p, slot0 + t, :], ST[:, p, :], start=True, stop=True),
                         reads=['V1', ('ST', p)], writes=['pb%d' % (3 + p)])
                    k.op('act', lambda e, p=p, t=t, RR=RR, rb=rb: e.activation(out=RR[rb:rb + 4, t, :], in_=pb[3 + p][0:4, 0:64], func=AF.Copy),
                         reads=['pb%d' % (3 + p), 'RR'], writes=[('RRs', p)])
                    if dummy_last and t == nst - 1:
                        continue
                    k.op('pe', lambda e, p=p, t=t, LL=LL, lb=lb, RR=RR, rb=rb: e.matmul(pb[3 + p][:, 64:128], LL[lb:lb + 6, t, :], RR[rb:rb + 6, t, :], start=True, stop=True),
                         reads=['LL', 'RR', ('RRs', p)], writes=['pb%d' % (3 + p)])
                    k.op('dve', lambda e, p=p, t=t: e.scalar_tensor_tensor(out=ST[:, p, :], in0=ST[:, p, :], scalar=WT[:, p, slot0 + t:slot0 + t + 1], in1=pb[3 + p][:, 64:128], op0=ALU.mult, op1=ALU.add),
                         reads=['pb%d' % (3 + p), 'WT', ('ST', p)], writes=[('ST', p)])

        def store_y(dst_rows, nst):
            for p in range(4):
                RR, rb = RRp(p)
                for hh in range(2):
                    c0 = (2 * p + hh) * 64
                    k.dma('sp', dst_rows[:, c0:c0 + 64].unsqueeze(0), RR[rb + 2 + hh:rb + 3 + hh, 0:nst, :], reads=[('RRs', q) for q in range(4)] + ['RR'], writes=['Ysc'])

        def sync_rr():
            pass

        def store_state(dst):
            for p in range(4):
                k.op('pe', lambda e, p=p: e.matmul(pb[2][0:64, 0:128], ST[:, p, :], identf[:, :], start=True, stop=True), reads=[('ST', p), 'identf'], writes=['pb2'])
                k.op('act', lambda e: e.activation(out=sout[:], in_=pb[2][0:64, 0:128], func=AF.Copy), reads=['pb2'], writes=['sout'])
                for hh in range(2):
                    k.dma('sp', dst[2 * p + hh, :, :], sout[:, hh * 64:(hh + 1) * 64], reads=['sout'], writes=['wkvout'])

        for p in range(4):
            k.op('pool', lambda e, p=p: e.memset(ST[:, p, :], 0.0), writes=[('ST', p)])
        for i in PT:
            prep(i * 128, 128, P[i * 128:(i + 1) * 128, :], 'p')
            k.op('dve', lambda e: e.tensor_copy(out=rlast[0:64, :], in_=V1[0:64, :, 128, 2]), reads=['V1'], writes=['rlast'])
            k.op('dve', lambda e: e.tensor_copy(out=rlast[64:128, :], in_=V1[64:128, :, 128, 3]), reads=['V1'], writes=['rlast'])
            for half in range(2):
                load_rows(i * 128 + half * 64, 64)
                steps(64, slot0=half * 64)
                store_y(Ysc[i * 128 + half * 64:i * 128 + (half + 1) * 64, :], 64)
        k.op('dve', lambda e: e.tensor_copy(out=V1[0:64, :, 0, 2], in_=rlast[0:64, :]), reads=['rlast', 'V1'], writes=['V1'])
        k.op('dve', lambda e: e.tensor_copy(out=V1[64:128, :, 0, 3], in_=rlast[64:128, :]), reads=['rlast', 'V1'], writes=['V1'])
        k.op('pool', lambda e: e.memset(V1[:, :, 0, 0:2], 0.0), reads=['V1'], writes=['V1'])
        steps(1, slot0=0, dummy_last=True)
        store_y(Ysc[T:T + 1, :], 1)
        store_state(wkvp)

        prep(T, NS, sshift[:, :], 's')
        for s in range(NS):
            for p in range(4):
                for hh in range(2):
                    k.dma('sp', sin[:, hh * 64:(hh + 1) * 64], swkv[s, 2 * p + hh, :, :], reads=['sin'], writes=['sin'])
                k.op('pe', lambda e: e.matmul(pb[2][:, 0:64], sin[:, :], identf[0:64, 0:64], start=True, stop=True), reads=['sin', 'identf'], writes=['pb2'])
                k.op('act', lambda e, p=p: e.activation(out=ST[:, p, :], in_=pb[2][:, 0:64], func=AF.Copy), reads=['pb2'], writes=[('ST', p)])
            load_rows(T + s, 1)
            for p in range(4):
                LL, lb = LLp(p); RR, rb = RRp(p)
                k.op('pe', lambda e, p=p, s=s: e.matmul(pb[3 + p][0:4, 0:64], V1[:, p, s, :], ST[:, p, :], start=True, stop=True), reads=['V1', ('ST', p)], writes=['pb%d' % (3 + p)])
                k.op('act', lambda e, p=p, RR=RR, rb=rb: e.activation(out=RR[rb:rb + 4, 0, :], in_=pb[3 + p][0:4, 0:64], func=AF.Copy), reads=['pb%d' % (3 + p), 'RR'], writes=[('RRs', p)])
                k.op('pe', lambda e, p=p, LL=LL, lb=lb, RR=RR, rb=rb: e.matmul(pb[3 + p][:, 64:128], LL[lb:lb + 6, 0, :], RR[rb:rb + 6, 0, :], start=True, stop=True), reads=['LL', 'RR', ('RRs', p)], writes=['pb%d' % (3 + p)])
                k.op('dve', lambda e, p=p, s=s: e.scalar_tensor_tensor(out=ST[:, p, :], in0=ST[:, p, :], scalar=WT[:, p, s:s + 1], in1=pb[3 + p][:, 64:128], op0=ALU.mult, op1=ALU.add),
                     reads=['pb%d' % (3 + p), 'WT', ('ST', p)], writes=[('ST', p)])
                k.op('pe', lambda e, p=p, s=s: e.matmul(pb[3 + p][0:4, 0:64], V1[:, p, s + 1, :], ST[:, p, :], start=True, stop=True), reads=['V1', ('ST', p)], writes=['pb%d' % (3 + p)])
                k.op('act', lambda e, p=p, RR=RR, rb=rb: e.activation(out=RR[rb:rb + 4, 1, :], in_=pb[3 + p][0:4, 0:64], func=AF.Copy), reads=['pb%d' % (3 + p), 'RR'], writes=[('RRs', p)])
            for p in range(4):
                RR, rb = RRp(p)
                for hh in range(2):
                    c0 = (2 * p + hh) * 64
                    k.dma('sp', Yss[s:s + 1, 1, c0:c0 + 64], RR[rb + 2 + hh:rb + 3 + hh, 1, :], reads=[('RRs', q) for q in range(4)] + ['RR'], writes=['Yss'])
            store_state(wkvs[s])

        yv = SB(es, "yv", [128, 512], F32); vv = SB(es, "vv", [128, 512], F32); gv = SB(es, "gv", [128, 512], F32)
        bv = SB(es, "bv", [128, 8], F32); m8 = SB(es, "m8", [128, 8], F32); rwb = SB(es, "rwb", [128, 512], BF16)
        rwT = SB(es, "rwT", [128, 4, 128], BF16)
        for i in PT + [32]:
            samp = (i == 32)
            nr = NS if samp else 128
            if samp:
                for (t_, n_) in ((yv, 'yv'), (vv, 'vv'), (gv, 'gv')):
                    k.op('pool', lambda e, t_=t_: e.memset(t_[:], 0.0), writes=[n_])
                k.op('pool', lambda e: e.memset(bv[:], 0.0), writes=['bv'])
                k.dma('sp', yv[0:NS, :], Yss[:, 1, :], reads=['Yss', 'yv'], writes=['yv'])
            else:
                k.dma('sp', yv[:, :], Ysc[i * 128 + 1:(i + 1) * 128 + 1, :], reads=['Ysc'], writes=['yv'])
            r0 = T if samp else i * 128
            k.dma('sp', vv[0:nr, :], Vs[r0:r0 + nr, :], reads=['Vs', 'vv'], writes=['vv'])
            k.dma('sp', gv[0:nr, :], Gs[r0:r0 + nr, :], reads=['Gs', 'gv'], writes=['gv'])
            k.dma('sp', bv[0:nr, :], BON[r0:r0 + nr, :], reads=['BON', 'bv'], writes=['bv'])
            y3 = yv[:].rearrange("p (h d) -> p h d", h=8)
            k.op('dve', lambda e: e.tensor_reduce(out=m8[:], in_=y3, axis=AX.X, op=ALU.add), reads=['yv'], writes=['m8'])
            k.op('dve', lambda e: e.tensor_scalar(out=m8[:], in0=m8[:], scalar1=1.0 / 64, scalar2=None, op0=ALU.mult), reads=['m8'], writes=['m8'])
            k.op('dve', lambda e: e.tensor_tensor(out=y3, in0=y3, in1=m8[:].unsqueeze(2).broadcast_to([128, 8, 64]), op=ALU.subtract), reads=['yv', 'm8'], writes=['yv'])
            k.op('pool', lambda e: e.tensor_tensor(out=t5[:], in0=yv[:], in1=yv[:], op=ALU.mult), reads=['yv'], writes=['t5'])
            k.op('dve', lambda e: e.tensor_reduce(out=m8[:], in_=t5[:].rearrange("p (h d) -> p h d", h=8), axis=AX.X, op=ALU.add), reads=['t5', 'yv'], writes=['m8'])
            k.op('act', lambda e: e.activation(out=m8[:], in_=m8[:], func=AF.Sqrt, scale=1.0 / 64, bias=GN_EPS), reads=['m8'], writes=['m8'])
            k.op('dve', lambda e: e.reciprocal(out=m8[:], in_=m8[:]), reads=['m8'], writes=['m8'])
            k.op('dve', lambda e: e.tensor_tensor(out=y3, in0=y3, in1=m8[:].unsqueeze(2).broadcast_to([128, 8, 64]), op=ALU.mult), reads=['yv', 'm8'], writes=['yv'])
            k.op('pool', lambda e: e.tensor_tensor(out=yv[:], in0=yv[:], in1=lnw_r[:], op=ALU.mult), reads=['yv', 'lnw_r'], writes=['yv'])
            k.op('pool', lambda e: e.tensor_tensor(out=yv[:], in0=yv[:], in1=lnb_r[:], op=ALU.add), reads=['yv', 'lnb_r'], writes=['yv'])
            v3 = vv[:].rearrange("p (h d) -> p h d", h=8)
            k.op('dve', lambda e: e.tensor_tensor(out=v3, in0=v3, in1=bv[:].unsqueeze(2).broadcast_to([128, 8, 64]), op=ALU.mult), reads=['vv', 'bv'], writes=['vv'])
            k.op('dve', lambda e: e.tensor_tensor(out=yv[:], in0=yv[:], in1=vv[:], op=ALU.add), reads=['yv', 'vv'], writes=['yv'])
            k.op('dve', lambda e: e.tensor_tensor(out=rwb[:], in0=yv[:], in1=gv[:], op=ALU.mult), reads=['yv', 'gv'], writes=['rwb'])
            for c in range(4):
                k.op('pe', lambda e, c=c: e.transpose(out=pbT[:, c * 128:(c + 1) * 128], in_=rwb[:, c * 128:(c + 1) * 128], identity=identb[:]), reads=['rwb', 'identb'], writes=['pbT'])
            k.op('act', lambda e: e.activation(out=rwT[:].rearrange("p a b -> p (a b)"), in_=pbT[:, 0:512], func=AF.Copy), reads=['pbT'], writes=['rwT'])
            for c in range(4):
                k.dma('sp', mixT[512 + c * 128:512 + (c + 1) * 128, r0:r0 + 128], rwT[:, c, :], reads=['rwT'], writes=['mixT'])

    k.barrier()
    with contextlib.ExitStack() as es:
      if KSTOP >= 4:
        wo = SB(es, "wo", [128, 8, D], BF16); wu = SB(es, "wu", [128, 8, 4096], BF16); wd = SB(es, "wd", [128, 32, D], BF16)
        stg3 = SB(es, "stg3", [128, 3328], F32)
        gf_r = SB(es, "gf_r", [128, D], F32)
        k.dma('sp', gf_r[:], gf.partition_broadcast(128), writes=['gf_r'])
        load_weight(wo, w_out, 8, D, None, stg3, 'wo')
        load_weight(wu, w_up, 8, 4096, g2, stg3, 'wu')
        load_weight(wd, w_dn, 32, D, None, stg3, 'wd')
        mxT = SB(es, "mxT", [128, 8, 128], BF16)
        xt3 = SB(es, "xt3", [128, D], F32); hh_ = SB(es, "hh_", [128, D], F32)
        ss3 = SB(es, "ss3", [128, 1], F32); rs3 = SB(es, "rs3", [128, 1], F32)
        hnb = SB(es, "hnb", [128, D], BF16); hnT = SB(es, "hnT", [128, 8, 128], BF16)
        ur = SB(es, "ur", [128, 512], F32); uT = SB(es, "uT", [128, 32, 128], BF16)
        yo = SB(es, "yo", [128, D], F32)
        for i in PT + [32]:
            samp = (i == 32)
            r0 = T if samp else i * 128
            k.dma('sp', mxT[:, :, :], mixT[:, r0:r0 + 128].rearrange("(a p) t -> p a t", p=128), reads=['mixT'], writes=['mxT'])
            if samp:
                k.op('pool', lambda e: e.memset(xt3[:], 0.0), writes=['xt3'])
                k.dma('sp', xt3[0:NS, :], xsm[:, :], reads=['xt3'], writes=['xt3'])
            else:
                k.dma('sp', xt3[:], xp[i * 128:(i + 1) * 128, :], writes=['xt3'])
            for c in range(2):
                for kc in range(8):
                    k.op('pe', lambda e, c=c, kc=kc: e.matmul(pb[c][:, :], mxT[:, kc, :], wo[:, kc, c * 512:(c + 1) * 512], start=(kc == 0), stop=(kc == 7)), reads=['mxT', 'wo'], writes=['pb%d' % c])
                k.op('dve', lambda e, c=c: e.tensor_tensor(out=hh_[:, c * 512:(c + 1) * 512], in0=pb[c][:, :], in1=xt3[:, c * 512:(c + 1) * 512], op=ALU.add), reads=['pb%d' % c, 'xt3'], writes=['hh_'])
            rmsnorm_T((ss3, rs3, hnb, hnT), hh_, 'hh_', '3')
            for fg in range(8):
                bank = pb[2 + fg % 2]; bn = 'pb%d' % (2 + fg % 2)
                for f4 in range(4):
                    fc = fg * 4 + f4
                    for kc in range(8):
                        k.op('pe', lambda e, fc=fc, f4=f4, kc=kc, bank=bank: e.matmul(bank[:, f4 * 128:(f4 + 1) * 128], wu[:, kc, fc * 128:(fc + 1) * 128], hnT[:, kc, :], start=(kc == 0), stop=(kc == 7)),
                             reads=['xnT3', 'wu'], writes=[bn])
                k.op('act', lambda e, bank=bank: e.activation(out=ur[:], in_=bank[:, :], func=AF.Relu), reads=[bn], writes=['ur'])
                k.op('dve', lambda e, fg=fg: e.tensor_tensor(out=uT[:, fg * 4:(fg + 1) * 4, :].rearrange("p a b -> p (a b)"), in0=ur[:], in1=ur[:], op=ALU.mult), reads=['ur'], writes=['uT'])
            for c in range(2):
                bank = pb[4 + c]; bn = 'pb%d' % (4 + c)
                for fc in range(32):
                    k.op('pe', lambda e, c=c, fc=fc, bank=bank: e.matmul(bank[:, :], uT[:, fc, :], wd[:, fc, c * 512:(c + 1) * 512], start=(fc == 0), stop=(fc == 31)), reads=['uT', 'wd'], writes=[bn])
                k.op('dve', lambda e, c=c, bank=bank: e.tensor_tensor(out=hh_[:, c * 512:(c + 1) * 512], in0=bank[:, :], in1=hh_[:, c * 512:(c + 1) * 512], op=ALU.add), reads=[bn, 'hh_'], writes=['hh_'])
            k.op('act', lambda e: e.activation(out=hnb[:], in_=hh_[:], func=AF.Square, accum_out=ss3[:, 0:1]), reads=['hh_'], writes=['xnb3', 'ss3'])
            k.op('act', lambda e: e.activation(out=rs3[:], in_=ss3[:], func=AF.Sqrt, scale=1.0 / D, bias=RMS_EPS), reads=['ss3'], writes=['rs3'])
            k.op('dve', lambda e: e.reciprocal(out=rs3[:], in_=rs3[:]), reads=['rs3'], writes=['rs3'])
            k.op('dve', lambda e: e.scalar_tensor_tensor(out=yo[:], in0=hh_[:], scalar=rs3[:, 0:1], in1=gf_r[:], op0=ALU.mult, op1=ALU.mult), reads=['hh_', 'rs3', 'gf_r'], writes=['yo'])
            if samp:
                k.dma('sp', ysm[:, :], yo[0:NS, :], reads=['yo'], writes=['ysm'])
            else:
                k.dma('sp', yp[i * 128:(i + 1) * 128, :], yo[:], reads=['yo'], writes=['yp'])

    k.wait_all('sp')
    top.close()
    return nc, consts, k


_CACHE = {}


def kernel(**inp):
    f = lambda a: np.ascontiguousarray(np.asarray(a))
    n_pool = inp['cache_k'].shape[1]
    if n_pool not in _CACHE:
        _CACHE[n_pool] = build(n_pool)
    nc, consts, _k = _CACHE[n_pool]
    ck = f(inp['cache_k'][0]).reshape(n_pool * 128, 512)
    cv = f(inp['cache_v'][0]).reshape(n_pool * 128, 512)
    shared = {
        'ck': ck, 'cv': cv,
        'w_in': f(inp['w_in'][0]), 'w_out': f(inp['w_out'][0]), 'w_up': f(inp['w_ffn_up'][0]), 'w_dn': f(inp['w_ffn_down'][0]),
        'g1': f(inp['norm_mix_g'][0]), 'g2': f(inp['norm_ffn_g'][0]), 'gf': f(inp['norm_final_g']),
        'mu': f(inp['mu_shift'][0]), 'w0': f(inp['decay_w0'][0]), 'decay_up': f(inp['decay_up'][0]),
        'a0': f(inp['iclr_a0'][0]), 'iclr_up': f(inp['iclr_up'][0]), 'gate_up': f(inp['gate_up'][0]),
        'k_k': f(inp['k_k'][0]), 'k_a': f(inp['k_a'][0]), 'r_k': f(inp['r_k'][0]).reshape(512),
        'lnw': f(inp['ln_x_w'][0]), 'lnb': f(inp['ln_x_b'][0]),
    }
    for n, a in consts.items():
        shared['c_' + n] = a
    in_maps = []
    for c in range(8):
        m = dict(shared)
        m['xp'] = f(inp['x_prompt'][c % 4])
        m['xsm'] = f(inp['x_sample'][4 * c:4 * c + 4, 0])
        m['pt'] = f(inp['page_table'][4 * c:4 * c + 4]).astype(np.int32)
        m['swkv'] = f(inp['state_wkv'][0, 4 * c:4 * c + 4])
        m['sshift'] = f(inp['state_shift'][0, 4 * c:4 * c + 4])
        in_maps.append(m)
    res = run_bass_kernel_spmd(nc, in_maps, core_ids=list(range(8)))
    R = res.results
    y_prompt = np.stack([R[b]['yp'] for b in range(4)])
    y_sample = np.concatenate([R[c]['ysm'] for c in range(8)])[:, None, :]
    k_prompt = np.stack([R[b]['kp'] for b in range(4)]).reshape(1, 4, T, 8, 64)
    v_prompt = np.stack([R[b]['vp'] for b in range(4)]).reshape(1, 4, T, 8, 64)
    wkv_prompt = np.stack([R[b]['wkvp'] for b in range(4)])[None]
    shift_prompt = np.stack([R[b]['shp'][0] for b in range(4)])[None]
    k_sample = np.concatenate([R[c]['ks'] for c in range(8)]).reshape(1, 32, 1, 8, 64)
    v_sample = np.concatenate([R[c]['vs'] for c in range(8)]).reshape(1, 32, 1, 8, 64)
    wkv_sample = np.concatenate([R[c]['wkvs'] for c in range(8)])[None]
    shift_sample = np.concatenate([R[c]['shs'] for c in range(8)])[None]
    return tuple(np.ascontiguousarray(a, dtype=np.float32) for a in
                 (y_prompt, y_sample, k_prompt, v_prompt, wkv_prompt, shift_prompt, k_sample, v_sample, wkv_sample, shift_sample))
```

```python
import contextlib
import numpy as np
import ml_dtypes
import concourse.bass as bass
import concourse.mybir as mybir
from concourse.bass_utils import run_bass_kernel_spmd

F32 = mybir.dt.float32; BF16 = mybir.dt.bfloat16; I32 = mybir.dt.int32
AF = mybir.ActivationFunctionType; ALU = mybir.AluOpType; AX = mybir.AxisListType
T = 4096; D = 1024; NS = 4; NPG = 64
RMS_EPS = 1e-6; GN_EPS = 64e-5
NBIG = 30000.0


class K:
    def __init__(s, nc, same=True):
        s.nc = nc
        s.eng = {'pe': nc.tensor, 'act': nc.scalar, 'dve': nc.vector, 'pool': nc.gpsimd, 'sp': nc.sync}
        s.csem = {}; s.cnt = {}; s.nsem = 0
        for n in s.eng:
            s._newsem(n)
        s.lastw = {}; s.reads = {}
        s.waited = {n: {} for n in s.eng}
        s.same = same
        s.dpool = {}; s.dpos = {}
        s.ninst = 0

    def _newsem(s, n):
        s.nsem += 1
        s.csem[n] = s.nc.alloc_semaphore('c%d_%s' % (s.nsem, n)); s.cnt[n] = 0

    def _need(s, e, reads, writes, extra=()):
        evs = list(extra)
        for r in reads:
            if r in s.lastw: evs.append(s.lastw[r])
        for w in writes:
            if w in s.lastw: evs.append(s.lastw[w])
            evs.extend(s.reads.get(w, ()))
        best = {}
        for (h, v, en) in evs:
            if en == e and (not s.same or e in ('pe', 'sp')): continue
            k = id(h)
            if k not in best or best[k][1] < v: best[k] = (h, v)
        for k, (h, v) in best.items():
            if s.waited[e].get(k, 0) >= v: continue
            s.eng[e].wait_ge(h, v); s.waited[e][k] = v; s.ninst += 1

    def _record(s, ev, reads, writes):
        for w in writes:
            s.lastw[w] = ev; s.reads[w] = []
        for r in reads:
            lst = s.reads.setdefault(r, [])
            lst.append(ev)
            if len(lst) > 16:
                m = {}
                for (h, v, en) in lst:
                    if id(h) not in m or m[id(h)][1] < v: m[id(h)] = (h, v, en)
                s.reads[r] = list(m.values())

    def op(s, e, fn, reads=(), writes=()):
        pr = [r for r in reads if isinstance(r, str) and r.startswith('pb')]
        if pr:
            reads = [r for r in reads if r not in pr]; writes = list(writes) + pr
        s._need(e, reads, writes)
        if s.cnt[e] >= 30000: s._newsem(e)
        ins = fn(s.eng[e])
        s.cnt[e] += 1; s.ninst += 1
        ins.then_inc(s.csem[e], 1)
        ev = (s.csem[e], s.cnt[e], e)
        s._record(ev, reads, writes)
        return ev

    def dma(s, q, out=None, in_=None, reads=(), writes=(), fn=None):
        if q not in s.dpool:
            s.dpool[q] = [[s.nc.alloc_semaphore('d_%s%d' % (q, i)), 0] for i in range(16)]; s.dpos[q] = 0
        slot = s.dpool[q][s.dpos[q] % 16]; s.dpos[q] += 1
        extra = [(slot[0], slot[1], None)] if slot[1] > 0 else []
        s._need(q, reads, writes, extra)
        if fn is None:
            ins = s.eng[q].dma_start(out=out, in_=in_)
        else:
            ins = fn(s.eng[q])
        slot[1] += 16; s.ninst += 1
        ins.then_inc(slot[0], 16)
        ev = (slot[0], slot[1], None)
        s._record(ev, reads, writes)
        return ev

    def barrier(s):
        evs = [(s.csem[n], s.cnt[n], n) for n in s.eng if s.cnt[n] > 0]
        for q in s.dpool:
            for slot in s.dpool[q]:
                if slot[1] > 0: evs.append((slot[0], slot[1], None))
        for e in s.eng:
            best = {}
            for (h, v, en) in evs:
                if en == e: continue
                best[id(h)] = (h, v)
            for kk_, (h, v) in best.items():
                if s.waited[e].get(kk_, 0) >= v: continue
                s.eng[e].wait_ge(h, v); s.waited[e][kk_] = v; s.ninst += 1

    def wait_all(s, e):
        evs = list(s.lastw.values())
        for q in s.dpool:
            for slot in s.dpool[q]:
                if slot[1] > 0: evs.append((slot[0], slot[1], None))
        s._need(e, (), (), evs)


def make_consts():
    bf = ml_dtypes.bfloat16
    c = {}
    c['identf'] = np.eye(128, dtype=np.float32)
    c['identb'] = np.eye(128, dtype=np.float32).astype(bf)
    p = np.arange(128)[:, None]; dl = np.arange(512)[None, :]
    cm = np.zeros((128, 4, 512), np.float32)
    for j in range(4):
        cm[:, j, :] = np.where(dl >= 128 * j + p, 0.0, -NBIG)
    c['cmask'] = cm.astype(bf)
    s = np.arange(T)
    ka = np.zeros((18, T), np.float32)
    for n in range(16):
        ka[n] = (s // 256 == n)
    ka[16] = 1; ka[17] = 1
    c['kaug'] = ka.astype(bf)
    slopes = 2.0 ** (-np.arange(1, 9, dtype=np.float64))
    qa = np.zeros((2, 8, 512), np.float32)
    d = np.arange(512)
    for h in range(8):
        qa[0, h] = -slopes[h] * (d % 256)
        qa[1, h] = -slopes[h] * 256 * (d // 256)
    c['qaug'] = qa.astype(bf)
    ab = np.zeros((128, 8, 36), np.float32)
    for h in range(8):
        for r in range(36):
            ab[:, h, r] = slopes[h] * (128 * (r - 28) + np.arange(128))
    c['ab'] = ab
    gm = np.zeros((17, 16), np.float32); oh = np.zeros((17, 16), np.float32)
    for nq in range(17):
        gm[nq, nq:] = -1e30
        if nq < 16: oh[nq, nq] = 1
    c['gmask'] = np.broadcast_to(gm[None], (128, 17, 16)).copy()
    c['ownhot'] = np.broadcast_to(oh[None], (128, 17, 16)).copy()
    al = np.zeros((128, 65, 8), np.float32)
    for h in range(8):
        for pg in range(64):
            al[:, pg, h] = -slopes[h] * (8192 - (128 * pg + np.arange(128)))
        al[:, 64, h] = -NBIG; al[0, 64, h] = 0.0
    c['alis'] = al
    dm = np.zeros((8, 512), np.float32)
    for h in range(8): dm[h, h * 64:(h + 1) * 64] = 1
    c['diagm'] = dm
    s4 = np.zeros((4, 4, 128), np.float32)
    for i in range(4): s4[i, i, :] = 1
    c['sel4'] = s4
    o4 = np.zeros((128, 4, 4), np.float32)
    for i in range(4): o4[:, i, i] = 1
    c['oh4'] = o4
    s8 = np.zeros((8, 4, 4), np.float32)
    for i in range(4): s8[:, i, i] = 1
    c['sel8'] = s8
    c['iotaf'] = np.arange(128, dtype=np.float32)[:, None].copy()
    return c


def build(n_pool):
    import os
    KSTOP = int(os.environ.get('KSTOP', '9')); NT = int(os.environ.get('KNT', '32'))
    PT = list(range(NT))
    nc = bass.Bass("TRN2", target_bir_lowering=False)
    consts = make_consts()
    dt_of = lambda a: BF16 if a.dtype == ml_dtypes.bfloat16 else (I32 if a.dtype == np.int32 else F32)

    def din(name, shape, dt=F32):
        return nc.dram_tensor(name, list(shape), dt, kind="ExternalInput").ap()

    def dout(name, shape, dt=F32):
        return nc.dram_tensor(name, list(shape), dt, kind="ExternalOutput").ap()

    def dscr(name, shape, dt=F32):
        return nc.dram_tensor(name, list(shape), dt).ap()

    xp = din("xp", [T, D]); xsm = din("xsm", [NS, D])
    ck = din("ck", [n_pool * 128, 512]); cv = din("cv", [n_pool * 128, 512])
    pt = din("pt", [NS, NPG], I32)
    swkv = din("swkv", [NS, 8, 64, 64]); sshift = din("sshift", [NS, 1792])
    w_in = din("w_in", [D, 3328]); w_out = din("w_out", [D, D]); w_up = din("w_up", [D, 4096]); w_dn = din("w_dn", [4096, D])
    g1 = din("g1", [D]); g2 = din("g2", [D]); gf = din("gf", [D])
    mu = din("mu", [1792]); w0 = din("w0", [512]); decay_up = din("decay_up", [64, 512])
    a0 = din("a0", [512]); iclr_up = din("iclr_up", [64, 512]); gate_up = din("gate_up", [128, 512])
    k_k = din("k_k", [512]); k_a = din("k_a", [512]); r_k = din("r_k", [512]); lnw = din("lnw", [512]); lnb = din("lnb", [512])
    cin = {n: din("c_" + n, a.shape, dt_of(a)) for n, a in consts.items()}

    yp = dout("yp", [T, D]); ysm = dout("ysm", [NS, D])
    kp = dout("kp", [T, 512]); vp = dout("vp", [T, 512])
    wkvp = dout("wkvp", [8, 64, 64]); shp = dout("shp", [1, 1792])
    ks = dout("ks", [NS, 512]); vs = dout("vs", [NS, 512])
    wkvs = dout("wkvs", [NS, 8, 64, 64]); shs = dout("shs", [NS, 1792])

    TT = T + 128
    P = dscr("P", [TT + 1, 1792])
    Bs = dscr("Bs", [TT, 512]); KMs = dscr("KMs", [TT, 512]); Vs = dscr("Vs", [TT, 512]); Gs = dscr("Gs", [TT, 512])
    BON = dscr("BON", [TT, 8])
    Ysc = dscr("Ysc", [T + 2, 512])
    Yss = dscr("Yss", [NS, 2, 512])
    mixT = dscr("mixT", [D, TT], BF16)
    QKVs = dscr("QKVs", [NS, 1536])

    k = K(nc)
    top = contextlib.ExitStack()
    top.enter_context(nc.allow_non_contiguous_dma(reason='small strided parameter loads'))
    def SB(es, name, shape, dt): return es.enter_context(nc.sbuf_tensor(name, list(shape), dt))
    pb = [top.enter_context(nc.psum_tensor("pb%d" % i, [128, 512], F32)) for i in range(7)]
    pbT = top.enter_context(nc.psum_tensor("pbT", [128, 1024], BF16))
    identf = SB(top, "identf", [128, 128], F32); identb = SB(top, "identb", [128, 128], BF16)
    zero_sb = SB(top, "zero_sb", [128, 16], F32)
    ones_sb = SB(top, "ones_sb", [128, 64], F32)
    k.dma('sp', identf[:], cin['identf'][:, :], writes=['identf'])
    k.dma('sp', identb[:], cin['identb'][:, :], writes=['identb'])
    k.op('pool', lambda e: e.memset(zero_sb[:], 0.0), writes=['zero_sb'])
    k.op('pool', lambda e: e.memset(ones_sb[:], 1.0), writes=['ones_sb'])
    k.dma('sp', P[0:1, :].rearrange("o (a b) -> (o a) b", b=16), zero_sb[0:112, 0:16], reads=['zero_sb'], writes=['P'])

    rr = [0]
    def evac(out, in_, reads, writes, scale=None):
        rr[0] += 1
        if rr[0] % 2 == 0 and scale is None:
            return k.op('dve', lambda e: e.tensor_copy(out=out, in_=in_), reads=reads, writes=writes)
        if scale is None:
            return k.op('act', lambda e: e.activation(out=out, in_=in_, func=AF.Copy), reads=reads, writes=writes)
        return k.op('act', lambda e: e.activation(out=out, in_=in_, func=AF.Copy, scale=scale), reads=reads, writes=writes)

    def rmsnorm_T(es_tensors, src_tile, srcname, nm):
        ss, rs, xnb, xnT = es_tensors
        k.op('act', lambda e: e.activation(out=xnb[:], in_=src_tile[:], func=AF.Square, accum_out=ss[:, 0:1]), reads=[srcname], writes=['xnb' + nm, 'ss' + nm])
        k.op('act', lambda e: e.activation(out=rs[:], in_=ss[:], func=AF.Sqrt, scale=1.0 / D, bias=RMS_EPS), reads=['ss' + nm], writes=['rs' + nm])
        k.op('dve', lambda e: e.reciprocal(out=rs[:], in_=rs[:]), reads=['rs' + nm], writes=['rs' + nm])
        k.op('dve', lambda e: e.tensor_scalar(out=xnb[:], in0=src_tile[:], scalar1=rs[:, 0:1], scalar2=None, op0=ALU.mult), reads=[srcname, 'rs' + nm], writes=['xnb' + nm])
        for kc in range(8):
            k.op('pe', lambda e, kc=kc: e.transpose(out=pbT[:, kc * 128:(kc + 1) * 128], in_=xnb[:, kc * 128:(kc + 1) * 128], identity=identb[:]),
                 reads=['xnb' + nm, 'identb'], writes=['pbT'])
        k.op('act', lambda e: e.activation(out=xnT[:].rearrange("p a b -> p (a b)"), in_=pbT[:, :], func=AF.Copy), reads=['pbT'], writes=['xnT' + nm])

    def load_weight(wsb, wdram, nkc, ncols, gdram, stg, es_name, cs=3328, lo=None):
        if gdram is not None:
            k.dma('sp', gcol[:, 0:nkc], gdram.rearrange("(a p) -> p a", p=128), writes=['gcol'])
        for kc in range(nkc):
            for c0 in range(0, ncols, cs):
                cw = min(cs, ncols - c0)
                k.dma('sp', stg[:, 0:cw], wdram[kc * 128:(kc + 1) * 128, c0:c0 + cw], writes=['stg'])
                if gdram is not None and lo is not None and c0 == 0:
                    k.op('dve', lambda e, kc=kc, cw=cw: e.tensor_scalar(out=stg[:, 0:cw], in0=stg[:, 0:cw], scalar1=gcol[:, kc:kc + 1], scalar2=None, op0=ALU.mult), reads=['stg', 'gcol'], writes=['stg'])
                    k.op('dve', lambda e, kc=kc, cw=cw: e.tensor_copy(out=wsb[:, kc, 0:cw], in_=stg[:, 0:cw]), reads=['stg'], writes=[es_name])
                    k.op('dve', lambda e, kc=kc: e.tensor_tensor(out=lo[:, kc, :], in0=stg[:, 0:512], in1=wsb[:, kc, 0:512], op=ALU.subtract), reads=['stg', es_name], writes=['wlo'])
                elif gdram is not None:
                    k.op('dve', lambda e, kc=kc, c0=c0, cw=cw: e.tensor_scalar(out=wsb[:, kc, c0:c0 + cw], in0=stg[:, 0:cw], scalar1=gcol[:, kc:kc + 1], scalar2=None, op0=ALU.mult),
                         reads=['stg', 'gcol'], writes=[es_name])
                else:
                    k.op('dve', lambda e, kc=kc, c0=c0, cw=cw: e.tensor_copy(out=wsb[:, kc, c0:c0 + cw], in_=stg[:, 0:cw]), reads=['stg'], writes=[es_name])

    gcol = SB(top, "gcol", [128, 8], F32)

    with contextlib.ExitStack() as es:
        win = SB(es, "win", [128, 8, 3328], BF16)
        kTa = [SB(es, "kTa%d" % h, [82, T], BF16) for h in range(8)]
        vaug = SB(es, "vaug", [128, 32, 8, 65], BF16)
        qTa = SB(es, "qTa", [82, 8, 512], BF16)
        qT32 = SB(es, "qT32", [64, 8, 128], F32)
        ksum2 = SB(es, "ksum2", [64, 8, 32], F32)
        kmT = SB(es, "kmT", [64, 8, 16], F32)
        xt = SB(es, "xt", [128, D], F32)
        ss = SB(es, "ss", [128, 1], F32); rs = SB(es, "rs", [128, 1], F32)
        xnb = SB(es, "xnb", [128, D], BF16); xnT = SB(es, "xnT", [128, 8, 128], BF16)
        xlo = SB(es, "xlo", [128, D], BF16); xloT = SB(es, "xloT", [128, 8, 128], BF16)
        wlo = SB(es, "wlo", [128, 8, 512], BF16)
        proj = SB(es, "proj", [128, 1792], F32)
        cmask = SB(es, "cmask", [128, 4, 512], BF16)
        ab = SB(es, "ab", [128, 8, 36], F32)
        gmaskc = SB(es, "gmaskc", [128, 17, 16], F32); ownhot = SB(es, "ownhot", [128, 17, 16], F32)
        gm = SB(es, "gm", [128, 8, 16], F32); m01 = gm
        top8 = SB(es, "top8", [128, 8, 8], F32); thr = SB(es, "thr", [128, 8], F32)
        selpad = SB(es, "selpad", [128, 8, 80], F32)
        pT = [SB(es, "pT%d" % i, [128, 512], BF16) for i in range(2)]
        stmp = SB(es, "stmp", [128, 512], F32)
        osb = stmp; lsb = stmp
        atT = [SB(es, "atT%d" % i, [64, 512], BF16) for i in range(2)]

        k.dma('sp', cmask[:], cin['cmask'][:, :, :], writes=['cmask'])
        k.dma('sp', ab[:], cin['ab'][:, :, :], writes=['ab'])
        k.dma('sp', gmaskc[:], cin['gmask'][:, :, :], writes=['gmaskc'])
        k.dma('sp', ownhot[:], cin['ownhot'][:, :, :], writes=['ownhot'])
        for h in range(8):
            k.dma('sp', kTa[h][64:82, :], cin['kaug'][:, :], writes=[('kTa', h)])
        k.dma('sp', qTa[80:82, :, :], cin['qaug'][:, :, :], writes=['qTa'])
        k.op('pool', lambda e: e.memset(vaug[:].rearrange("p a b c -> p (a b c)"), 1.0), writes=['vaug'])
        k.op('pool', lambda e: e.memset(kmT[:].rearrange("p a b -> p (a b)"), 0.0), writes=['kmT'])
        k.op('pool', lambda e: e.memset(selpad[:].rearrange("p a b -> p (a b)"), 0.0), writes=['selpad'])
        load_weight(win, w_in, 8, 3328, g1, proj, 'win', cs=1664, lo=wlo)

        chunks = [(0, 512), (512, 512), (1024, 512), (1536, 512), (2048, 512), (2560, 512), (3072, 256)]
        for i in PT + [32]:
            samp = (i == 32)
            if samp:
                k.op('pool', lambda e: e.memset(xt[:], 0.0), writes=['xt'])
                k.dma('sp', xt[0:NS, :], xsm[:, :], reads=['xt'], writes=['xt'])
            else:
                k.dma('sp', xt[:], xp[i * 128:(i + 1) * 128, :], writes=['xt'])
            rmsnorm_T((ss, rs, xnb, xnT), xt, 'xt', '1')
            k.op('dve', lambda e: e.scalar_tensor_tensor(out=xlo[:], in0=xt[:], scalar=rs[:, 0:1], in1=xnb[:], op0=ALU.mult, op1=ALU.subtract), reads=['xt', 'rs1', 'xnb1'], writes=['xlo'])
            for kc in range(8):
                k.op('pe', lambda e, kc=kc: e.transpose(out=pbT[:, kc * 128:(kc + 1) * 128], in_=xlo[:, kc * 128:(kc + 1) * 128], identity=identb[:]), reads=['xlo', 'identb'], writes=['pbT'])
            k.op('act', lambda e: e.activation(out=xloT[:].rearrange("p a b -> p (a b)"), in_=pbT[:, :], func=AF.Copy), reads=['pbT'], writes=['xloT'])
            def pcol(ci):
                return (chunks[ci][0] if ci < 3 else chunks[ci][0] - 1536), (('proj', ci % 3) if ci < 6 else ('proj', 3))
            for ci, (c0, cw) in enumerate(chunks):
                bank = pb[ci % 2]; bn = 'pb%d' % (ci % 2)
                if ci == 0 and not samp:
                    continue
                for kc in range(8):
                    k.op('pe', lambda e, kc=kc, c0=c0, cw=cw, bank=bank: e.matmul(bank[:, 0:cw], xnT[:, kc, :], win[:, kc, c0:c0 + cw], start=(kc == 0), stop=(kc == 7 and ci != 0)),
                         reads=['xnT1', 'win'], writes=[bn])
                if ci == 0:
                    for kc in range(8):
                        k.op('pe', lambda e, kc=kc, bank=bank: e.matmul(bank[:, 0:512], xnT[:, kc, :], wlo[:, kc, :], start=False, stop=False), reads=['xnT1', 'wlo'], writes=[bn])
                    for kc in range(8):
                        k.op('pe', lambda e, kc=kc, bank=bank: e.matmul(bank[:, 0:512], xloT[:, kc, :], win[:, kc, 0:512], start=False, stop=(kc == 7)), reads=['xloT', 'win'], writes=[bn])
                pc0, pres = pcol(ci)
                evac(proj[:, pc0:pc0 + cw], bank[:, 0:cw], [bn], [pres])
                if ci == 2:
                    pr3 = [('proj', 0), ('proj', 1), ('proj', 2)]
                    if not samp:
                        k.dma('sp', kp[i * 128:(i + 1) * 128, :], proj[:, 512:1024], reads=[('proj', 1)], writes=['kp'])
                        k.dma('sp', vp[i * 128:(i + 1) * 128, :], proj[:, 1024:1536], reads=[('proj', 2)], writes=['vp'])
                        k.op('pool', lambda e, i=i: e.tensor_copy(out=vaug[:, i, :, 0:64], in_=proj[:, 1024:1536].rearrange("p (h d) -> p h d", h=8)), reads=[('proj', 2)], writes=['vaug'])
                    else:
                        k.dma('sp', ks[:, :], proj[0:NS, 512:1024], reads=[('proj', 1)], writes=['ks'])
                        k.dma('sp', vs[:, :], proj[0:NS, 1024:1536], reads=[('proj', 2)], writes=['vs'])
                        k.dma('sp', QKVs[:, :], proj[0:NS, 0:1536], reads=pr3, writes=['QKVs'])
            pr4 = [('proj', 0), ('proj', 1), ('proj', 2), ('proj', 3)]
            if not samp:
                k.dma('sp', P[1 + i * 128:1 + (i + 1) * 128, :], proj[:, 0:1792], reads=pr4, writes=['P'])
                if i == 31:
                    k.dma('sp', shp[0:1, :], proj[127:128, 0:1792], reads=pr4, writes=['shp'])
            else:
                k.dma('sp', P[1 + T:1 + T + NS, :], proj[0:NS, 0:1792], reads=pr4, writes=['P'])
                k.dma('sp', shs[:, :], proj[0:NS, 0:1792], reads=pr4, writes=['shs'])
                continue
            tcol = (i % 4) * 128
            nq = i // 2
            for h in range(8):
                bank = pb[2 + h // 4]; bn = 'pb%d' % (2 + h // 4); col = (h % 4) * 128
                for kc in range(8):
                    k.op('pe', lambda e, kc=kc, h=h, bank=bank, col=col: e.matmul(bank[0:64, col:col + 128], win[:, kc, h * 64:(h + 1) * 64], xnT[:, kc, :], start=(kc == 0), stop=False),
                         reads=['xnT1', 'win'], writes=[bn])
                for kc in range(8):
                    k.op('pe', lambda e, kc=kc, h=h, bank=bank, col=col: e.matmul(bank[0:64, col:col + 128], wlo[:, kc, h * 64:(h + 1) * 64], xnT[:, kc, :], start=False, stop=False),
                         reads=['xnT1', 'wlo'], writes=[bn])
                for kc in range(8):
                    k.op('pe', lambda e, kc=kc, h=h, bank=bank, col=col: e.matmul(bank[0:64, col:col + 128], win[:, kc, h * 64:(h + 1) * 64], xloT[:, kc, :], start=False, stop=(kc == 7)),
                         reads=['xloT', 'win'], writes=[bn])
            for h in range(8):
                bank = pb[2 + h // 4]; bn = 'pb%d' % (2 + h // 4); col = (h % 4) * 128
                k.op('act', lambda e, h=h, bank=bank, col=col: e.activation(out=qTa[0:64, h, tcol:tcol + 128], in_=bank[0:64, col:col + 128], func=AF.Copy, scale=0.125),
                     reads=[bn], writes=['qTa'])
                k.op('dve', lambda e, h=h, bank=bank, col=col: e.tensor_copy(out=qT32[:, h, :], in_=bank[0:64, col:col + 128]), reads=[bn], writes=['qT32'])
            for h in range(8):
                bank = pb[2 + h // 4]; bn = 'pb%d' % (2 + h // 4); col = (h % 4) * 128
                for kc in range(8):
                    k.op('pe', lambda e, kc=kc, h=h, bank=bank, col=col: e.matmul(bank[0:64, col:col + 128], win[:, kc, 512 + h * 64:512 + (h + 1) * 64], xnT[:, kc, :], start=(kc == 0), stop=(kc == 7)),
                         reads=['xnT1', 'win'], writes=[bn])
            for h in range(8):
                bank = pb[2 + h // 4]; bn = 'pb%d' % (2 + h // 4); col = (h % 4) * 128
                k.op('act', lambda e, h=h, bank=bank, col=col: e.activation(out=kTa[h][0:64, i * 128:(i + 1) * 128], in_=bank[0:64, col:col + 128], func=AF.Copy, accum_out=ksum2[:, h, i:i + 1]),
                     reads=[bn], writes=[('kTa', h), 'ksum2'])
            for h in range(8):
                k.op('pe', lambda e, h=h: e.matmul(pb[4][:, h * 16:(h + 1) * 16], qT32[:, h, :], kmT[:, h, :], start=True, stop=True), reads=['qT32', 'kmT'], writes=['pb4'])
            k.op('dve', lambda e: e.tensor_tensor(out=gm[:], in0=pb[4][:, 0:128].rearrange("p (h n) -> p h n", h=8), in1=gmaskc[:, nq, :].unsqueeze(1).broadcast_to([128, 8, 16]), op=ALU.add),
                 reads=['pb4', 'gmaskc'], writes=['gm'])
            for h in range(8):
                k.op('dve', lambda e, h=h: e.max(out=top8[:, h, :], in_=gm[:, h, :]), reads=['gm'], writes=['top8'])
            k.op('dve', lambda e: e.tensor_scalar(out=thr[:], in0=top8[:, :, 2], scalar1=-1e29, scalar2=None, op0=ALU.max), reads=['top8'], writes=['thr'])
            k.op('dve', lambda e: e.tensor_tensor(out=m01[:], in0=gm[:], in1=thr[:].unsqueeze(2).broadcast_to([128, 8, 16]), op=ALU.is_ge), reads=['gm', 'thr'], writes=['gm'])
            k.op('dve', lambda e: e.tensor_tensor(out=m01[:], in0=m01[:], in1=ownhot[:, nq, :].unsqueeze(1).broadcast_to([128, 8, 16]), op=ALU.add), reads=['gm', 'ownhot'], writes=['gm'])
            k.op('dve', lambda e: e.tensor_scalar(out=selpad[:, :, 64:80], in0=m01[:], scalar1=-1.0, scalar2=NBIG, op0=ALU.add, op1=ALU.mult), reads=['gm'], writes=['selpad'])
            for h in range(8):
                bank = pb[2 + h // 4]; bn = 'pb%d' % (2 + h // 4); col = (h % 4) * 128
                k.op('pe', lambda e, h=h, bank=bank, col=col: e.matmul(bank[0:80, col:col + 128], selpad[:, h, :], identf[:, :], start=True, stop=True), reads=['selpad', 'identf'], writes=[bn])
            for g in range(2):
                k.op('act', lambda e, g=g: e.activation(out=qTa[64:80, 4 * g:4 * g + 4, tcol:tcol + 128], in_=pb[2 + g][64:80, :].rearrange("p (h t) -> p h t", h=4), func=AF.Copy),
                     reads=['pb%d' % (2 + g)], writes=['qTa'])
            if i % 2 == 1:
                k.op('dve', lambda e: e.tensor_tensor(out=kmT[:, :, nq], in0=ksum2[:, :, i - 1], in1=ksum2[:, :, i], op=ALU.add), reads=['ksum2'], writes=['kmT'])
            if i % 4 != 3:
                continue
            TQ = i // 4
            nkt = 4 * TQ + 4
            for h in range(8):
                def emit_o(kt):
                    pt_ = pT[kt % 2]; ptn = 'pT%d' % (kt % 2)
                    k.op('pe', lambda e, h=h, kt=kt, pt_=pt_: e.matmul(pb[2][0:65, :], vaug[:, kt, h, :], pt_[:], start=(kt == 0), stop=(kt == nkt - 1)),
                         reads=['vaug', ptn], writes=['pb2'])
                for kt in range(nkt):
                    sbk = pb[5 + kt % 2]; sbn = 'pb%d' % (5 + kt % 2); pt_ = pT[kt % 2]; ptn = 'pT%d' % (kt % 2)
                    k.op('pe', lambda e, h=h, kt=kt, sbk=sbk: e.matmul(sbk[:, :], kTa[h][0:82, kt * 128:(kt + 1) * 128], qTa[0:82, h, :], start=True, stop=True),
                         reads=[('kTa', h), 'qTa'], writes=[sbn])
                    if kt >= 1:
                        emit_o(kt - 1)
                    rel = kt - 4 * TQ + 28
                    if kt >= 4 * TQ:
                        j = kt - 4 * TQ
                        k.op('dve', lambda e, h=h, sbk=sbk, rel=rel, j=j: e.scalar_tensor_tensor(out=stmp[:], in0=sbk[:, :], scalar=ab[:, h, rel:rel + 1], in1=cmask[:, j, :], op0=ALU.add, op1=ALU.add),
                             reads=[sbn, 'ab', 'cmask'], writes=['stmp', 'osb', 'lsb'])
                        k.op('act', lambda e, pt_=pt_: e.activation(out=pt_[:], in_=stmp[:], func=AF.Exp), reads=['stmp'], writes=[ptn])
                    else:
                        k.op('act', lambda e, h=h, sbk=sbk, pt_=pt_, rel=rel: e.activation(out=pt_[:], in_=sbk[:, :], func=AF.Exp, bias=ab[:, h, rel:rel + 1], scale=1.0),
                             reads=[sbn, 'ab'], writes=[ptn])
                emit_o(nkt - 1)
                k.op('act', lambda e: e.activation(out=lsb[64:65, :], in_=pb[2][64:65, :], func=AF.Copy), reads=['pb2', 'stmp'], writes=['lsb'])
                k.op('dve', lambda e: e.reciprocal(out=lsb[64:65, :], in_=lsb[64:65, :]), reads=['lsb'], writes=['lsb'])
                k.op('pe', lambda e: e.matmul(pb[3][0:64, :], ones_sb[64:65, 0:64], lsb[64:65, :], start=True, stop=True), reads=['ones_sb', 'lsb'], writes=['pb3'])
                k.op('act', lambda e: e.activation(out=osb[0:64, :], in_=pb[2][0:64, :], func=AF.Copy), reads=['pb2', 'stmp'], writes=['osb'])
                at = atT[h % 2]; atn = 'atT%d' % (h % 2)
                k.op('dve', lambda e, at=at: e.tensor_tensor(out=at[:], in0=osb[0:64, :], in1=pb[3][0:64, :], op=ALU.mult), reads=['osb', 'pb3', 'stmp'], writes=[atn])
                k.dma('sp', mixT[h * 64:(h + 1) * 64, TQ * 512:(TQ + 1) * 512], at[:], reads=[atn], writes=['mixT'])

    k.barrier()
    with contextlib.ExitStack() as es:
        ptb = SB(es, "ptb", [128, NS * NPG], I32); ptf = SB(es, "ptf", [128, NS * NPG], F32)
        iotaf = SB(es, "iotaf", [128, 1], F32); idx = SB(es, "idx", [128, NS * NPG], I32)
        q4s = SB(es, "q4s", [4, 512], F32)
        qkv4 = SB(es, "qkv4", [4, 1536], F32)
        k.dma('sp', qkv4[:, :], QKVs[:, :], reads=['QKVs'], writes=['qkv4'])
        sel4 = SB(es, "sel4", [4, 4, 128], F32); oh4 = SB(es, "oh4", [128, 4, 4], F32); sel8 = SB(es, "sel8", [8, 4, 4], F32)
        alis = SB(es, "alis", [128, 65, 8], F32); diagm = SB(es, "diagm", [8, 512], F32)
        qrep = SB(es, "qrep", [128, 512], F32)
        kx = SB(es, "kx", [128, 512], F32); vx = SB(es, "vx", [128, 512], F32)
        kpg = [SB(es, "kpg%d" % i, [128, 512], F32) for i in range(3)]
        tmp = SB(es, "tmp", [128, 512], F32)
        sc = SB(es, "sc", [128, 65, 8], F32); pS = SB(es, "pS", [128, 65, 8], F32)
        t4 = SB(es, "t4", [4, 512], F32); gate4 = SB(es, "gate4", [4, 32, 8], F32)
        top84 = SB(es, "top84", [4, 8, 8], F32); m4 = SB(es, "m4", [4, 32, 8], F32)
        psp = SB(es, "psp", [128, 8], F32); rl = SB(es, "rl", [8, 1], F32); o8 = SB(es, "o8", [8, 512], F32)
        at4 = SB(es, "at4", [4, 512], BF16); at4T = SB(es, "at4T", [128, 4, 4], BF16)
        k.dma('sp', ptb[:], pt.rearrange("s p -> (s p)").partition_broadcast(128), writes=['ptb'])
        k.dma('sp', iotaf[:], cin['iotaf'][:, :], writes=['iotaf'])
        for (t_, n_) in ((sel4, 'sel4'), (oh4, 'oh4'), (sel8, 'sel8'), (alis, 'alis')):
            k.dma('sp', t_[:], cin[n_][:, :, :], writes=[n_])
        k.dma('sp', diagm[:], cin['diagm'][:, :], writes=['diagm'])
        k.op('dve', lambda e: e.tensor_copy(out=ptf[:], in_=ptb[:]), reads=['ptb'], writes=['ptf'])
        k.op('dve', lambda e: e.tensor_scalar(out=ptf[:], in0=ptf[:], scalar1=128.0, scalar2=iotaf[:, 0:1], op0=ALU.mult, op1=ALU.add), reads=['ptf', 'iotaf'], writes=['ptf'])
        k.op('dve', lambda e: e.tensor_copy(out=idx[:], in_=ptf[:]), reads=['ptf'], writes=['idx'])
        k.op('dve', lambda e: e.tensor_scalar(out=q4s[:], in0=qkv4[:, 0:512], scalar1=0.125, scalar2=None, op0=ALU.mult), reads=['qkv4'], writes=['q4s'])
        k.op('pool', lambda e: e.memset(kx[:], 0.0), writes=['kx'])
        k.op('pool', lambda e: e.memset(vx[:], 0.0), writes=['vx'])
        for s in range(NS):
            k.op('pe', lambda e, s=s: e.matmul(pb[0][:, :], sel4[0:4, s, :], q4s[0:4, :], start=True, stop=True), reads=['sel4', 'q4s'], writes=['pb0'])
            k.op('act', lambda e: e.activation(out=qrep[:], in_=pb[0][:, :], func=AF.Copy), reads=['pb0'], writes=['qrep'])
            k.dma('sp', kx[0:1, :], qkv4[s:s + 1, 512:1024], reads=['qkv4', 'kx'], writes=['kx'])
            k.dma('sp', vx[0:1, :], qkv4[s:s + 1, 1024:1536], reads=['qkv4', 'vx'], writes=['vx'])
            for pg in range(65):
                if pg < 64:
                    kb = kpg[pg % 3]; kbn = 'kpg%d' % (pg % 3); col = s * NPG + pg
                    k.dma('pool', reads=['idx'], writes=[kbn], fn=lambda e, kb=kb, col=col: e.indirect_dma_start(
                        out=kb[:, :], out_offset=None, in_=ck[:, :], in_offset=bass.IndirectOffsetOnAxis(ap=idx[:, col:col + 1], axis=0)))
                else:
                    kb = kx; kbn = 'kx'
                k.op('dve', lambda e, kb=kb: e.tensor_tensor(out=tmp[:], in0=kb[:], in1=qrep[:], op=ALU.mult), reads=[kbn, 'qrep'], writes=['tmp'])
                k.op('dve', lambda e, pg=pg: e.tensor_reduce(out=sc[:, pg, :], in_=tmp[:].rearrange("p (h d) -> p h d", h=8), axis=AX.X, op=ALU.add), reads=['tmp'], writes=['sc'])
                if pg < 64:
                    n = pg // 2; bank = pb[1 + n % 2]; bn = 'pb%d' % (1 + n % 2)
                    k.op('pe', lambda e, s=s, kb=kb, bank=bank, pg=pg: e.matmul(bank[0:4, :], oh4[:, s, :], kb[:], start=(pg % 2 == 0), stop=(pg % 2 == 1)), reads=[kbn, 'oh4'], writes=[bn])
                    if pg % 2 == 1:
                        k.op('dve', lambda e, bank=bank: e.tensor_tensor(out=t4[:], in0=bank[0:4, :], in1=q4s[:], op=ALU.mult), reads=[bn, 'q4s'], writes=['t4'])
                        k.op('dve', lambda e, n=n: e.tensor_reduce(out=gate4[:, n, :], in_=t4[:].rearrange("p (h d) -> p h d", h=8), axis=AX.X, op=ALU.add), reads=['t4'], writes=['gate4'])
            for h in range(8):
                k.op('dve', lambda e, h=h: e.max(out=top84[:, h, :], in_=gate4[:, :, h]), reads=['gate4'], writes=['top84'])
            for h in range(8):
                k.op('dve', lambda e, h=h: e.tensor_scalar(out=m4[:, :, h], in0=gate4[:, :, h], scalar1=top84[:, h, 2:3], scalar2=None, op0=ALU.is_ge), reads=['gate4', 'top84'], writes=['m4'])
            k.op('dve', lambda e: e.tensor_scalar(out=m4[:].rearrange("p a b -> p (a b)"), in0=m4[:].rearrange("p a b -> p (a b)"), scalar1=-1.0, scalar2=NBIG, op0=ALU.add, op1=ALU.mult), reads=['m4'], writes=['m4'])
            k.op('pe', lambda e, s=s: e.matmul(pb[0][:, 0:256], sel4[0:4, s, :], m4[:].rearrange("p a b -> p (a b)"), start=True, stop=True), reads=['sel4', 'm4'], writes=['pb0'])
            k.op('dve', lambda e: e.tensor_tensor(out=sc[:, 0:64, :].rearrange("p (n two) h -> p n two h", two=2), in0=sc[:, 0:64, :].rearrange("p (n two) h -> p n two h", two=2),
                                                  in1=pb[0][:, 0:256].rearrange("p (n h) -> p n h", h=8).unsqueeze(2).broadcast_to([128, 32, 2, 8]), op=ALU.add), reads=['sc', 'pb0'], writes=['sc'])
            k.op('dve', lambda e: e.tensor_tensor(out=sc[:].rearrange("p a b -> p (a b)"), in0=sc[:].rearrange("p a b -> p (a b)"), in1=alis[:].rearrange("p a b -> p (a b)"), op=ALU.add), reads=['sc', 'alis'], writes=['sc'])
            k.op('act', lambda e: e.activation(out=pS[:].rearrange("p a b -> p (a b)"), in_=sc[:].rearrange("p a b -> p (a b)"), func=AF.Exp), reads=['sc'], writes=['pS'])
            k.op('dve', lambda e: e.tensor_reduce(out=psp[:], in_=pS[:].rearrange("p g h -> p h g"), axis=AX.X, op=ALU.add), reads=['pS'], writes=['psp'])
            k.op('pe', lambda e: e.matmul(pb[3][0:8, 0:1], psp[:], ones_sb[:, 0:1], start=True, stop=True), reads=['psp', 'ones_sb'], writes=['pb3'])
            for pg in range(65):
                if pg < 64:
                    vb = kpg[pg % 3]; vbn = 'kpg%d' % (pg % 3); col = s * NPG + pg
                    k.dma('pool', reads=['idx'], writes=[vbn], fn=lambda e, vb=vb, col=col: e.indirect_dma_start(
                        out=vb[:, :], out_offset=None, in_=cv[:, :], in_offset=bass.IndirectOffsetOnAxis(ap=idx[:, col:col + 1], axis=0)))
                else:
                    vb = vx; vbn = 'vx'
                k.op('pe', lambda e, vb=vb, pg=pg: e.matmul(pb[4][0:8, :], pS[:, pg, :], vb[:], start=(pg == 0), stop=(pg == 64)), reads=[vbn, 'pS'], writes=['pb4'])
            k.op('dve', lambda e: e.reciprocal(out=rl[:], in_=pb[3][0:8, 0:1]), reads=['pb3'], writes=['rl'])
            k.op('dve', lambda e: e.scalar_tensor_tensor(out=o8[:], in0=pb[4][0:8, :], scalar=rl[:, 0:1], in1=diagm[:], op0=ALU.mult, op1=ALU.mult), reads=['pb4', 'rl', 'diagm'], writes=['o8'])
            k.op('pe', lambda e, s=s: e.matmul(pb[5][0:4, :], sel8[0:8, s, :], o8[:], start=(s == 0), stop=(s == NS - 1)), reads=['sel8', 'o8'], writes=['pb5'])
        k.op('act', lambda e: e.activation(out=at4[:], in_=pb[5][0:4, :], func=AF.Copy), reads=['pb5'], writes=['at4'])
        for c in range(4):
            k.op('pe', lambda e, c=c: e.transpose(out=pbT[:, c * 4:c * 4 + 4], in_=at4[0:4, c * 128:(c + 1) * 128], identity=identb[0:4, 0:4]), reads=['at4', 'identb'], writes=['pbT'])
        k.op('act', lambda e: e.activation(out=at4T[:].rearrange("p a b -> p (a b)"), in_=pbT[:, 0:16], func=AF.Copy), reads=['pbT'], writes=['at4T'])
        for c in range(4):
            k.dma('sp', mixT[c * 128:(c + 1) * 128, T:T + NS], at4T[:, c, :], reads=['at4T'], writes=['mixT'])

    k.barrier()
    EM05 = float(np.exp(-0.5))
    with contextlib.ExitStack() as es:
      if KSTOP >= 3:
        def rep(name, src, n):
            t_ = SB(es, name, [128, n], F32)
            k.dma('sp', t_[:], src.partition_broadcast(128), writes=[name])
            return t_
        mu_r = rep("mu_r", mu, 1792); w0_r = rep("w0_r", w0, 512); a0_r = rep("a0_r", a0, 512)
        kk_r = rep("kk_r", k_k, 512); ka_r = rep("ka_r", k_a, 512); rk_r = rep("rk_r", r_k, 512)
        lnw_r = rep("lnw_r", lnw, 512); lnb_r = rep("lnb_r", lnb, 512)
        dup = SB(es, "dup", [64, 512], BF16); iup = SB(es, "iup", [64, 512], BF16); gup = SB(es, "gup", [128, 512], BF16)
        stg2 = SB(es, "stg2", [128, 512], F32)
        for (wsb, wd, nr, nm) in ((dup, decay_up, 64, 'dup'), (iup, iclr_up, 64, 'iup'), (gup, gate_up, 128, 'gup')):
            k.dma('sp', stg2[0:nr, :], wd[:, :], writes=['stg2'])
            k.op('dve', lambda e, wsb=wsb, nr=nr: e.tensor_copy(out=wsb[0:nr, :], in_=stg2[0:nr, :]), reads=['stg2'], writes=[nm])
        pc = SB(es, "pc", [128, 1792], F32); pp = SB(es, "pp", [128, 1792], F32)
        lor = SB(es, "lor", [128, 256], BF16); lorT = SB(es, "lorT", [128, 3, 128], BF16)
        dec = SB(es, "dec", [128, 512], F32); aa = SB(es, "aa", [128, 512], F32); gg = SB(es, "gg", [128, 512], F32)
        kkn = SB(es, "kkn", [128, 512], F32); bb = SB(es, "bb", [128, 512], F32); km = SB(es, "km", [128, 512], F32)
        t5 = SB(es, "t5", [128, 512], F32); s8 = SB(es, "s8", [128, 8], F32); bon = SB(es, "bon", [128, 8], F32)
        WT = SB(es, "WT", [128, 4, 128], F32); V1 = SB(es, "V1", [128, 4, 129, 4], F32)
        rlast = SB(es, "rlast", [128, 4], F32)
        ST = SB(es, "ST", [128, 4, 64], F32)
        LLa = SB(es, "LLa", [128, 64, 128], F32); RRa = SB(es, "RRa", [128, 64, 64], F32)
        LLb = SB(es, "LLb", [128, 64, 128], F32); RRb = SB(es, "RRb", [128, 64, 64], F32)
        def LLp(p): return (LLa, 32 * p) if p < 3 else (LLb, 0)
        def RRp(p): return (RRa, 32 * p) if p < 3 else (RRb, 0)
        sin = SB(es, "sin", [64, 128], F32); sout = SB(es, "sout", [64, 128], F32)
        for t_ in (LLa, LLb):
            k.op('pool', lambda e, t_=t_: e.memset(t_[:].rearrange("p a b -> p (a b)"), 0.0), writes=['LL'])
        for t_ in (RRa, RRb):
            k.op('pool', lambda e, t_=t_: e.memset(t_[:].rearrange("p a b -> p (a b)"), 0.0), writes=['RR'])
        k.op('pool', lambda e: e.memset(V1[:].rearrange("p a b c -> p (a b c)"), 0.0), writes=['V1'])
        k.op('pool', lambda e: e.memset(rlast[:], 0.0), writes=['rlast'])

        def prep(row0, nrows, prev_ap, nm):
            if nrows < 128:
                k.op('pool', lambda e: e.memset(pc[:], 0.0), writes=['pc'])
                k.op('pool', lambda e: e.memset(pp[:], 0.0), writes=['pp'])
            k.dma('sp', pc[0:nrows, :], P[1 + row0:1 + row0 + nrows, :], reads=['P', 'pc'], writes=['pc'])
            k.dma('sp', pp[0:nrows, :], prev_ap, reads=['P', 'pp'], writes=['pp'])
            k.op('dve', lambda e: e.tensor_tensor(out=pp[:], in0=pp[:], in1=pc[:], op=ALU.subtract), reads=['pp', 'pc'], writes=['pp'])
            k.op('pool', lambda e: e.tensor_tensor(out=pp[:], in0=pp[:], in1=mu_r[:], op=ALU.mult), reads=['pp', 'mu_r'], writes=['pp'])
            k.op('dve', lambda e: e.tensor_tensor(out=pc[:], in0=pc[:], in1=pp[:], op=ALU.add), reads=['pp', 'pc'], writes=['pc'])
            r_ = pc[:, 0:512]; xw = pc[:, 512:576]; k_ = pc[:, 576:1088]; v_ = pc[:, 1088:1600]; xa = pc[:, 1600:1664]; xg = pc[:, 1664:1792]
            k.op('act', lambda e: e.activation(out=lor[:, 0:64], in_=xw, func=AF.Tanh), reads=['pc'], writes=['lor'])
            k.op('dve', lambda e: e.tensor_copy(out=lor[:, 64:128], in_=xa), reads=['pc'], writes=['lor'])
            k.op('act', lambda e: e.activation(out=lor[:, 128:256], in_=xg, func=AF.Sigmoid), reads=['pc'], writes=['lor'])
            k.op('pe', lambda e: e.transpose(out=pbT[0:64, 0:128], in_=lor[:, 0:64], identity=identb[:]), reads=['lor', 'identb'], writes=['pbT'])
            k.op('pe', lambda e: e.transpose(out=pbT[0:64, 128:256], in_=lor[:, 64:128], identity=identb[:]), reads=['lor', 'identb'], writes=['pbT'])
            k.op('pe', lambda e: e.transpose(out=pbT[:, 256:384], in_=lor[:, 128:256], identity=identb[:]), reads=['lor', 'identb'], writes=['pbT'])
            k.op('act', lambda e: e.activation(out=lorT[0:64, 0:2, :], in_=pbT[0:64, 0:256].rearrange("p (a b) -> p a b", a=2), func=AF.Copy), reads=['pbT'], writes=['lorT'])
            k.op('act', lambda e: e.activation(out=lorT[:, 2, :], in_=pbT[:, 256:384], func=AF.Copy), reads=['pbT'], writes=['lorT'])
            k.op('pe', lambda e: e.matmul(pb[0][:, :], lorT[0:64, 0, :], dup[:, :], start=True, stop=True), reads=['lorT', 'dup'], writes=['pb0'])
            k.op('pe', lambda e: e.matmul(pb[1][:, :], lorT[0:64, 1, :], iup[:, :], start=True, stop=True), reads=['lorT', 'iup'], writes=['pb1'])
            k.op('pe', lambda e: e.matmul(pb[2][:, :], lorT[:, 2, :], gup[:, :], start=True, stop=True), reads=['lorT', 'gup'], writes=['pb2'])
            k.op('dve', lambda e: e.tensor_tensor(out=dec[:], in0=pb[0][:, :], in1=w0_r[:], op=ALU.add), reads=['pb0', 'w0_r'], writes=['dec'])
            k.op('act', lambda e: e.activation(out=dec[:], in_=dec[:], func=AF.Sigmoid), reads=['dec'], writes=['dec'])
            k.op('act', lambda e: e.activation(out=dec[:], in_=dec[:], func=AF.Exp, scale=-EM05), reads=['dec'], writes=['dec'])
            k.op('dve', lambda e: e.tensor_tensor(out=aa[:], in0=pb[1][:, :], in1=a0_r[:], op=ALU.add), reads=['pb1', 'a0_r'], writes=['aa'])
            k.op('act', lambda e: e.activation(out=aa[:], in_=aa[:], func=AF.Sigmoid), reads=['aa'], writes=['aa'])
            k.op('act', lambda e: e.activation(out=gg[:], in_=pb[2][:, :], func=AF.Copy), reads=['pb2'], writes=['gg'])
            k.op('dve', lambda e: e.tensor_tensor(out=kkn[:], in0=k_, in1=kk_r[:], op=ALU.mult), reads=['pc', 'kk_r'], writes=['kkn'])
            k.op('pool', lambda e: e.tensor_tensor(out=t5[:], in0=kkn[:], in1=kkn[:], op=ALU.mult), reads=['kkn'], writes=['t5'])
            k.op('dve', lambda e: e.tensor_reduce(out=s8[:], in_=t5[:].rearrange("p (h d) -> p h d", h=8), axis=AX.X, op=ALU.add), reads=['t5'], writes=['s8'])
            k.op('dve', lambda e: e.tensor_scalar(out=s8[:], in0=s8[:], scalar1=1e-24, scalar2=None, op0=ALU.max), reads=['s8'], writes=['s8'])
            k.op('act', lambda e: e.activation(out=s8[:], in_=s8[:], func=AF.Sqrt), reads=['s8'], writes=['s8'])
            k.op('dve', lambda e: e.reciprocal(out=s8[:], in_=s8[:]), reads=['s8'], writes=['s8'])
            k.op('dve', lambda e: e.tensor_tensor(out=kkn[:].rearrange("p (h d) -> p h d", h=8), in0=kkn[:].rearrange("p (h d) -> p h d", h=8), in1=s8[:].unsqueeze(2).broadcast_to([128, 8, 64]), op=ALU.mult),
                 reads=['kkn', 's8'], writes=['kkn'])
            k.op('pool', lambda e: e.tensor_tensor(out=bb[:], in0=kkn[:], in1=aa[:], op=ALU.mult), reads=['kkn', 'aa'], writes=['bb'])
            k.op('dve', lambda e: e.tensor_scalar(out=kkn[:], in0=kkn[:], scalar1=-1.0, scalar2=None, op0=ALU.mult), reads=['kkn', 'bb'], writes=['kkn'])
            k.op('dve', lambda e: e.scalar_tensor_tensor(out=km[:], in0=aa[:], scalar=-1.0, in1=ka_r[:], op0=ALU.add, op1=ALU.mult), reads=['aa', 'ka_r'], writes=['km'])
            k.op('dve', lambda e: e.scalar_tensor_tensor(out=km[:], in0=km[:], scalar=1.0, in1=k_, op0=ALU.add, op1=ALU.mult), reads=['km', 'pc'], writes=['km'])
            k.op('pool', lambda e: e.tensor_tensor(out=t5[:], in0=r_, in1=km[:], op=ALU.mult), reads=['pc', 'km', 's8'], writes=['t5'])
            k.op('pool', lambda e: e.tensor_tensor(out=t5[:], in0=t5[:], in1=rk_r[:], op=ALU.mult), reads=['t5', 'rk_r'], writes=['t5'])
            k.op('dve', lambda e: e.tensor_reduce(out=bon[:], in_=t5[:].rearrange("p (h d) -> p h d", h=8), axis=AX.X, op=ALU.add), reads=['t5'], writes=['bon'])
            k.dma('sp', Bs[row0:row0 + nrows, :], bb[0:nrows, :], reads=['bb'], writes=['Bs'])
            k.dma('sp', KMs[row0:row0 + nrows, :], km[0:nrows, :], reads=['km'], writes=['KMs'])
            k.dma('sp', Vs[row0:row0 + nrows, :], pc[0:nrows, 1088:1600], reads=['pc'], writes=['Vs'])
            k.dma('sp', Gs[row0:row0 + nrows, :], gg[0:nrows, :], reads=['gg'], writes=['Gs'])
            k.dma('sp', BON[row0:row0 + nrows, :], bon[0:nrows, :], reads=['bon'], writes=['BON'])
            for p in range(4):
                for (src, sname, bank) in ((dec, 'dec', 3), (kkn, 'kkn', 4), (pc, 'pc', 5)):
                    k.op('pe', lambda e, src=src, p=p, bank=bank: e.matmul(pb[bank][:, p * 128:(p + 1) * 128], src[:, p * 128:(p + 1) * 128], identf[:, :], start=True, stop=True),
                         reads=[sname, 'identf'], writes=['pb%d' % bank])
            k.op('act', lambda e: e.activation(out=WT[:].rearrange("p a b -> p (a b)"), in_=pb[3][:, :], func=AF.Copy), reads=['pb3'], writes=['WT'])
            k.op('dve', lambda e: e.tensor_copy(out=V1[0:64, :, 0, 2], in_=rlast[0:64, :]), reads=['rlast', 'V1'], writes=['V1'])
            k.op('dve', lambda e: e.tensor_copy(out=V1[64:128, :, 0, 3], in_=rlast[64:128, :]), reads=['rlast', 'V1'], writes=['V1'])
            k.op('dve', lambda e: e.tensor_copy(out=V1[0:64, :, 0:128, 0], in_=pb[4][0:64, :].rearrange("p (a b) -> p a b", a=4)), reads=['pb4', 'V1'], writes=['V1'])
            k.op('dve', lambda e: e.tensor_copy(out=V1[64:128, :, 0:128, 1], in_=pb[4][64:128, :].rearrange("p (a b) -> p a b", a=4)), reads=['pb4', 'V1'], writes=['V1'])
            k.op('act', lambda e: e.activation(out=V1[0:64, :, 1:129, 2], in_=pb[5][0:64, :].rearrange("p (a b) -> p a b", a=4), func=AF.Copy), reads=['pb5', 'V1'], writes=['V1'])
            k.op('act', lambda e: e.activation(out=V1[64:128, :, 1:129, 3], in_=pb[5][64:128, :].rearrange("p (a b) -> p a b", a=4), func=AF.Copy), reads=['pb5', 'V1'], writes=['V1'])

        def load_rows(row0, nst):
            for p in range(4):
                LL, lb = LLp(p); RR, rb = RRp(p)
                for hh in range(2):
                    c0 = (2 * p + hh) * 64
                    k.dma('sp', LL[lb + hh:lb + hh + 1, 0:nst, hh * 64:(hh + 1) * 64], Bs[row0:row0 + nst, c0:c0 + 64].unsqueeze(0), reads=['Bs', 'LL'], writes=['LL'])
                    k.dma('sp', LL[lb + 4 + hh:lb + 5 + hh, 0:nst, hh * 64:(hh + 1) * 64], KMs[row0:row0 + nst, c0:c0 + 64].unsqueeze(0), reads=['KMs', 'LL'], writes=['LL'])
                    k.dma('sp', RR[rb + 4 + hh:rb + 5 + hh, 0:nst, :], Vs[row0:row0 + nst, c0:c0 + 64].unsqueeze(0), reads=['Vs', 'RR'], writes=['RR'])

        def steps(nst, slot0=0, dummy_last=False):
            banks = ['pb%d' % (3 + p) for p in range(4)]
            for t in range(nst):
                for p in range(4):
                    k.op('pe', lambda e, p=p, t=t: e.matmul(pb[3 + p][0:4, 0:64], V1[:, p, slot0 + t, :], ST[:, p, :], start=True, stop=True),
                         reads=['V1', ('ST', p)], writes=['pb%d' % (3 + p)])
                for p in range(4):
                    RR, rb = RRp(p)
                    k.op('act', lambda e, p=p, t=t, RR=RR, rb=rb: e.activation(out=RR[rb:rb + 4, t, :], in_=pb[3 + p][0:4, 0:64], func=AF.Copy),
                         reads=['pb%d' % (3 + p), 'RR'], writes=[('RRs', p)])
                if dummy_last and t == nst - 1:
                    continue
                for p in range(4):
                    LL, lb = LLp(p); RR, rb = RRp(p)
                    k.op('pe', lambda e, p=p, t=t, LL=LL, lb=lb, RR=RR, rb=rb: e.matmul(pb[3 + p][:, 64:128], LL[lb:lb + 6, t, :], RR[rb:rb + 6, t, :], start=True, stop=True),
                         reads=['LL', 'RR', ('RRs', p)], writes=['pb%d' % (3 + p)])
                for p in range(4):
                    k.op('dve', lambda e, p=p, t=t: e.scalar_tensor_tensor(out=ST[:, p, :], in0=ST[:, p, :], scalar=WT[:, p, slot0 + t:slot0 + t + 1], in1=pb[3 + p][:, 64:128], op0=ALU.mult, op1=ALU.add),
                         reads=['pb%d' % (3 + p), 'WT', ('ST', p)], writes=[('ST', p)])

        def store_y(dst_rows, nst):
            for p in range(4):
                RR, rb = RRp(p)
                for hh in range(2):
                    c0 = (2 * p + hh) * 64
                    k.dma('sp', dst_rows[:, c0:c0 + 64].unsqueeze(0), RR[rb + 2 + hh:rb + 3 + hh, 0:nst, :], reads=[('RRs', q) for q in range(4)] + ['RR'], writes=['Ysc'])

        def sync_rr():
            pass

        def store_state(dst):
            for p in range(4):
                k.op('pe', lambda e, p=p: e.matmul(pb[2][0:64, 0:128], ST[:, p, :], identf[:, :], start=True, stop=True), reads=[('ST', p), 'identf'], writes=['pb2'])
                k.op('act', lambda e: e.activation(out=sout[:], in_=pb[2][0:64, 0:128], func=AF.Copy), reads=['pb2'], writes=['sout'])
                for hh in range(2):
                    k.dma('sp', dst[2 * p + hh, :, :], sout[:, hh * 64:(hh + 1) * 64], reads=['sout'], writes=['wkvout'])

        for p in range(4):
            k.op('pool', lambda e, p=p: e.memset(ST[:, p, :], 0.0), writes=[('ST', p)])
        for i in PT:
            prep(i * 128, 128, P[i * 128:(i + 1) * 128, :], 'p')
            k.op('dve', lambda e: e.tensor_copy(out=rlast[0:64, :], in_=V1[0:64, :, 128, 2]), reads=['V1'], writes=['rlast'])
            k.op('dve', lambda e: e.tensor_copy(out=rlast[64:128, :], in_=V1[64:128, :, 128, 3]), reads=['V1'], writes=['rlast'])
            for half in range(2):
                load_rows(i * 128 + half * 64, 64)
                steps(64, slot0=half * 64)
                store_y(Ysc[i * 128 + half * 64:i * 128 + (half + 1) * 64, :], 64)
        k.op('dve', lambda e: e.tensor_copy(out=V1[0:64, :, 0, 2], in_=rlast[0:64, :]), reads=['rlast', 'V1'], writes=['V1'])
        k.op('dve', lambda e: e.tensor_copy(out=V1[64:128, :, 0, 3], in_=rlast[64:128, :]), reads=['rlast', 'V1'], writes=['V1'])
        k.op('pool', lambda e: e.memset(V1[:, :, 0, 0:2], 0.0), reads=['V1'], writes=['V1'])
        steps(1, slot0=0, dummy_last=True)
        store_y(Ysc[T:T + 1, :], 1)
        store_state(wkvp)

        prep(T, NS, sshift[:, :], 's')
        for s in range(NS):
            for p in range(4):
                for hh in range(2):
                    k.dma('sp', sin[:, hh * 64:(hh + 1) * 64], swkv[s, 2 * p + hh, :, :], reads=['sin'], writes=['sin'])
                k.op('pe', lambda e: e.matmul(pb[2][:, 0:64], sin[:, :], identf[0:64, 0:64], start=True, stop=True), reads=['sin', 'identf'], writes=['pb2'])
                k.op('act', lambda e, p=p: e.activation(out=ST[:, p, :], in_=pb[2][:, 0:64], func=AF.Copy), reads=['pb2'], writes=[('ST', p)])
            load_rows(T + s, 1)
            for p in range(4):
                LL, lb = LLp(p); RR, rb = RRp(p)
                k.op('pe', lambda e, p=p, s=s: e.matmul(pb[3 + p][0:4, 0:64], V1[:, p, s, :], ST[:, p, :], start=True, stop=True), reads=['V1', ('ST', p)], writes=['pb%d' % (3 + p)])
                k.op('act', lambda e, p=p, RR=RR, rb=rb: e.activation(out=RR[rb:rb + 4, 0, :], in_=pb[3 + p][0:4, 0:64], func=AF.Copy), reads=['pb%d' % (3 + p), 'RR'], writes=[('RRs', p)])
                k.op('pe', lambda e, p=p, LL=LL, lb=lb, RR=RR, rb=rb: e.matmul(pb[3 + p][:, 64:128], LL[lb:lb + 6, 0, :], RR[rb:rb + 6, 0, :], start=True, stop=True), reads=['LL', 'RR', ('RRs', p)], writes=['pb%d' % (3 + p)])
                k.op('dve', lambda e, p=p, s=s: e.scalar_tensor_tensor(out=ST[:, p, :], in0=ST[:, p, :], scalar=WT[:, p, s:s + 1], in1=pb[3 + p][:, 64:128], op0=ALU.mult, op1=ALU.add),
                     reads=['pb%d' % (3 + p), 'WT', ('ST', p)], writes=[('ST', p)])
                k.op('pe', lambda e, p=p, s=s: e.matmul(pb[3 + p][0:4, 0:64], V1[:, p, s + 1, :], ST[:, p, :], start=True, stop=True), reads=['V1', ('ST', p)], writes=['pb%d' % (3 + p)])
                k.op('act', lambda e, p=p, RR=RR, rb=rb: e.activation(out=RR[rb:rb + 4, 1, :], in_=pb[3 + p][0:4, 0:64], func=AF.Copy), reads=['pb%d' % (3 + p), 'RR'], writes=[('RRs', p)])
            for p in range(4):
                RR, rb = RRp(p)
                for hh in range(2):
                    c0 = (2 * p + hh) * 64
                    k.dma('sp', Yss[s:s + 1, 1, c0:c0 + 64], RR[rb + 2 + hh:rb + 3 + hh, 1, :], reads=[('RRs', q) for q in range(4)] + ['RR'], writes=['Yss'])
            store_state(wkvs[s])

        yv = SB(es, "yv", [128, 512], F32); vv = SB(es, "vv", [128, 512], F32); gv = SB(es, "gv", [128, 512], F32)
        bv = SB(es, "bv", [128, 8], F32); m8 = SB(es, "m8", [128, 8], F32); rwb = SB(es, "rwb", [128, 512], BF16)
        rwT = SB(es, "rwT", [128, 4, 128], BF16)
        for i in PT + [32]:
            samp = (i == 32)
            nr = NS if samp else 128
            if samp:
                for (t_, n_) in ((yv, 'yv'), (vv, 'vv'), (gv, 'gv')):
                    k.op('pool', lambda e, t_=t_: e.memset(t_[:], 0.0), writes=[n_])
                k.op('pool', lambda e: e.memset(bv[:], 0.0), writes=['bv'])
                k.dma('sp', yv[0:NS, :], Yss[:, 1, :], reads=['Yss', 'yv'], writes=['yv'])
            else:
                k.dma('sp', yv[:, :], Ysc[i * 128 + 1:(i + 1) * 128 + 1, :], reads=['Ysc'], writes=['yv'])
            r0 = T if samp else i * 128
            k.dma('sp', vv[0:nr, :], Vs[r0:r0 + nr, :], reads=['Vs', 'vv'], writes=['vv'])
            k.dma('sp', gv[0:nr, :], Gs[r0:r0 + nr, :], reads=['Gs', 'gv'], writes=['gv'])
            k.dma('sp', bv[0:nr, :], BON[r0:r0 + nr, :], reads=['BON', 'bv'], writes=['bv'])
            y3 = yv[:].rearrange("p (h d) -> p h d", h=8)
            k.op('dve', lambda e: e.tensor_reduce(out=m8[:], in_=y3, axis=AX.X, op=ALU.add), reads=['yv'], writes=['m8'])
            k.op('dve', lambda e: e.tensor_scalar(out=m8[:], in0=m8[:], scalar1=1.0 / 64, scalar2=None, op0=ALU.mult), reads=['m8'], writes=['m8'])
            k.op('dve', lambda e: e.tensor_tensor(out=y3, in0=y3, in1=m8[:].unsqueeze(2).broadcast_to([128, 8, 64]), op=ALU.subtract), reads=['yv', 'm8'], writes=['yv'])
            k.op('pool', lambda e: e.tensor_tensor(out=t5[:], in0=yv[:], in1=yv[:], op=ALU.mult), reads=['yv'], writes=['t5'])
            k.op('dve', lambda e: e.tensor_reduce(out=m8[:], in_=t5[:].rearrange("p (h d) -> p h d", h=8), axis=AX.X, op=ALU.add), reads=['t5', 'yv'], writes=['m8'])
            k.op('act', lambda e: e.activation(out=m8[:], in_=m8[:], func=AF.Sqrt, scale=1.0 / 64, bias=GN_EPS), reads=['m8'], writes=['m8'])
            k.op('dve', lambda e: e.reciprocal(out=m8[:], in_=m8[:]), reads=['m8'], writes=['m8'])
            k.op('dve', lambda e: e.tensor_tensor(out=y3, in0=y3, in1=m8[:].unsqueeze(2).broadcast_to([128, 8, 64]), op=ALU.mult), reads=['yv', 'm8'], writes=['yv'])
            k.op('pool', lambda e: e.tensor_tensor(out=yv[:], in0=yv[:], in1=lnw_r[:], op=ALU.mult), reads=['yv', 'lnw_r'], writes=['yv'])
            k.op('pool', lambda e: e.tensor_tensor(out=yv[:], in0=yv[:], in1=lnb_r[:], op=ALU.add), reads=['yv', 'lnb_r'], writes=['yv'])
            v3 = vv[:].rearrange("p (h d) -> p h d", h=8)
            k.op('dve', lambda e: e.tensor_tensor(out=v3, in0=v3, in1=bv[:].unsqueeze(2).broadcast_to([128, 8, 64]), op=ALU.mult), reads=['vv', 'bv'], writes=['vv'])
            k.op('dve', lambda e: e.tensor_tensor(out=yv[:], in0=yv[:], in1=vv[:], op=ALU.add), reads=['yv', 'vv'], writes=['yv'])
            k.op('dve', lambda e: e.tensor_tensor(out=rwb[:], in0=yv[:], in1=gv[:], op=ALU.mult), reads=['yv', 'gv'], writes=['rwb'])
            for c in range(4):
                k.op('pe', lambda e, c=c: e.transpose(out=pbT[:, c * 128:(c + 1) * 128], in_=rwb[:, c * 128:(c + 1) * 128], identity=identb[:]), reads=['rwb', 'identb'], writes=['pbT'])
            k.op('act', lambda e: e.activation(out=rwT[:].rearrange("p a b -> p (a b)"), in_=pbT[:, 0:512], func=AF.Copy), reads=['pbT'], writes=['rwT'])
            for c in range(4):
                k.dma('sp', mixT[512 + c * 128:512 + (c + 1) * 128, r0:r0 + 128], rwT[:, c, :], reads=['rwT'], writes=['mixT'])

    k.barrier()
    with contextlib.ExitStack() as es:
      if KSTOP >= 4:
        wo = SB(es, "wo", [128, 8, D], BF16); wu = SB(es, "wu", [128, 8, 4096], BF16); wd = SB(es, "wd", [128, 32, D], BF16)
        stg3 = SB(es, "stg3", [128, 3328], F32)
        gf_r = SB(es, "gf_r", [128, D], F32)
        k.dma('sp', gf_r[:], gf.partition_broadcast(128), writes=['gf_r'])
        load_weight(wo, w_out, 8, D, None, stg3, 'wo')
        load_weight(wu, w_up, 8, 4096, g2, stg3, 'wu')
        load_weight(wd, w_dn, 32, D, None, stg3, 'wd')
        mxT = SB(es, "mxT", [128, 8, 128], BF16)
        xt3 = SB(es, "xt3", [128, D], F32); hh_ = SB(es, "hh_", [128, D], F32)
        ss3 = SB(es, "ss3", [128, 1], F32); rs3 = SB(es, "rs3", [128, 1], F32)
        hnb = SB(es, "hnb", [128, D], BF16); hnT = SB(es, "hnT", [128, 8, 128], BF16)
        ur = SB(es, "ur", [128, 512], F32); uT = SB(es, "uT", [128, 32, 128], BF16)
        yo = SB(es, "yo", [128, D], F32)
        for i in PT + [32]:
            samp = (i == 32)
            r0 = T if samp else i * 128
            k.dma('sp', mxT[:, :, :], mixT[:, r0:r0 + 128].rearrange("(a p) t -> p a t", p=128), reads=['mixT'], writes=['mxT'])
            if samp:
                k.op('pool', lambda e: e.memset(xt3[:], 0.0), writes=['xt3'])
                k.dma('sp', xt3[0:NS, :], xsm[:, :], reads=['xt3'], writes=['xt3'])
            else:
                k.dma('sp', xt3[:], xp[i * 128:(i + 1) * 128, :], writes=['xt3'])
            for c in range(2):
                for kc in range(8):
                    k.op('pe', lambda e, c=c, kc=kc: e.matmul(pb[c][:, :], mxT[:, kc, :], wo[:, kc, c * 512:(c + 1) * 512], start=(kc == 0), stop=(kc == 7)), reads=['mxT', 'wo'], writes=['pb%d' % c])
                k.op('dve', lambda e, c=c: e.tensor_tensor(out=hh_[:, c * 512:(c + 1) * 512], in0=pb[c][:, :], in1=xt3[:, c * 512:(c + 1) * 512], op=ALU.add), reads=['pb%d' % c, 'xt3'], writes=['hh_'])
            rmsnorm_T((ss3, rs3, hnb, hnT), hh_, 'hh_', '3')
            for fg in range(8):
                bank = pb[2 + fg % 2]; bn = 'pb%d' % (2 + fg % 2)
                for f4 in range(4):
                    fc = fg * 4 + f4
                    for kc in range(8):
                        k.op('pe', lambda e, fc=fc, f4=f4, kc=kc, bank=bank: e.matmul(bank[:, f4 * 128:(f4 + 1) * 128], wu[:, kc, fc * 128:(fc + 1) * 128], hnT[:, kc, :], start=(kc == 0), stop=(kc == 7)),
                             reads=['xnT3', 'wu'], writes=[bn])
                k.op('act', lambda e, bank=bank: e.activation(out=ur[:], in_=bank[:, :], func=AF.Relu), reads=[bn], writes=['ur'])
                k.op('dve', lambda e, fg=fg: e.tensor_tensor(out=uT[:, fg * 4:(fg + 1) * 4, :].rearrange("p a b -> p (a b)"), in0=ur[:], in1=ur[:], op=ALU.mult), reads=['ur'], writes=['uT'])
            for c in range(2):
                bank = pb[4 + c]; bn = 'pb%d' % (4 + c)
                for fc in range(32):
                    k.op('pe', lambda e, c=c, fc=fc, bank=bank: e.matmul(bank[:, :], uT[:, fc, :], wd[:, fc, c * 512:(c + 1) * 512], start=(fc == 0), stop=(fc == 31)), reads=['uT', 'wd'], writes=[bn])
                k.op('dve', lambda e, c=c, bank=bank: e.tensor_tensor(out=hh_[:, c * 512:(c + 1) * 512], in0=bank[:, :], in1=hh_[:, c * 512:(c + 1) * 512], op=ALU.add), reads=[bn, 'hh_'], writes=['hh_'])
            k.op('act', lambda e: e.activation(out=hnb[:], in_=hh_[:], func=AF.Square, accum_out=ss3[:, 0:1]), reads=['hh_'], writes=['xnb3', 'ss3'])
            k.op('act', lambda e: e.activation(out=rs3[:], in_=ss3[:], func=AF.Sqrt, scale=1.0 / D, bias=RMS_EPS), reads=['ss3'], writes=['rs3'])
            k.op('dve', lambda e: e.reciprocal(out=rs3[:], in_=rs3[:]), reads=['rs3'], writes=['rs3'])
            k.op('dve', lambda e: e.scalar_tensor_tensor(out=yo[:], in0=hh_[:], scalar=rs3[:, 0:1], in1=gf_r[:], op0=ALU.mult, op1=ALU.mult), reads=['hh_', 'rs3', 'gf_r'], writes=['yo'])
            if samp:
                k.dma('sp', ysm[:, :], yo[0:NS, :], reads=['yo'], writes=['ysm'])
            else:
                k.dma('sp', yp[i * 128:(i + 1) * 128, :], yo[:], reads=['yo'], writes=['yp'])

    k.wait_all('sp')
    top.close()
    return nc, consts, k


_CACHE = {}


def kernel(**inp):
    f = lambda a: np.ascontiguousarray(np.asarray(a))
    n_pool = inp['cache_k'].shape[1]
    if n_pool not in _CACHE:
        _CACHE[n_pool] = build(n_pool)
    nc, consts, _k = _CACHE[n_pool]
    ck = f(inp['cache_k'][0]).reshape(n_pool * 128, 512)
    cv = f(inp['cache_v'][0]).reshape(n_pool * 128, 512)
    shared = {
        'ck': ck, 'cv': cv,
        'w_in': f(inp['w_in'][0]), 'w_out': f(inp['w_out'][0]), 'w_up': f(inp['w_ffn_up'][0]), 'w_dn': f(inp['w_ffn_down'][0]),
        'g1': f(inp['norm_mix_g'][0]), 'g2': f(inp['norm_ffn_g'][0]), 'gf': f(inp['norm_final_g']),
        'mu': f(inp['mu_shift'][0]), 'w0': f(inp['decay_w0'][0]), 'decay_up': f(inp['decay_up'][0]),
        'a0': f(inp['iclr_a0'][0]), 'iclr_up': f(inp['iclr_up'][0]), 'gate_up': f(inp['gate_up'][0]),
        'k_k': f(inp['k_k'][0]), 'k_a': f(inp['k_a'][0]), 'r_k': f(inp['r_k'][0]).reshape(512),
        'lnw': f(inp['ln_x_w'][0]), 'lnb': f(inp['ln_x_b'][0]),
    }
    for n, a in consts.items():
        shared['c_' + n] = a
    in_maps = []
    for c in range(8):
        m = dict(shared)
        m['xp'] = f(inp['x_prompt'][c % 4])
        m['xsm'] = f(inp['x_sample'][4 * c:4 * c + 4, 0])
        m['pt'] = f(inp['page_table'][4 * c:4 * c + 4]).astype(np.int32)
        m['swkv'] = f(inp['state_wkv'][0, 4 * c:4 * c + 4])
        m['sshift'] = f(inp['state_shift'][0, 4 * c:4 * c + 4])
        in_maps.append(m)
    res = run_bass_kernel_spmd(nc, in_maps, core_ids=list(range(8)))
    R = res.results
    y_prompt = np.stack([R[b]['yp'] for b in range(4)])
    y_sample = np.concatenate([R[c]['ysm'] for c in range(8)])[:, None, :]
    k_prompt = np.stack([R[b]['kp'] for b in range(4)]).reshape(1, 4, T, 8, 64)
    v_prompt = np.stack([R[b]['vp'] for b in range(4)]).reshape(1, 4, T, 8, 64)
    wkv_prompt = np.stack([R[b]['wkvp'] for b in range(4)])[None]
    shift_prompt = np.stack([R[b]['shp'][0] for b in range(4)])[None]
    k_sample = np.concatenate([R[c]['ks'] for c in range(8)]).reshape(1, 32, 1, 8, 64)
    v_sample = np.concatenate([R[c]['vs'] for c in range(8)]).reshape(1, 32, 1, 8, 64)
    wkv_sample = np.concatenate([R[c]['wkvs'] for c in range(8)])[None]
    shift_sample = np.concatenate([R[c]['shs'] for c in range(8)])[None]
    return tuple(np.ascontiguousarray(a, dtype=np.float32) for a in
                 (y_prompt, y_sample, k_prompt, v_prompt, wkv_prompt, shift_prompt, k_sample, v_sample, wkv_sample, shift_sample))
```

```python
import contextlib
import os
import numpy as np
import ml_dtypes
import concourse.bass as bass
import concourse.mybir as mybir
from concourse.bass_utils import run_bass_kernel_spmd

F32 = mybir.dt.float32; BF16 = mybir.dt.bfloat16; I32 = mybir.dt.int32
AF = mybir.ActivationFunctionType; ALU = mybir.AluOpType; AX = mybir.AxisListType
T = 4096; D = 1024; NS = 4; NPG = 64
RMS_EPS = 1e-6; GN_EPS = 64e-5
NBIG = 30000.0


class K:
    def __init__(s, nc, same=True):
        s.nc = nc
        s.eng = {'pe': nc.tensor, 'act': nc.scalar, 'dve': nc.vector, 'pool': nc.gpsimd, 'sp': nc.sync}
        s.csem = {}; s.cnt = {}; s.nsem = 0
        for n in s.eng:
            s._newsem(n)
        s.lastw = {}; s.reads = {}
        s.waited = {n: {} for n in s.eng}
        s.same = same
        s.embed = (os.environ.get('KEMBED', '1') == '1')
        s.dpool = {}; s.dpos = {}
        s.ninst = 0

    def _newsem(s, n):
        s.nsem += 1
        s.csem[n] = s.nc.alloc_semaphore('c%d_%s' % (s.nsem, n)); s.cnt[n] = 0

    def _need(s, e, reads, writes, extra=(), defer_last=False):
        evs = list(extra)
        for r in reads:
            if r in s.lastw: evs.append(s.lastw[r])
        for w in writes:
            if w in s.lastw: evs.append(s.lastw[w])
            evs.extend(s.reads.get(w, ()))
        best = {}
        for (h, v, en) in evs:
            if en == e and (not s.same or e in ('pe', 'sp')): continue
            k = id(h)
            if k not in best or best[k][1] < v: best[k] = (h, v)
        todo = []
        for k, (h, v) in best.items():
            if s.waited[e].get(k, 0) >= v: continue
            todo.append((h, v)); s.waited[e][k] = v
        last = None
        if defer_last and todo:
            last = todo.pop()
        for (h, v) in todo:
            s.eng[e].wait_ge(h, v); s.ninst += 1
        return last

    def _record(s, ev, reads, writes):
        for w in writes:
            s.lastw[w] = ev; s.reads[w] = []
        for r in reads:
            lst = s.reads.setdefault(r, [])
            lst.append(ev)
            if len(lst) > 16:
                m = {}
                for (h, v, en) in lst:
                    if id(h) not in m or m[id(h)][1] < v: m[id(h)] = (h, v, en)
                s.reads[r] = list(m.values())

    def op(s, e, fn, reads=(), writes=()):
        pr = [r for r in reads if isinstance(r, str) and r.startswith('pb')]
        if pr:
            reads = [r for r in reads if r not in pr]; writes = list(writes) + pr
        last = s._need(e, reads, writes, defer_last=s.embed)
        if s.cnt[e] >= 30000: s._newsem(e)
        ins = fn(s.eng[e])
        if last is not None:
            ins._wait_ge(last[0], last[1])
        s.cnt[e] += 1; s.ninst += 1
        ins.then_inc(s.csem[e], 1)
        ev = (s.csem[e], s.cnt[e], e)
        s._record(ev, reads, writes)
        return ev

    def dma(s, q, out=None, in_=None, reads=(), writes=(), fn=None):
        if q not in s.dpool:
            s.dpool[q] = [[s.nc.alloc_semaphore('d_%s%d' % (q, i)), 0] for i in range(16)]; s.dpos[q] = 0
        slot = s.dpool[q][s.dpos[q] % 16]; s.dpos[q] += 1
        extra = [(slot[0], slot[1], None)] if slot[1] > 0 else []
        s._need(q, reads, writes, extra)
        if fn is None:
            ins = s.eng[q].dma_start(out=out, in_=in_)
        else:
            ins = fn(s.eng[q])
        slot[1] += 16; s.ninst += 1
        ins.then_inc(slot[0], 16)
        ev = (slot[0], slot[1], None)
        s._record(ev, reads, writes)
        return ev

    def barrier(s):
        evs = [(s.csem[n], s.cnt[n], n) for n in s.eng if s.cnt[n] > 0]
        for q in s.dpool:
            for slot in s.dpool[q]:
                if slot[1] > 0: evs.append((slot[0], slot[1], None))
        for e in s.eng:
            best = {}
            for (h, v, en) in evs:
                if en == e: continue
                best[id(h)] = (h, v)
            for kk_, (h, v) in best.items():
                if s.waited[e].get(kk_, 0) >= v: continue
                s.eng[e].wait_ge(h, v); s.waited[e][kk_] = v; s.ninst += 1

    def wait_all(s, e):
        evs = list(s.lastw.values())
        for q in s.dpool:
            for slot in s.dpool[q]:
                if slot[1] > 0: evs.append((slot[0], slot[1], None))
        s._need(e, (), (), evs)


def make_consts():
    bf = ml_dtypes.bfloat16
    c = {}
    c['identf'] = np.eye(128, dtype=np.float32)
    c['identb'] = np.eye(128, dtype=np.float32).astype(bf)
    p = np.arange(128)[:, None]; dl = np.arange(512)[None, :]
    cm = np.zeros((128, 4, 512), np.float32)
    for j in range(4):
        cm[:, j, :] = np.where(dl >= 128 * j + p, 0.0, -NBIG)
    c['cmask'] = cm.astype(bf)
    s = np.arange(T)
    ka = np.zeros((18, T), np.float32)
    for n in range(16):
        ka[n] = (s // 256 == n)
    ka[16] = 1; ka[17] = 1
    c['kaug'] = ka.astype(bf)
    slopes = 2.0 ** (-np.arange(1, 9, dtype=np.float64))
    qa = np.zeros((2, 8, 512), np.float32)
    d = np.arange(512)
    for h in range(8):
        qa[0, h] = -slopes[h] * (d % 256)
        qa[1, h] = -slopes[h] * 256 * (d // 256)
    c['qaug'] = qa.astype(bf)
    ab = np.zeros((128, 8, 36), np.float32)
    for h in range(8):
        for r in range(36):
            ab[:, h, r] = slopes[h] * (128 * (r - 28) + np.arange(128))
    c['ab'] = ab
    gm = np.zeros((17, 16), np.float32); oh = np.zeros((17, 16), np.float32)
    for nq in range(17):
        gm[nq, nq:] = -1e30
        if nq < 16: oh[nq, nq] = 1
    c['gmask'] = np.broadcast_to(gm[None], (128, 17, 16)).copy()
    c['ownhot'] = np.broadcast_to(oh[None], (128, 17, 16)).copy()
    al = np.zeros((128, 65, 8), np.float32)
    for h in range(8):
        for pg in range(64):
            al[:, pg, h] = -slopes[h] * (8192 - (128 * pg + np.arange(128)))
        al[:, 64, h] = -NBIG; al[0, 64, h] = 0.0
    c['alis'] = al
    dm = np.zeros((8, 512), np.float32)
    for h in range(8): dm[h, h * 64:(h + 1) * 64] = 1
    c['diagm'] = dm
    s4 = np.zeros((4, 4, 128), np.float32)
    for i in range(4): s4[i, i, :] = 1
    c['sel4'] = s4
    o4 = np.zeros((128, 4, 4), np.float32)
    for i in range(4): o4[:, i, i] = 1
    c['oh4'] = o4
    s8 = np.zeros((8, 4, 4), np.float32)
    for i in range(4): s8[:, i, i] = 1
    c['sel8'] = s8
    c['iotaf'] = np.arange(128, dtype=np.float32)[:, None].copy()
    return c


def build(n_pool):
    import os
    KSTOP = int(os.environ.get('KSTOP', '9')); NT = int(os.environ.get('KNT', '32'))
    PT = list(range(NT))
    nc = bass.Bass("TRN2", target_bir_lowering=False)
    consts = make_consts()
    dt_of = lambda a: BF16 if a.dtype == ml_dtypes.bfloat16 else (I32 if a.dtype == np.int32 else F32)

    def din(name, shape, dt=F32):
        return nc.dram_tensor(name, list(shape), dt, kind="ExternalInput").ap()

    def dout(name, shape, dt=F32):
        return nc.dram_tensor(name, list(shape), dt, kind="ExternalOutput").ap()

    def dscr(name, shape, dt=F32):
        return nc.dram_tensor(name, list(shape), dt).ap()

    xp = din("xp", [T, D]); xsm = din("xsm", [NS, D])
    ck = din("ck", [n_pool * 128, 512]); cv = din("cv", [n_pool * 128, 512])
    pt = din("pt", [NS, NPG], I32)
    swkv = din("swkv", [NS, 8, 64, 64]); sshift = din("sshift", [NS, 1792])
    w_in = din("w_in", [D, 3328]); w_out = din("w_out", [D, D]); w_up = din("w_up", [D, 4096]); w_dn = din("w_dn", [4096, D])
    g1 = din("g1", [D]); g2 = din("g2", [D]); gf = din("gf", [D])
    mu = din("mu", [1792]); w0 = din("w0", [512]); decay_up = din("decay_up", [64, 512])
    a0 = din("a0", [512]); iclr_up = din("iclr_up", [64, 512]); gate_up = din("gate_up", [128, 512])
    k_k = din("k_k", [512]); k_a = din("k_a", [512]); r_k = din("r_k", [512]); lnw = din("lnw", [512]); lnb = din("lnb", [512])
    cin = {n: din("c_" + n, a.shape, dt_of(a)) for n, a in consts.items()}

    yp = dout("yp", [T, D]); ysm = dout("ysm", [NS, D])
    kp = dout("kp", [T, 512]); vp = dout("vp", [T, 512])
    wkvp = dout("wkvp", [8, 64, 64]); shp = dout("shp", [1, 1792])
    ks = dout("ks", [NS, 512]); vs = dout("vs", [NS, 512])
    wkvs = dout("wkvs", [NS, 8, 64, 64]); shs = dout("shs", [NS, 1792])

    TT = T + 128
    P = dscr("P", [TT + 1, 1792])
    Bs = dscr("Bs", [TT, 512]); KMs = dscr("KMs", [TT, 512]); Vs = dscr("Vs", [TT, 512]); Gs = dscr("Gs", [TT, 512])
    BON = dscr("BON", [TT, 8])
    Ysc = dscr("Ysc", [T + 2, 512])
    Yss = dscr("Yss", [NS, 2, 512])
    mixT = dscr("mixT", [D, TT], BF16)
    QKVs = dscr("QKVs", [NS, 1536])

    k = K(nc)
    top = contextlib.ExitStack()
    top.enter_context(nc.allow_non_contiguous_dma(reason='small strided parameter loads'))
    def SB(es, name, shape, dt): return es.enter_context(nc.sbuf_tensor(name, list(shape), dt))
    pb = [top.enter_context(nc.psum_tensor("pb%d" % i, [128, 512], F32)) for i in range(7)]
    pbT = top.enter_context(nc.psum_tensor("pbT", [128, 1024], BF16))
    identf = SB(top, "identf", [128, 128], F32); identb = SB(top, "identb", [128, 128], BF16)
    zero_sb = SB(top, "zero_sb", [128, 16], F32)
    ones_sb = SB(top, "ones_sb", [128, 64], F32)
    k.dma('sp', identf[:], cin['identf'][:, :], writes=['identf'])
    k.dma('sp', identb[:], cin['identb'][:, :], writes=['identb'])
    k.op('pool', lambda e: e.memset(zero_sb[:], 0.0), writes=['zero_sb'])
    k.op('pool', lambda e: e.memset(ones_sb[:], 1.0), writes=['ones_sb'])
    k.dma('sp', P[0:1, :].rearrange("o (a b) -> (o a) b", b=16), zero_sb[0:112, 0:16], reads=['zero_sb'], writes=['P'])

    rr = [0]
    def evac(out, in_, reads, writes, scale=None):
        rr[0] += 1
        if rr[0] % 2 == 0 and scale is None:
            return k.op('dve', lambda e: e.tensor_copy(out=out, in_=in_), reads=reads, writes=writes)
        if scale is None:
            return k.op('act', lambda e: e.activation(out=out, in_=in_, func=AF.Copy), reads=reads, writes=writes)
        return k.op('act', lambda e: e.activation(out=out, in_=in_, func=AF.Copy, scale=scale), reads=reads, writes=writes)

    def rmsnorm_T(es_tensors, src_tile, srcname, nm):
        ss, rs, xnb, xnT = es_tensors
        k.op('act', lambda e: e.activation(out=xnb[:], in_=src_tile[:], func=AF.Square, accum_out=ss[:, 0:1]), reads=[srcname], writes=['xnb' + nm, 'ss' + nm])
        k.op('act', lambda e: e.activation(out=rs[:], in_=ss[:], func=AF.Sqrt, scale=1.0 / D, bias=RMS_EPS), reads=['ss' + nm], writes=['rs' + nm])
        k.op('dve', lambda e: e.reciprocal(out=rs[:], in_=rs[:]), reads=['rs' + nm], writes=['rs' + nm])
        k.op('dve', lambda e: e.tensor_scalar(out=xnb[:], in0=src_tile[:], scalar1=rs[:, 0:1], scalar2=None, op0=ALU.mult), reads=[srcname, 'rs' + nm], writes=['xnb' + nm])
        for kc in range(8):
            k.op('pe', lambda e, kc=kc: e.transpose(out=pbT[:, kc * 128:(kc + 1) * 128], in_=xnb[:, kc * 128:(kc + 1) * 128], identity=identb[:]),
                 reads=['xnb' + nm, 'identb'], writes=['pbT'])
        k.op('act', lambda e: e.activation(out=xnT[:].rearrange("p a b -> p (a b)"), in_=pbT[:, :], func=AF.Copy), reads=['pbT'], writes=['xnT' + nm])

    def load_weight(wsb, wdram, nkc, ncols, gdram, stg, es_name, cs=3328, lo=None):
        if gdram is not None:
            k.dma('sp', gcol[:, 0:nkc], gdram.rearrange("(a p) -> p a", p=128), writes=['gcol'])
        for kc in range(nkc):
            for c0 in range(0, ncols, cs):
                cw = min(cs, ncols - c0)
                k.dma('sp', stg[:, 0:cw], wdram[kc * 128:(kc + 1) * 128, c0:c0 + cw], writes=['stg'])
                if gdram is not None and lo is not None and c0 == 0:
                    k.op('dve', lambda e, kc=kc, cw=cw: e.tensor_scalar(out=stg[:, 0:cw], in0=stg[:, 0:cw], scalar1=gcol[:, kc:kc + 1], scalar2=None, op0=ALU.mult), reads=['stg', 'gcol'], writes=['stg'])
                    k.op('dve', lambda e, kc=kc, cw=cw: e.tensor_copy(out=wsb[:, kc, 0:cw], in_=stg[:, 0:cw]), reads=['stg'], writes=[es_name])
                    k.op('dve', lambda e, kc=kc: e.tensor_tensor(out=lo[:, kc, :], in0=stg[:, 0:512], in1=wsb[:, kc, 0:512], op=ALU.subtract), reads=['stg', es_name], writes=['wlo'])
                elif gdram is not None:
                    k.op('dve', lambda e, kc=kc, c0=c0, cw=cw: e.tensor_scalar(out=wsb[:, kc, c0:c0 + cw], in0=stg[:, 0:cw], scalar1=gcol[:, kc:kc + 1], scalar2=None, op0=ALU.mult),
                         reads=['stg', 'gcol'], writes=[es_name])
                else:
                    k.op('dve', lambda e, kc=kc, c0=c0, cw=cw: e.tensor_copy(out=wsb[:, kc, c0:c0 + cw], in_=stg[:, 0:cw]), reads=['stg'], writes=[es_name])

    gcol = SB(top, "gcol", [128, 8], F32)

    with contextlib.ExitStack() as es:
        win = SB(es, "win", [128, 8, 3328], BF16)
        kTa = [SB(es, "kTa%d" % h, [82, T], BF16) for h in range(8)]
        vaug = SB(es, "vaug", [128, 32, 8, 65], BF16)
        qTa = SB(es, "qTa", [82, 8, 512], BF16)
        qT32 = SB(es, "qT32", [64, 8, 128], F32)
        ksum2 = SB(es, "ksum2", [64, 8, 32], F32)
        kmT = SB(es, "kmT", [64, 8, 16], F32)
        xt = SB(es, "xt", [128, D], F32)
        ss = SB(es, "ss", [128, 1], F32); rs = SB(es, "rs", [128, 1], F32)
        xnb = SB(es, "xnb", [128, D], BF16); xnT = SB(es, "xnT", [128, 8, 128], BF16)
        xlo = SB(es, "xlo", [128, D], BF16); xloT = SB(es, "xloT", [128, 8, 128], BF16)
        wlo = SB(es, "wlo", [128, 8, 512], BF16)
        proj = SB(es, "proj", [128, 1792], F32)
        cmask = SB(es, "cmask", [128, 4, 512], BF16)
        ab = SB(es, "ab", [128, 8, 36], F32)
        gmaskc = SB(es, "gmaskc", [128, 17, 16], F32); ownhot = SB(es, "ownhot", [128, 17, 16], F32)
        gm = SB(es, "gm", [128, 8, 16], F32); m01 = gm
        top8 = SB(es, "top8", [128, 8, 8], F32); thr = SB(es, "thr", [128, 8], F32)
        selpad = SB(es, "selpad", [128, 8, 80], F32)
        pT = [SB(es, "pT%d" % i, [128, 512], BF16) for i in range(2)]
        stmp = SB(es, "stmp", [128, 512], F32)
        osb = stmp; lsb = stmp
        atT = [SB(es, "atT%d" % i, [64, 512], BF16) for i in range(2)]

        k.dma('sp', cmask[:], cin['cmask'][:, :, :], writes=['cmask'])
        k.dma('sp', ab[:], cin['ab'][:, :, :], writes=['ab'])
        k.dma('sp', gmaskc[:], cin['gmask'][:, :, :], writes=['gmaskc'])
        k.dma('sp', ownhot[:], cin['ownhot'][:, :, :], writes=['ownhot'])
        for h in range(8):
            k.dma('sp', kTa[h][64:82, :], cin['kaug'][:, :], writes=[('kTa', h)])
        k.dma('sp', qTa[80:82, :, :], cin['qaug'][:, :, :], writes=['qTa'])
        k.op('pool', lambda e: e.memset(vaug[:].rearrange("p a b c -> p (a b c)"), 1.0), writes=['vaug'])
        k.op('pool', lambda e: e.memset(kmT[:].rearrange("p a b -> p (a b)"), 0.0), writes=['kmT'])
        k.op('pool', lambda e: e.memset(selpad[:].rearrange("p a b -> p (a b)"), 0.0), writes=['selpad'])
        load_weight(win, w_in, 8, 3328, g1, proj, 'win', cs=1664, lo=wlo)

        chunks = [(0, 512), (512, 512), (1024, 512), (1536, 512), (2048, 512), (2560, 512), (3072, 256)]
        for i in PT + [32]:
            samp = (i == 32)
            if samp:
                k.op('pool', lambda e: e.memset(xt[:], 0.0), writes=['xt'])
                k.dma('sp', xt[0:NS, :], xsm[:, :], reads=['xt'], writes=['xt'])
            else:
                k.dma('sp', xt[:], xp[i * 128:(i + 1) * 128, :], writes=['xt'])
            rmsnorm_T((ss, rs, xnb, xnT), xt, 'xt', '1')
            k.op('dve', lambda e: e.scalar_tensor_tensor(out=xlo[:], in0=xt[:], scalar=rs[:, 0:1], in1=xnb[:], op0=ALU.mult, op1=ALU.subtract), reads=['xt', 'rs1', 'xnb1'], writes=['xlo'])
            for kc in range(8):
                k.op('pe', lambda e, kc=kc: e.transpose(out=pbT[:, kc * 128:(kc + 1) * 128], in_=xlo[:, kc * 128:(kc + 1) * 128], identity=identb[:]), reads=['xlo', 'identb'], writes=['pbT'])
            k.op('act', lambda e: e.activation(out=xloT[:].rearrange("p a b -> p (a b)"), in_=pbT[:, :], func=AF.Copy), reads=['pbT'], writes=['xloT'])
            def pcol(ci):
                return (chunks[ci][0] if ci < 3 else chunks[ci][0] - 1536), (('proj', ci % 3) if ci < 6 else ('proj', 3))
            for ci, (c0, cw) in enumerate(chunks):
                bank = pb[ci % 2]; bn = 'pb%d' % (ci % 2)
                if ci == 0 and not samp:
                    continue
                for kc in range(8):
                    k.op('pe', lambda e, kc=kc, c0=c0, cw=cw, bank=bank: e.matmul(bank[:, 0:cw], xnT[:, kc, :], win[:, kc, c0:c0 + cw], start=(kc == 0), stop=(kc == 7 and ci != 0)),
                         reads=['xnT1', 'win'], writes=[bn])
                if ci == 0:
                    for kc in range(8):
                        k.op('pe', lambda e, kc=kc, bank=bank: e.matmul(bank[:, 0:512], xnT[:, kc, :], wlo[:, kc, :], start=False, stop=False), reads=['xnT1', 'wlo'], writes=[bn])
                    for kc in range(8):
                        k.op('pe', lambda e, kc=kc, bank=bank: e.matmul(bank[:, 0:512], xloT[:, kc, :], win[:, kc, 0:512], start=False, stop=(kc == 7)), reads=['xloT', 'win'], writes=[bn])
                pc0, pres = pcol(ci)
                evac(proj[:, pc0:pc0 + cw], bank[:, 0:cw], [bn], [pres])
                if ci == 2:
                    pr3 = [('proj', 0), ('proj', 1), ('proj', 2)]
                    if not samp:
                        k.dma('sp', kp[i * 128:(i + 1) * 128, :], proj[:, 512:1024], reads=[('proj', 1)], writes=['kp'])
                        k.dma('sp', vp[i * 128:(i + 1) * 128, :], proj[:, 1024:1536], reads=[('proj', 2)], writes=['vp'])
                        k.op('pool', lambda e, i=i: e.tensor_copy(out=vaug[:, i, :, 0:64], in_=proj[:, 1024:1536].rearrange("p (h d) -> p h d", h=8)), reads=[('proj', 2)], writes=['vaug'])
                    else:
                        k.dma('sp', ks[:, :], proj[0:NS, 512:1024], reads=[('proj', 1)], writes=['ks'])
                        k.dma('sp', vs[:, :], proj[0:NS, 1024:1536], reads=[('proj', 2)], writes=['vs'])
                        k.dma('sp', QKVs[:, :], proj[0:NS, 0:1536], reads=pr3, writes=['QKVs'])
            pr4 = [('proj', 0), ('proj', 1), ('proj', 2), ('proj', 3)]
            if not samp:
                k.dma('sp', P[1 + i * 128:1 + (i + 1) * 128, :], proj[:, 0:1792], reads=pr4, writes=['P'])
                if i == 31:
                    k.dma('sp', shp[0:1, :], proj[127:128, 0:1792], reads=pr4, writes=['shp'])
            else:
                k.dma('sp', P[1 + T:1 + T + NS, :], proj[0:NS, 0:1792], reads=pr4, writes=['P'])
                k.dma('sp', shs[:, :], proj[0:NS, 0:1792], reads=pr4, writes=['shs'])
                continue
            tcol = (i % 4) * 128
            nq = i // 2
            for h in range(8):
                bank = pb[2 + h // 4]; bn = 'pb%d' % (2 + h // 4); col = (h % 4) * 128
                for kc in range(8):
                    k.op('pe', lambda e, kc=kc, h=h, bank=bank, col=col: e.matmul(bank[0:64, col:col + 128], win[:, kc, h * 64:(h + 1) * 64], xnT[:, kc, :], start=(kc == 0), stop=False),
                         reads=['xnT1', 'win'], writes=[bn])
                for kc in range(8):
                    k.op('pe', lambda e, kc=kc, h=h, bank=bank, col=col: e.matmul(bank[0:64, col:col + 128], wlo[:, kc, h * 64:(h + 1) * 64], xnT[:, kc, :], start=False, stop=False),
                         reads=['xnT1', 'wlo'], writes=[bn])
                for kc in range(8):
                    k.op('pe', lambda e, kc=kc, h=h, bank=bank, col=col: e.matmul(bank[0:64, col:col + 128], win[:, kc, h * 64:(h + 1) * 64], xloT[:, kc, :], start=False, stop=(kc == 7)),
                         reads=['xloT', 'win'], writes=[bn])
            for h in range(8):
                bank = pb[2 + h // 4]; bn = 'pb%d' % (2 + h // 4); col = (h % 4) * 128
                k.op('act', lambda e, h=h, bank=bank, col=col: e.activation(out=qTa[0:64, h, tcol:tcol + 128], in_=bank[0:64, col:col + 128], func=AF.Copy, scale=0.125),
                     reads=[bn], writes=['qTa'])
                k.op('dve', lambda e, h=h, bank=bank, col=col: e.tensor_copy(out=qT32[:, h, :], in_=bank[0:64, col:col + 128]), reads=[bn], writes=['qT32'])
            for h in range(8):
                bank = pb[2 + h // 4]; bn = 'pb%d' % (2 + h // 4); col = (h % 4) * 128
                for kc in range(8):
                    k.op('pe', lambda e, kc=kc, h=h, bank=bank, col=col: e.matmul(bank[0:64, col:col + 128], win[:, kc, 512 + h * 64:512 + (h + 1) * 64], xnT[:, kc, :], start=(kc == 0), stop=(kc == 7)),
                         reads=['xnT1', 'win'], writes=[bn])
            for h in range(8):
                bank = pb[2 + h // 4]; bn = 'pb%d' % (2 + h // 4); col = (h % 4) * 128
                k.op('act', lambda e, h=h, bank=bank, col=col: e.activation(out=kTa[h][0:64, i * 128:(i + 1) * 128], in_=bank[0:64, col:col + 128], func=AF.Copy, accum_out=ksum2[:, h, i:i + 1]),
                     reads=[bn], writes=[('kTa', h), 'ksum2'])
            for h in range(8):
                k.op('pe', lambda e, h=h: e.matmul(pb[4][:, h * 16:(h + 1) * 16], qT32[:, h, :], kmT[:, h, :], start=True, stop=True), reads=['qT32', 'kmT'], writes=['pb4'])
            k.op('dve', lambda e: e.tensor_tensor(out=gm[:], in0=pb[4][:, 0:128].rearrange("p (h n) -> p h n", h=8), in1=gmaskc[:, nq, :].unsqueeze(1).broadcast_to([128, 8, 16]), op=ALU.add),
                 reads=['pb4', 'gmaskc'], writes=['gm'])
            for h in range(8):
                k.op('dve', lambda e, h=h: e.max(out=top8[:, h, :], in_=gm[:, h, :]), reads=['gm'], writes=['top8'])
            k.op('dve', lambda e: e.tensor_scalar(out=thr[:], in0=top8[:, :, 2], scalar1=-1e29, scalar2=None, op0=ALU.max), reads=['top8'], writes=['thr'])
            k.op('dve', lambda e: e.tensor_tensor(out=m01[:], in0=gm[:], in1=thr[:].unsqueeze(2).broadcast_to([128, 8, 16]), op=ALU.is_ge), reads=['gm', 'thr'], writes=['gm'])
            k.op('dve', lambda e: e.tensor_tensor(out=m01[:], in0=m01[:], in1=ownhot[:, nq, :].unsqueeze(1).broadcast_to([128, 8, 16]), op=ALU.add), reads=['gm', 'ownhot'], writes=['gm'])
            k.op('dve', lambda e: e.tensor_scalar(out=selpad[:, :, 64:80], in0=m01[:], scalar1=-1.0, scalar2=NBIG, op0=ALU.add, op1=ALU.mult), reads=['gm'], writes=['selpad'])
            for h in range(8):
                bank = pb[2 + h // 4]; bn = 'pb%d' % (2 + h // 4); col = (h % 4) * 128
                k.op('pe', lambda e, h=h, bank=bank, col=col: e.matmul(bank[0:80, col:col + 128], selpad[:, h, :], identf[:, :], start=True, stop=True), reads=['selpad', 'identf'], writes=[bn])
            for g in range(2):
                k.op('act', lambda e, g=g: e.activation(out=qTa[64:80, 4 * g:4 * g + 4, tcol:tcol + 128], in_=pb[2 + g][64:80, :].rearrange("p (h t) -> p h t", h=4), func=AF.Copy),
                     reads=['pb%d' % (2 + g)], writes=['qTa'])
            if i % 2 == 1:
                k.op('dve', lambda e: e.tensor_tensor(out=kmT[:, :, nq], in0=ksum2[:, :, i - 1], in1=ksum2[:, :, i], op=ALU.add), reads=['ksum2'], writes=['kmT'])
            if i % 4 != 3:
                continue
            TQ = i // 4
            nkt = 4 * TQ + 4
            for h in range(8):
                def emit_o(kt):
                    pt_ = pT[kt % 2]; ptn = 'pT%d' % (kt % 2)
                    k.op('pe', lambda e, h=h, kt=kt, pt_=pt_: e.matmul(pb[2][0:65, :], vaug[:, kt, h, :], pt_[:], start=(kt == 0), stop=(kt == nkt - 1)),
                         reads=['vaug', ptn], writes=['pb2'])
                for kt in range(nkt):
                    sbk = pb[5 + kt % 2]; sbn = 'pb%d' % (5 + kt % 2); pt_ = pT[kt % 2]; ptn = 'pT%d' % (kt % 2)
                    k.op('pe', lambda e, h=h, kt=kt, sbk=sbk: e.matmul(sbk[:, :], kTa[h][0:82, kt * 128:(kt + 1) * 128], qTa[0:82, h, :], start=True, stop=True),
                         reads=[('kTa', h), 'qTa'], writes=[sbn])
                    if kt >= 1:
                        emit_o(kt - 1)
                    rel = kt - 4 * TQ + 28
                    if kt >= 4 * TQ:
                        j = kt - 4 * TQ
                        k.op('dve', lambda e, h=h, sbk=sbk, rel=rel, j=j: e.scalar_tensor_tensor(out=stmp[:], in0=sbk[:, :], scalar=ab[:, h, rel:rel + 1], in1=cmask[:, j, :], op0=ALU.add, op1=ALU.add),
                             reads=[sbn, 'ab', 'cmask'], writes=['stmp', 'osb', 'lsb'])
                        k.op('act', lambda e, pt_=pt_: e.activation(out=pt_[:], in_=stmp[:], func=AF.Exp), reads=['stmp'], writes=[ptn])
                    else:
                        k.op('act', lambda e, h=h, sbk=sbk, pt_=pt_, rel=rel: e.activation(out=pt_[:], in_=sbk[:, :], func=AF.Exp, bias=ab[:, h, rel:rel + 1], scale=1.0),
                             reads=[sbn, 'ab'], writes=[ptn])
                emit_o(nkt - 1)
                k.op('act', lambda e: e.activation(out=lsb[64:65, :], in_=pb[2][64:65, :], func=AF.Copy), reads=['pb2', 'stmp'], writes=['lsb'])
                k.op('dve', lambda e: e.reciprocal(out=lsb[64:65, :], in_=lsb[64:65, :]), reads=['lsb'], writes=['lsb'])
                k.op('pe', lambda e: e.matmul(pb[3][0:64, :], ones_sb[64:65, 0:64], lsb[64:65, :], start=True, stop=True), reads=['ones_sb', 'lsb'], writes=['pb3'])
                k.op('act', lambda e: e.activation(out=osb[0:64, :], in_=pb[2][0:64, :], func=AF.Copy), reads=['pb2', 'stmp'], writes=['osb'])
                at = atT[h % 2]; atn = 'atT%d' % (h % 2)
                k.op('dve', lambda e, at=at: e.tensor_tensor(out=at[:], in0=osb[0:64, :], in1=pb[3][0:64, :], op=ALU.mult), reads=['osb', 'pb3', 'stmp'], writes=[atn])
                k.dma('sp', mixT[h * 64:(h + 1) * 64, TQ * 512:(TQ + 1) * 512], at[:], reads=[atn], writes=['mixT'])

    k.barrier()
    with contextlib.ExitStack() as es:
        ptb = SB(es, "ptb", [128, NS * NPG], I32); ptf = SB(es, "ptf", [128, NS * NPG], F32)
        iotaf = SB(es, "iotaf", [128, 1], F32); idx = SB(es, "idx", [128, NS * NPG], I32)
        q4s = SB(es, "q4s", [4, 512], F32)
        qkv4 = SB(es, "qkv4", [4, 1536], F32)
        k.dma('sp', qkv4[:, :], QKVs[:, :], reads=['QKVs'], writes=['qkv4'])
        sel4 = SB(es, "sel4", [4, 4, 128], F32); oh4 = SB(es, "oh4", [128, 4, 4], F32); sel8 = SB(es, "sel8", [8, 4, 4], F32)
        alis = SB(es, "alis", [128, 65, 8], F32); diagm = SB(es, "diagm", [8, 512], F32)
        qrep = SB(es, "qrep", [128, 512], F32)
        kx = SB(es, "kx", [128, 512], F32); vx = SB(es, "vx", [128, 512], F32)
        kpg = [SB(es, "kpg%d" % i, [128, 512], F32) for i in range(3)]
        tmp = SB(es, "tmp", [128, 512], F32)
        sc = SB(es, "sc", [128, 65, 8], F32); pS = SB(es, "pS", [128, 65, 8], F32)
        t4 = SB(es, "t4", [4, 512], F32); gate4 = SB(es, "gate4", [4, 32, 8], F32)
        top84 = SB(es, "top84", [4, 8, 8], F32); m4 = SB(es, "m4", [4, 32, 8], F32)
        psp = SB(es, "psp", [128, 8], F32); rl = SB(es, "rl", [8, 1], F32); o8 = SB(es, "o8", [8, 512], F32)
        at4 = SB(es, "at4", [4, 512], BF16); at4T = SB(es, "at4T", [128, 4, 4], BF16)
        k.dma('sp', ptb[:], pt.rearrange("s p -> (s p)").partition_broadcast(128), writes=['ptb'])
        k.dma('sp', iotaf[:], cin['iotaf'][:, :], writes=['iotaf'])
        for (t_, n_) in ((sel4, 'sel4'), (oh4, 'oh4'), (sel8, 'sel8'), (alis, 'alis')):
            k.dma('sp', t_[:], cin[n_][:, :, :], writes=[n_])
        k.dma('sp', diagm[:], cin['diagm'][:, :], writes=['diagm'])
        k.op('dve', lambda e: e.tensor_copy(out=ptf[:], in_=ptb[:]), reads=['ptb'], writes=['ptf'])
        k.op('dve', lambda e: e.tensor_scalar(out=ptf[:], in0=ptf[:], scalar1=128.0, scalar2=iotaf[:, 0:1], op0=ALU.mult, op1=ALU.add), reads=['ptf', 'iotaf'], writes=['ptf'])
        k.op('dve', lambda e: e.tensor_copy(out=idx[:], in_=ptf[:]), reads=['ptf'], writes=['idx'])
        k.op('dve', lambda e: e.tensor_scalar(out=q4s[:], in0=qkv4[:, 0:512], scalar1=0.125, scalar2=None, op0=ALU.mult), reads=['qkv4'], writes=['q4s'])
        k.op('pool', lambda e: e.memset(kx[:], 0.0), writes=['kx'])
        k.op('pool', lambda e: e.memset(vx[:], 0.0), writes=['vx'])
        for s in range(NS):
            k.op('pe', lambda e, s=s: e.matmul(pb[0][:, :], sel4[0:4, s, :], q4s[0:4, :], start=True, stop=True), reads=['sel4', 'q4s'], writes=['pb0'])
            k.op('act', lambda e: e.activation(out=qrep[:], in_=pb[0][:, :], func=AF.Copy), reads=['pb0'], writes=['qrep'])
            k.dma('sp', kx[0:1, :], qkv4[s:s + 1, 512:1024], reads=['qkv4', 'kx'], writes=['kx'])
            k.dma('sp', vx[0:1, :], qkv4[s:s + 1, 1024:1536], reads=['qkv4', 'vx'], writes=['vx'])
            for pg in range(65):
                if pg < 64:
                    kb = kpg[pg % 3]; kbn = 'kpg%d' % (pg % 3); col = s * NPG + pg
                    k.dma('pool', reads=['idx'], writes=[kbn], fn=lambda e, kb=kb, col=col: e.indirect_dma_start(
                        out=kb[:, :], out_offset=None, in_=ck[:, :], in_offset=bass.IndirectOffsetOnAxis(ap=idx[:, col:col + 1], axis=0)))
                else:
                    kb = kx; kbn = 'kx'
                k.op('dve', lambda e, kb=kb: e.tensor_tensor(out=tmp[:], in0=kb[:], in1=qrep[:], op=ALU.mult), reads=[kbn, 'qrep'], writes=['tmp'])
                k.op('dve', lambda e, pg=pg: e.tensor_reduce(out=sc[:, pg, :], in_=tmp[:].rearrange("p (h d) -> p h d", h=8), axis=AX.X, op=ALU.add), reads=['tmp'], writes=['sc'])
                if pg < 64:
                    n = pg // 2; bank = pb[1 + n % 2]; bn = 'pb%d' % (1 + n % 2)
                    k.op('pe', lambda e, s=s, kb=kb, bank=bank, pg=pg: e.matmul(bank[0:4, :], oh4[:, s, :], kb[:], start=(pg % 2 == 0), stop=(pg % 2 == 1)), reads=[kbn, 'oh4'], writes=[bn])
                    if pg % 2 == 1:
                        k.op('dve', lambda e, bank=bank: e.tensor_tensor(out=t4[:], in0=bank[0:4, :], in1=q4s[:], op=ALU.mult), reads=[bn, 'q4s'], writes=['t4'])
                        k.op('dve', lambda e, n=n: e.tensor_reduce(out=gate4[:, n, :], in_=t4[:].rearrange("p (h d) -> p h d", h=8), axis=AX.X, op=ALU.add), reads=['t4'], writes=['gate4'])
            for h in range(8):
                k.op('dve', lambda e, h=h: e.max(out=top84[:, h, :], in_=gate4[:, :, h]), reads=['gate4'], writes=['top84'])
            for h in range(8):
                k.op('dve', lambda e, h=h: e.tensor_scalar(out=m4[:, :, h], in0=gate4[:, :, h], scalar1=top84[:, h, 2:3], scalar2=None, op0=ALU.is_ge), reads=['gate4', 'top84'], writes=['m4'])
            k.op('dve', lambda e: e.tensor_scalar(out=m4[:].rearrange("p a b -> p (a b)"), in0=m4[:].rearrange("p a b -> p (a b)"), scalar1=-1.0, scalar2=NBIG, op0=ALU.add, op1=ALU.mult), reads=['m4'], writes=['m4'])
            k.op('pe', lambda e, s=s: e.matmul(pb[0][:, 0:256], sel4[0:4, s, :], m4[:].rearrange("p a b -> p (a b)"), start=True, stop=True), reads=['sel4', 'm4'], writes=['pb0'])
            k.op('dve', lambda e: e.tensor_tensor(out=sc[:, 0:64, :].rearrange("p (n two) h -> p n two h", two=2), in0=sc[:, 0:64, :].rearrange("p (n two) h -> p n two h", two=2),
                                                  in1=pb[0][:, 0:256].rearrange("p (n h) -> p n h", h=8).unsqueeze(2).broadcast_to([128, 32, 2, 8]), op=ALU.add), reads=['sc', 'pb0'], writes=['sc'])
            k.op('dve', lambda e: e.tensor_tensor(out=sc[:].rearrange("p a b -> p (a b)"), in0=sc[:].rearrange("p a b -> p (a b)"), in1=alis[:].rearrange("p a b -> p (a b)"), op=ALU.add), reads=['sc', 'alis'], writes=['sc'])
            k.op('act', lambda e: e.activation(out=pS[:].rearrange("p a b -> p (a b)"), in_=sc[:].rearrange("p a b -> p (a b)"), func=AF.Exp), reads=['sc'], writes=['pS'])
            k.op('dve', lambda e: e.tensor_reduce(out=psp[:], in_=pS[:].rearrange("p g h -> p h g"), axis=AX.X, op=ALU.add), reads=['pS'], writes=['psp'])
            k.op('pe', lambda e: e.matmul(pb[3][0:8, 0:1], psp[:], ones_sb[:, 0:1], start=True, stop=True), reads=['psp', 'ones_sb'], writes=['pb3'])
            for pg in range(65):
                if pg < 64:
                    vb = kpg[pg % 3]; vbn = 'kpg%d' % (pg % 3); col = s * NPG + pg
                    k.dma('pool', reads=['idx'], writes=[vbn], fn=lambda e, vb=vb, col=col: e.indirect_dma_start(
                        out=vb[:, :], out_offset=None, in_=cv[:, :], in_offset=bass.IndirectOffsetOnAxis(ap=idx[:, col:col + 1], axis=0)))
                else:
                    vb = vx; vbn = 'vx'
                k.op('pe', lambda e, vb=vb, pg=pg: e.matmul(pb[4][0:8, :], pS[:, pg, :], vb[:], start=(pg == 0), stop=(pg == 64)), reads=[vbn, 'pS'], writes=['pb4'])
            k.op('dve', lambda e: e.reciprocal(out=rl[:], in_=pb[3][0:8, 0:1]), reads=['pb3'], writes=['rl'])
            k.op('dve', lambda e: e.scalar_tensor_tensor(out=o8[:], in0=pb[4][0:8, :], scalar=rl[:, 0:1], in1=diagm[:], op0=ALU.mult, op1=ALU.mult), reads=['pb4', 'rl', 'diagm'], writes=['o8'])
            k.op('pe', lambda e, s=s: e.matmul(pb[5][0:4, :], sel8[0:8, s, :], o8[:], start=(s == 0), stop=(s == NS - 1)), reads=['sel8', 'o8'], writes=['pb5'])
        k.op('act', lambda e: e.activation(out=at4[:], in_=pb[5][0:4, :], func=AF.Copy), reads=['pb5'], writes=['at4'])
        for c in range(4):
            k.op('pe', lambda e, c=c: e.transpose(out=pbT[:, c * 4:c * 4 + 4], in_=at4[0:4, c * 128:(c + 1) * 128], identity=identb[0:4, 0:4]), reads=['at4', 'identb'], writes=['pbT'])
        k.op('act', lambda e: e.activation(out=at4T[:].rearrange("p a b -> p (a b)"), in_=pbT[:, 0:16], func=AF.Copy), reads=['pbT'], writes=['at4T'])
        for c in range(4):
            k.dma('sp', mixT[c * 128:(c + 1) * 128, T:T + NS], at4T[:, c, :], reads=['at4T'], writes=['mixT'])

    k.barrier()
    EM05 = float(np.exp(-0.5))
    with contextlib.ExitStack() as es:
      if KSTOP >= 3:
        def rep(name, src, n):
            t_ = SB(es, name, [128, n], F32)
            k.dma('sp', t_[:], src.partition_broadcast(128), writes=[name])
            return t_
        mu_r = rep("mu_r", mu, 1792); w0_r = rep("w0_r", w0, 512); a0_r = rep("a0_r", a0, 512)
        kk_r = rep("kk_r", k_k, 512); ka_r = rep("ka_r", k_a, 512); rk_r = rep("rk_r", r_k, 512)
        lnw_r = rep("lnw_r", lnw, 512); lnb_r = rep("lnb_r", lnb, 512)
        dup = SB(es, "dup", [64, 512], BF16); iup = SB(es, "iup", [64, 512], BF16); gup = SB(es, "gup", [128, 512], BF16)
        stg2 = SB(es, "stg2", [128, 512], F32)
        for (wsb, wd, nr, nm) in ((dup, decay_up, 64, 'dup'), (iup, iclr_up, 64, 'iup'), (gup, gate_up, 128, 'gup')):
            k.dma('sp', stg2[0:nr, :], wd[:, :], writes=['stg2'])
            k.op('dve', lambda e, wsb=wsb, nr=nr: e.tensor_copy(out=wsb[0:nr, :], in_=stg2[0:nr, :]), reads=['stg2'], writes=[nm])
        pc = SB(es, "pc", [128, 1792], F32); pp = SB(es, "pp", [128, 1792], F32)
        lor = SB(es, "lor", [128, 256], BF16); lorT = SB(es, "lorT", [128, 3, 128], BF16)
        dec = SB(es, "dec", [128, 512], F32); aa = SB(es, "aa", [128, 512], F32); gg = SB(es, "gg", [128, 512], F32)
        kkn = SB(es, "kkn", [128, 512], F32); bb = SB(es, "bb", [128, 512], F32); km = SB(es, "km", [128, 512], F32)
        t5 = SB(es, "t5", [128, 512], F32); s8 = SB(es, "s8", [128, 8], F32); bon = SB(es, "bon", [128, 8], F32)
        WT = SB(es, "WT", [128, 4, 128], F32); V1 = SB(es, "V1", [128, 4, 129, 4], F32)
        rlast = SB(es, "rlast", [128, 4], F32)
        ST = SB(es, "ST", [128, 4, 64], F32)
        LLa = SB(es, "LLa", [128, 64, 128], F32); RRa = SB(es, "RRa", [128, 64, 64], F32)
        LLb = SB(es, "LLb", [128, 64, 128], F32); RRb = SB(es, "RRb", [128, 64, 64], F32)
        def LLp(p): return (LLa, 32 * p) if p < 3 else (LLb, 0)
        def RRp(p): return (RRa, 32 * p) if p < 3 else (RRb, 0)
        sin = SB(es, "sin", [64, 128], F32); sout = SB(es, "sout", [64, 128], F32)
        for t_ in (LLa, LLb):
            k.op('pool', lambda e, t_=t_: e.memset(t_[:].rearrange("p a b -> p (a b)"), 0.0), writes=['LL'])
        for t_ in (RRa, RRb):
            k.op('pool', lambda e, t_=t_: e.memset(t_[:].rearrange("p a b -> p (a b)"), 0.0), writes=['RR'])
        k.op('pool', lambda e: e.memset(V1[:].rearrange("p a b c -> p (a b c)"), 0.0), writes=['V1'])
        k.op('pool', lambda e: e.memset(rlast[:], 0.0), writes=['rlast'])

        def prep(row0, nrows, prev_ap, nm):
            if nrows < 128:
                k.op('pool', lambda e: e.memset(pc[:], 0.0), writes=['pc'])
                k.op('pool', lambda e: e.memset(pp[:], 0.0), writes=['pp'])
            k.dma('sp', pc[0:nrows, :], P[1 + row0:1 + row0 + nrows, :], reads=['P', 'pc'], writes=['pc'])
            k.dma('sp', pp[0:nrows, :], prev_ap, reads=['P', 'pp'], writes=['pp'])
            k.op('dve', lambda e: e.tensor_tensor(out=pp[:], in0=pp[:], in1=pc[:], op=ALU.subtract), reads=['pp', 'pc'], writes=['pp'])
            k.op('pool', lambda e: e.tensor_tensor(out=pp[:], in0=pp[:], in1=mu_r[:], op=ALU.mult), reads=['pp', 'mu_r'], writes=['pp'])
            k.op('dve', lambda e: e.tensor_tensor(out=pc[:], in0=pc[:], in1=pp[:], op=ALU.add), reads=['pp', 'pc'], writes=['pc'])
            r_ = pc[:, 0:512]; xw = pc[:, 512:576]; k_ = pc[:, 576:1088]; v_ = pc[:, 1088:1600]; xa = pc[:, 1600:1664]; xg = pc[:, 1664:1792]
            k.op('act', lambda e: e.activation(out=lor[:, 0:64], in_=xw, func=AF.Tanh), reads=['pc'], writes=['lor'])
            k.op('dve', lambda e: e.tensor_copy(out=lor[:, 64:128], in_=xa), reads=['pc'], writes=['lor'])
            k.op('act', lambda e: e.activation(out=lor[:, 128:256], in_=xg, func=AF.Sigmoid), reads=['pc'], writes=['lor'])
            k.op('pe', lambda e: e.transpose(out=pbT[0:64, 0:128], in_=lor[:, 0:64], identity=identb[:]), reads=['lor', 'identb'], writes=['pbT'])
            k.op('pe', lambda e: e.transpose(out=pbT[0:64, 128:256], in_=lor[:, 64:128], identity=identb[:]), reads=['lor', 'identb'], writes=['pbT'])
            k.op('pe', lambda e: e.transpose(out=pbT[:, 256:384], in_=lor[:, 128:256], identity=identb[:]), reads=['lor', 'identb'], writes=['pbT'])
            k.op('act', lambda e: e.activation(out=lorT[0:64, 0:2, :], in_=pbT[0:64, 0:256].rearrange("p (a b) -> p a b", a=2), func=AF.Copy), reads=['pbT'], writes=['lorT'])
            k.op('act', lambda e: e.activation(out=lorT[:, 2, :], in_=pbT[:, 256:384], func=AF.Copy), reads=['pbT'], writes=['lorT'])
            k.op('pe', lambda e: e.matmul(pb[0][:, :], lorT[0:64, 0, :], dup[:, :], start=True, stop=True), reads=['lorT', 'dup'], writes=['pb0'])
            k.op('pe', lambda e: e.matmul(pb[1][:, :], lorT[0:64, 1, :], iup[:, :], start=True, stop=True), reads=['lorT', 'iup'], writes=['pb1'])
            k.op('pe', lambda e: e.matmul(pb[2][:, :], lorT[:, 2, :], gup[:, :], start=True, stop=True), reads=['lorT', 'gup'], writes=['pb2'])
            k.op('dve', lambda e: e.tensor_tensor(out=dec[:], in0=pb[0][:, :], in1=w0_r[:], op=ALU.add), reads=['pb0', 'w0_r'], writes=['dec'])
            k.op('act', lambda e: e.activation(out=dec[:], in_=dec[:], func=AF.Sigmoid), reads=['dec'], writes=['dec'])
            k.op('act', lambda e: e.activation(out=dec[:], in_=dec[:], func=AF.Exp, scale=-EM05), reads=['dec'], writes=['dec'])
            k.op('dve', lambda e: e.tensor_tensor(out=aa[:], in0=pb[1][:, :], in1=a0_r[:], op=ALU.add), reads=['pb1', 'a0_r'], writes=['aa'])
            k.op('act', lambda e: e.activation(out=aa[:], in_=aa[:], func=AF.Sigmoid), reads=['aa'], writes=['aa'])
            k.op('act', lambda e: e.activation(out=gg[:], in_=pb[2][:, :], func=AF.Copy), reads=['pb2'], writes=['gg'])
            k.op('dve', lambda e: e.tensor_tensor(out=kkn[:], in0=k_, in1=kk_r[:], op=ALU.mult), reads=['pc', 'kk_r'], writes=['kkn'])
            k.op('pool', lambda e: e.tensor_tensor(out=t5[:], in0=kkn[:], in1=kkn[:], op=ALU.mult), reads=['kkn'], writes=['t5'])
            k.op('dve', lambda e: e.tensor_reduce(out=s8[:], in_=t5[:].rearrange("p (h d) -> p h d", h=8), axis=AX.X, op=ALU.add), reads=['t5'], writes=['s8'])
            k.op('dve', lambda e: e.tensor_scalar(out=s8[:], in0=s8[:], scalar1=1e-24, scalar2=None, op0=ALU.max), reads=['s8'], writes=['s8'])
            k.op('act', lambda e: e.activation(out=s8[:], in_=s8[:], func=AF.Sqrt), reads=['s8'], writes=['s8'])
            k.op('dve', lambda e: e.reciprocal(out=s8[:], in_=s8[:]), reads=['s8'], writes=['s8'])
            k.op('dve', lambda e: e.tensor_tensor(out=kkn[:].rearrange("p (h d) -> p h d", h=8), in0=kkn[:].rearrange("p (h d) -> p h d", h=8), in1=s8[:].unsqueeze(2).broadcast_to([128, 8, 64]), op=ALU.mult),
                 reads=['kkn', 's8'], writes=['kkn'])
            k.op('pool', lambda e: e.tensor_tensor(out=bb[:], in0=kkn[:], in1=aa[:], op=ALU.mult), reads=['kkn', 'aa'], writes=['bb'])
            k.op('dve', lambda e: e.tensor_scalar(out=kkn[:], in0=kkn[:], scalar1=-1.0, scalar2=None, op0=ALU.mult), reads=['kkn', 'bb'], writes=['kkn'])
            k.op('dve', lambda e: e.scalar_tensor_tensor(out=km[:], in0=aa[:], scalar=-1.0, in1=ka_r[:], op0=ALU.add, op1=ALU.mult), reads=['aa', 'ka_r'], writes=['km'])
            k.op('dve', lambda e: e.scalar_tensor_tensor(out=km[:], in0=km[:], scalar=1.0, in1=k_, op0=ALU.add, op1=ALU.mult), reads=['km', 'pc'], writes=['km'])
            k.op('pool', lambda e: e.tensor_tensor(out=t5[:], in0=r_, in1=km[:], op=ALU.mult), reads=['pc', 'km', 's8'], writes=['t5'])
            k.op('pool', lambda e: e.tensor_tensor(out=t5[:], in0=t5[:], in1=rk_r[:], op=ALU.mult), reads=['t5', 'rk_r'], writes=['t5'])
            k.op('dve', lambda e: e.tensor_reduce(out=bon[:], in_=t5[:].rearrange("p (h d) -> p h d", h=8), axis=AX.X, op=ALU.add), reads=['t5'], writes=['bon'])
            k.dma('sp', Bs[row0:row0 + nrows, :], bb[0:nrows, :], reads=['bb'], writes=['Bs'])
            k.dma('sp', KMs[row0:row0 + nrows, :], km[0:nrows, :], reads=['km'], writes=['KMs'])
            k.dma('sp', Vs[row0:row0 + nrows, :], pc[0:nrows, 1088:1600], reads=['pc'], writes=['Vs'])
            k.dma('sp', Gs[row0:row0 + nrows, :], gg[0:nrows, :], reads=['gg'], writes=['Gs'])
            k.dma('sp', BON[row0:row0 + nrows, :], bon[0:nrows, :], reads=['bon'], writes=['BON'])
            for p in range(4):
                for (src, sname, bank) in ((dec, 'dec', 3), (kkn, 'kkn', 4), (pc, 'pc', 5)):
                    k.op('pe', lambda e, src=src, p=p, bank=bank: e.matmul(pb[bank][:, p * 128:(p + 1) * 128], src[:, p * 128:(p + 1) * 128], identf[:, :], start=True, stop=True),
                         reads=[sname, 'identf'], writes=['pb%d' % bank])
            k.op('act', lambda e: e.activation(out=WT[:].rearrange("p a b -> p (a b)"), in_=pb[3][:, :], func=AF.Copy), reads=['pb3'], writes=['WT'])
            k.op('dve', lambda e: e.tensor_copy(out=V1[0:64, :, 0, 2], in_=rlast[0:64, :]), reads=['rlast', 'V1'], writes=['V1'])
            k.op('dve', lambda e: e.tensor_copy(out=V1[64:128, :, 0, 3], in_=rlast[64:128, :]), reads=['rlast', 'V1'], writes=['V1'])
            k.op('dve', lambda e: e.tensor_copy(out=V1[0:64, :, 0:128, 0], in_=pb[4][0:64, :].rearrange("p (a b) -> p a b", a=4)), reads=['pb4', 'V1'], writes=['V1'])
            k.op('dve', lambda e: e.tensor_copy(out=V1[64:128, :, 0:128, 1], in_=pb[4][64:128, :].rearrange("p (a b) -> p a b", a=4)), reads=['pb4', 'V1'], writes=['V1'])
            k.op('act', lambda e: e.activation(out=V1[0:64, :, 1:129, 2], in_=pb[5][0:64, :].rearrange("p (a b) -> p a b", a=4), func=AF.Copy), reads=['pb5', 'V1'], writes=['V1'])
            k.op('act', lambda e: e.activation(out=V1[64:128, :, 1:129, 3], in_=pb[5][64:128, :].rearrange("p (a b) -> p a b", a=4), func=AF.Copy), reads=['pb5', 'V1'], writes=['V1'])

        def load_rows(row0, nst):
            for p in range(4):
                LL, lb = LLp(p); RR, rb = RRp(p)
                for hh in range(2):
                    c0 = (2 * p + hh) * 64
                    k.dma('sp', LL[lb + hh:lb + hh + 1, 0:nst, hh * 64:(hh + 1) * 64], Bs[row0:row0 + nst, c0:c0 + 64].unsqueeze(0), reads=['Bs', 'LL'], writes=['LL'])
                    k.dma('sp', LL[lb + 4 + hh:lb + 5 + hh, 0:nst, hh * 64:(hh + 1) * 64], KMs[row0:row0 + nst, c0:c0 + 64].unsqueeze(0), reads=['KMs', 'LL'], writes=['LL'])
                    k.dma('sp', RR[rb + 4 + hh:rb + 5 + hh, 0:nst, :], Vs[row0:row0 + nst, c0:c0 + 64].unsqueeze(0), reads=['Vs', 'RR'], writes=['RR'])

        def steps(nst, slot0=0, dummy_last=False):
            banks = ['pb%d' % (3 + p) for p in range(4)]
            for t in range(nst):
                for p in range(4):
                    k.op('pe', lambda e, p=p, t=t: e.matmul(pb[3 + p][0:4, 0:64], V1[:, p, slot0 + t, :], ST[:, p, :], start=True, stop=True),
                         reads=['V1', ('ST', p)], writes=['pb%d' % (3 + p)])
                for p in range(4):
                    RR, rb = RRp(p)
                    k.op('dve', lambda e, p=p, t=t, RR=RR, rb=rb: e.tensor_copy(out=RR[rb:rb + 4, t, :], in_=pb[3 + p][0:4, 0:64]),
                         reads=['pb%d' % (3 + p), 'RR'], writes=[('RRs', p)])
                if dummy_last and t == nst - 1:
                    continue
                for p in range(4):
                    LL, lb = LLp(p); RR, rb = RRp(p)
                    k.op('pe', lambda e, p=p, t=t, LL=LL, lb=lb, RR=RR, rb=rb: e.matmul(pb[3 + p][:, 64:128], LL[lb:lb + 6, t, :], RR[rb:rb + 6, t, :], start=True, stop=True),
                         reads=['LL', 'RR', ('RRs', p)], writes=['pb%d' % (3 + p)])
                for p in range(4):
                    k.op('dve', lambda e, p=p, t=t: e.scalar_tensor_tensor(out=ST[:, p, :], in0=ST[:, p, :], scalar=WT[:, p, slot0 + t:slot0 + t + 1], in1=pb[3 + p][:, 64:128], op0=ALU.mult, op1=ALU.add),
                         reads=['pb%d' % (3 + p), 'WT', ('ST', p)], writes=[('ST', p)])

        def store_y(dst_rows, nst):
            for p in range(4):
                RR, rb = RRp(p)
                for hh in range(2):
                    c0 = (2 * p + hh) * 64
                    k.dma('sp', dst_rows[:, c0:c0 + 64].unsqueeze(0), RR[rb + 2 + hh:rb + 3 + hh, 0:nst, :], reads=[('RRs', q) for q in range(4)] + ['RR'], writes=['Ysc'])

        def sync_rr():
            pass

        def store_state(dst):
            for p in range(4):
                k.op('pe', lambda e, p=p: e.matmul(pb[2][0:64, 0:128], ST[:, p, :], identf[:, :], start=True, stop=True), reads=[('ST', p), 'identf'], writes=['pb2'])
                k.op('act', lambda e: e.activation(out=sout[:], in_=pb[2][0:64, 0:128], func=AF.Copy), reads=['pb2'], writes=['sout'])
                for hh in range(2):
                    k.dma('sp', dst[2 * p + hh, :, :], sout[:, hh * 64:(hh + 1) * 64], reads=['sout'], writes=['wkvout'])

        for p in range(4):
            k.op('pool', lambda e, p=p: e.memset(ST[:, p, :], 0.0), writes=[('ST', p)])
        for i in PT:
            prep(i * 128, 128, P[i * 128:(i + 1) * 128, :], 'p')
            k.op('dve', lambda e: e.tensor_copy(out=rlast[0:64, :], in_=V1[0:64, :, 128, 2]), reads=['V1'], writes=['rlast'])
            k.op('dve', lambda e: e.tensor_copy(out=rlast[64:128, :], in_=V1[64:128, :, 128, 3]), reads=['V1'], writes=['rlast'])
            for half in range(2):
                load_rows(i * 128 + half * 64, 64)
                steps(64, slot0=half * 64)
                store_y(Ysc[i * 128 + half * 64:i * 128 + (half + 1) * 64, :], 64)
        k.op('dve', lambda e: e.tensor_copy(out=V1[0:64, :, 0, 2], in_=rlast[0:64, :]), reads=['rlast', 'V1'], writes=['V1'])
        k.op('dve', lambda e: e.tensor_copy(out=V1[64:128, :, 0, 3], in_=rlast[64:128, :]), reads=['rlast', 'V1'], writes=['V1'])
        k.op('pool', lambda e: e.memset(V1[:, :, 0, 0:2], 0.0), reads=['V1'], writes=['V1'])
        steps(1, slot0=0, dummy_last=True)
        store_y(Ysc[T:T + 1, :], 1)
        store_state(wkvp)

        prep(T, NS, sshift[:, :], 's')
        for s in range(NS):
            for p in range(4):
                for hh in range(2):
                    k.dma('sp', sin[:, hh * 64:(hh + 1) * 64], swkv[s, 2 * p + hh, :, :], reads=['sin'], writes=['sin'])
                k.op('pe', lambda e: e.matmul(pb[2][:, 0:64], sin[:, :], identf[0:64, 0:64], start=True, stop=True), reads=['sin', 'identf'], writes=['pb2'])
                k.op('act', lambda e, p=p: e.activation(out=ST[:, p, :], in_=pb[2][:, 0:64], func=AF.Copy), reads=['pb2'], writes=[('ST', p)])
            load_rows(T + s, 1)
            for p in range(4):
                LL, lb = LLp(p); RR, rb = RRp(p)
                k.op('pe', lambda e, p=p, s=s: e.matmul(pb[3 + p][0:4, 0:64], V1[:, p, s, :], ST[:, p, :], start=True, stop=True), reads=['V1', ('ST', p)], writes=['pb%d' % (3 + p)])
                k.op('act', lambda e, p=p, RR=RR, rb=rb: e.activation(out=RR[rb:rb + 4, 0, :], in_=pb[3 + p][0:4, 0:64], func=AF.Copy), reads=['pb%d' % (3 + p), 'RR'], writes=[('RRs', p)])
                k.op('pe', lambda e, p=p, LL=LL, lb=lb, RR=RR, rb=rb: e.matmul(pb[3 + p][:, 64:128], LL[lb:lb + 6, 0, :], RR[rb:rb + 6, 0, :], start=True, stop=True), reads=['LL', 'RR', ('RRs', p)], writes=['pb%d' % (3 + p)])
                k.op('dve', lambda e, p=p, s=s: e.scalar_tensor_tensor(out=ST[:, p, :], in0=ST[:, p, :], scalar=WT[:, p, s:s + 1], in1=pb[3 + p][:, 64:128], op0=ALU.mult, op1=ALU.add),
                     reads=['pb%d' % (3 + p), 'WT', ('ST', p)], writes=[('ST', p)])
                k.op('pe', lambda e, p=p, s=s: e.matmul(pb[3 + p][0:4, 0:64], V1[:, p, s + 1, :], ST[:, p, :], start=True, stop=True), reads=['V1', ('ST', p)], writes=['pb%d' % (3 + p)])
                k.op('act', lambda e, p=p, RR=RR, rb=rb: e.activation(out=RR[rb:rb + 4, 1, :], in_=pb[3 + p][0:4, 0:64], func=AF.Copy), reads=['pb%d' % (3 + p), 'RR'], writes=[('RRs', p)])
            for p in range(4):
                RR, rb = RRp(p)
                for hh in range(2):
                    c0 = (2 * p + hh) * 64
                    k.dma('sp', Yss[s:s + 1, 1, c0:c0 + 64], RR[rb + 2 + hh:rb + 3 + hh, 1, :], reads=[('RRs', q) for q in range(4)] + ['RR'], writes=['Yss'])
            store_state(wkvs[s])

        yv = SB(es, "yv", [128, 512], F32); vv = SB(es, "vv", [128, 512], F32); gv = SB(es, "gv", [128, 512], F32)
        bv = SB(es, "bv", [128, 8], F32); m8 = SB(es, "m8", [128, 8], F32); rwb = SB(es, "rwb", [128, 512], BF16)
        rwT = SB(es, "rwT", [128, 4, 128], BF16)
        for i in PT + [32]:
            samp = (i == 32)
            nr = NS if samp else 128
            if samp:
                for (t_, n_) in ((yv, 'yv'), (vv, 'vv'), (gv, 'gv')):
                    k.op('pool', lambda e, t_=t_: e.memset(t_[:], 0.0), writes=[n_])
                k.op('pool', lambda e: e.memset(bv[:], 0.0), writes=['bv'])
                k.dma('sp', yv[0:NS, :], Yss[:, 1, :], reads=['Yss', 'yv'], writes=['yv'])
            else:
                k.dma('sp', yv[:, :], Ysc[i * 128 + 1:(i + 1) * 128 + 1, :], reads=['Ysc'], writes=['yv'])
            r0 = T if samp else i * 128
            k.dma('sp', vv[0:nr, :], Vs[r0:r0 + nr, :], reads=['Vs', 'vv'], writes=['vv'])
            k.dma('sp', gv[0:nr, :], Gs[r0:r0 + nr, :], reads=['Gs', 'gv'], writes=['gv'])
            k.dma('sp', bv[0:nr, :], BON[r0:r0 + nr, :], reads=['BON', 'bv'], writes=['bv'])
            y3 = yv[:].rearrange("p (h d) -> p h d", h=8)
            k.op('dve', lambda e: e.tensor_reduce(out=m8[:], in_=y3, axis=AX.X, op=ALU.add), reads=['yv'], writes=['m8'])
            k.op('dve', lambda e: e.tensor_scalar(out=m8[:], in0=m8[:], scalar1=1.0 / 64, scalar2=None, op0=ALU.mult), reads=['m8'], writes=['m8'])
            k.op('dve', lambda e: e.tensor_tensor(out=y3, in0=y3, in1=m8[:].unsqueeze(2).broadcast_to([128, 8, 64]), op=ALU.subtract), reads=['yv', 'm8'], writes=['yv'])
            k.op('pool', lambda e: e.tensor_tensor(out=t5[:], in0=yv[:], in1=yv[:], op=ALU.mult), reads=['yv'], writes=['t5'])
            k.op('dve', lambda e: e.tensor_reduce(out=m8[:], in_=t5[:].rearrange("p (h d) -> p h d", h=8), axis=AX.X, op=ALU.add), reads=['t5', 'yv'], writes=['m8'])
            k.op('act', lambda e: e.activation(out=m8[:], in_=m8[:], func=AF.Sqrt, scale=1.0 / 64, bias=GN_EPS), reads=['m8'], writes=['m8'])
            k.op('dve', lambda e: e.reciprocal(out=m8[:], in_=m8[:]), reads=['m8'], writes=['m8'])
            k.op('dve', lambda e: e.tensor_tensor(out=y3, in0=y3, in1=m8[:].unsqueeze(2).broadcast_to([128, 8, 64]), op=ALU.mult), reads=['yv', 'm8'], writes=['yv'])
            k.op('pool', lambda e: e.tensor_tensor(out=yv[:], in0=yv[:], in1=lnw_r[:], op=ALU.mult), reads=['yv', 'lnw_r'], writes=['yv'])
            k.op('pool', lambda e: e.tensor_tensor(out=yv[:], in0=yv[:], in1=lnb_r[:], op=ALU.add), reads=['yv', 'lnb_r'], writes=['yv'])
            v3 = vv[:].rearrange("p (h d) -> p h d", h=8)
            k.op('dve', lambda e: e.tensor_tensor(out=v3, in0=v3, in1=bv[:].unsqueeze(2).broadcast_to([128, 8, 64]), op=ALU.mult), reads=['vv', 'bv'], writes=['vv'])
            k.op('dve', lambda e: e.tensor_tensor(out=yv[:], in0=yv[:], in1=vv[:], op=ALU.add), reads=['yv', 'vv'], writes=['yv'])
            k.op('dve', lambda e: e.tensor_tensor(out=rwb[:], in0=yv[:], in1=gv[:], op=ALU.mult), reads=['yv', 'gv'], writes=['rwb'])
            for c in range(4):
                k.op('pe', lambda e, c=c: e.transpose(out=pbT[:, c * 128:(c + 1) * 128], in_=rwb[:, c * 128:(c + 1) * 128], identity=identb[:]), reads=['rwb', 'identb'], writes=['pbT'])
            k.op('act', lambda e: e.activation(out=rwT[:].rearrange("p a b -> p (a b)"), in_=pbT[:, 0:512], func=AF.Copy), reads=['pbT'], writes=['rwT'])
            for c in range(4):
                k.dma('sp', mixT[512 + c * 128:512 + (c + 1) * 128, r0:r0 + 128], rwT[:, c, :], reads=['rwT'], writes=['mixT'])

    k.barrier()
    with contextlib.ExitStack() as es:
      if KSTOP >= 4:
        wo = SB(es, "wo", [128, 8, D], BF16); wu = SB(es, "wu", [128, 8, 4096], BF16); wd = SB(es, "wd", [128, 32, D], BF16)
        stg3 = SB(es, "stg3", [128, 3328], F32)
        gf_r = SB(es, "gf_r", [128, D], F32)
        k.dma('sp', gf_r[:], gf.partition_broadcast(128), writes=['gf_r'])
        load_weight(wo, w_out, 8, D, None, stg3, 'wo')
        load_weight(wu, w_up, 8, 4096, g2, stg3, 'wu')
        load_weight(wd, w_dn, 32, D, None, stg3, 'wd')
        mxT = SB(es, "mxT", [128, 8, 128], BF16)
        xt3 = SB(es, "xt3", [128, D], F32); hh_ = SB(es, "hh_", [128, D], F32)
        ss3 = SB(es, "ss3", [128, 1], F32); rs3 = SB(es, "rs3", [128, 1], F32)
        hnb = SB(es, "hnb", [128, D], BF16); hnT = SB(es, "hnT", [128, 8, 128], BF16)
        ur = SB(es, "ur", [128, 512], F32); uT = SB(es, "uT", [128, 32, 128], BF16)
        yo = SB(es, "yo", [128, D], F32)
        for i in PT + [32]:
            samp = (i == 32)
            r0 = T if samp else i * 128
            k.dma('sp', mxT[:, :, :], mixT[:, r0:r0 + 128].rearrange("(a p) t -> p a t", p=128), reads=['mixT'], writes=['mxT'])
            if samp:
                k.op('pool', lambda e: e.memset(xt3[:], 0.0), writes=['xt3'])
                k.dma('sp', xt3[0:NS, :], xsm[:, :], reads=['xt3'], writes=['xt3'])
            else:
                k.dma('sp', xt3[:], xp[i * 128:(i + 1) * 128, :], writes=['xt3'])
            for c in range(2):
                for kc in range(8):
                    k.op('pe', lambda e, c=c, kc=kc: e.matmul(pb[c][:, :], mxT[:, kc, :], wo[:, kc, c * 512:(c + 1) * 512], start=(kc == 0), stop=(kc == 7)), reads=['mxT', 'wo'], writes=['pb%d' % c])
                k.op('dve', lambda e, c=c: e.tensor_tensor(out=hh_[:, c * 512:(c + 1) * 512], in0=pb[c][:, :], in1=xt3[:, c * 512:(c + 1) * 512], op=ALU.add), reads=['pb%d' % c, 'xt3'], writes=['hh_'])
            rmsnorm_T((ss3, rs3, hnb, hnT), hh_, 'hh_', '3')
            for fg in range(8):
                bank = pb[2 + fg % 2]; bn = 'pb%d' % (2 + fg % 2)
                for f4 in range(4):
                    fc = fg * 4 + f4
                    for kc in range(8):
                        k.op('pe', lambda e, fc=fc, f4=f4, kc=kc, bank=bank: e.matmul(bank[:, f4 * 128:(f4 + 1) * 128], wu[:, kc, fc * 128:(fc + 1) * 128], hnT[:, kc, :], start=(kc == 0), stop=(kc == 7)),
                             reads=['xnT3', 'wu'], writes=[bn])
                k.op('act', lambda e, bank=bank: e.activation(out=ur[:], in_=bank[:, :], func=AF.Relu), reads=[bn], writes=['ur'])
                k.op('dve', lambda e, fg=fg: e.tensor_tensor(out=uT[:, fg * 4:(fg + 1) * 4, :].rearrange("p a b -> p (a b)"), in0=ur[:], in1=ur[:], op=ALU.mult), reads=['ur'], writes=['uT'])
            for c in range(2):
                bank = pb[4 + c]; bn = 'pb%d' % (4 + c)
                for fc in range(32):
                    k.op('pe', lambda e, c=c, fc=fc, bank=bank: e.matmul(bank[:, :], uT[:, fc, :], wd[:, fc, c * 512:(c + 1) * 512], start=(fc == 0), stop=(fc == 31)), reads=['uT', 'wd'], writes=[bn])
                k.op('dve', lambda e, c=c, bank=bank: e.tensor_tensor(out=hh_[:, c * 512:(c + 1) * 512], in0=bank[:, :], in1=hh_[:, c * 512:(c + 1) * 512], op=ALU.add), reads=[bn, 'hh_'], writes=['hh_'])
            k.op('act', lambda e: e.activation(out=hnb[:], in_=hh_[:], func=AF.Square, accum_out=ss3[:, 0:1]), reads=['hh_'], writes=['xnb3', 'ss3'])
            k.op('act', lambda e: e.activation(out=rs3[:], in_=ss3[:], func=AF.Sqrt, scale=1.0 / D, bias=RMS_EPS), reads=['ss3'], writes=['rs3'])
            k.op('dve', lambda e: e.reciprocal(out=rs3[:], in_=rs3[:]), reads=['rs3'], writes=['rs3'])
            k.op('dve', lambda e: e.scalar_tensor_tensor(out=yo[:], in0=hh_[:], scalar=rs3[:, 0:1], in1=gf_r[:], op0=ALU.mult, op1=ALU.mult), reads=['hh_', 'rs3', 'gf_r'], writes=['yo'])
            if samp:
                k.dma('sp', ysm[:, :], yo[0:NS, :], reads=['yo'], writes=['ysm'])
            else:
                k.dma('sp', yp[i * 128:(i + 1) * 128, :], yo[:], reads=['yo'], writes=['yp'])

    k.wait_all('sp')
    top.close()
    return nc, consts, k


_CACHE = {}


def kernel(**inp):
    f = lambda a: np.ascontiguousarray(np.asarray(a))
    n_pool = inp['cache_k'].shape[1]
    if n_pool not in _CACHE:
        _CACHE[n_pool] = build(n_pool)
    nc, consts, _k = _CACHE[n_pool]
    ck = f(inp['cache_k'][0]).reshape(n_pool * 128, 512)
    cv = f(inp['cache_v'][0]).reshape(n_pool * 128, 512)
    shared = {
        'ck': ck, 'cv': cv,
        'w_in': f(inp['w_in'][0]), 'w_out': f(inp['w_out'][0]), 'w_up': f(inp['w_ffn_up'][0]), 'w_dn': f(inp['w_ffn_down'][0]),
        'g1': f(inp['norm_mix_g'][0]), 'g2': f(inp['norm_ffn_g'][0]), 'gf': f(inp['norm_final_g']),
        'mu': f(inp['mu_shift'][0]), 'w0': f(inp['decay_w0'][0]), 'decay_up': f(inp['decay_up'][0]),
        'a0': f(inp['iclr_a0'][0]), 'iclr_up': f(inp['iclr_up'][0]), 'gate_up': f(inp['gate_up'][0]),
        'k_k': f(inp['k_k'][0]), 'k_a': f(inp['k_a'][0]), 'r_k': f(inp['r_k'][0]).reshape(512),
        'lnw': f(inp['ln_x_w'][0]), 'lnb': f(inp['ln_x_b'][0]),
    }
    for n, a in consts.items():
        shared['c_' + n] = a
    in_maps = []
    for c in range(8):
        m = dict(shared)
        m['xp'] = f(inp['x_prompt'][c % 4])
        m['xsm'] = f(inp['x_sample'][4 * c:4 * c + 4, 0])
        m['pt'] = f(inp['page_table'][4 * c:4 * c + 4]).astype(np.int32)
        m['swkv'] = f(inp['state_wkv'][0, 4 * c:4 * c + 4])
        m['sshift'] = f(inp['state_shift'][0, 4 * c:4 * c + 4])
        in_maps.append(m)
    res = run_bass_kernel_spmd(nc, in_maps, core_ids=list(range(8)))
    R = res.results
    y_prompt = np.stack([R[b]['yp'] for b in range(4)])
    y_sample = np.concatenate([R[c]['ysm'] for c in range(8)])[:, None, :]
    k_prompt = np.stack([R[b]['kp'] for b in range(4)]).reshape(1, 4, T, 8, 64)
    v_prompt = np.stack([R[b]['vp'] for b in range(4)]).reshape(1, 4, T, 8, 64)
    wkv_prompt = np.stack([R[b]['wkvp'] for b in range(4)])[None]
    shift_prompt = np.stack([R[b]['shp'][0] for b in range(4)])[None]
    k_sample = np.concatenate([R[c]['ks'] for c in range(8)]).reshape(1, 32, 1, 8, 64)
    v_sample = np.concatenate([R[c]['vs'] for c in range(8)]).reshape(1, 32, 1, 8, 64)
    wkv_sample = np.concatenate([R[c]['wkvs'] for c in range(8)])[None]
    shift_sample = np.concatenate([R[c]['shs'] for c in range(8)])[None]
    return tuple(np.ascontiguousarray(a, dtype=np.float32) for a in
                 (y_prompt, y_sample, k_prompt, v_prompt, wkv_prompt, shift_prompt, k_sample, v_sample, wkv_sample, shift_sample))
```
